# Optimizing a Trainium2 kernel written in Bass

```python
import math
import jax, jax.numpy as jnp
from jax import lax
import numpy as np

D_MODEL = 1024
BATCH = 32
SEQ = 2048
DEPTH = 2

N_BRANCH = 4
BRANCH_WIDTH = D_MODEL // 4
HEAD_DIM = 64
N_HEADS = BRANCH_WIDTH // HEAD_DIM
CONV_WIDTH = 4
GDN_CHUNK = 64
MOBA_BLOCK = 256
MOBA_TOPK = 3
MOBA_QCHUNK = 32
SB_QBLOCK = 128
SSD_STATE = 128
SSD_GROUPS = 2
SSD_CHUNK = 128
D_FF = 4 * D_MODEL
EPS = 1e-6

GDN_IN = 4 * BRANCH_WIDTH + 2 * N_HEADS
MOBA_IN = 3 * BRANCH_WIDTH
SB_IN = 3 * BRANCH_WIDTH
SSD_CONV_DIM = BRANCH_WIDTH + 2 * SSD_GROUPS * SSD_STATE
SSD_IN = BRANCH_WIDTH + SSD_CONV_DIM + N_HEADS
OFF_GDN = 0
OFF_MOBA = OFF_GDN + GDN_IN
OFF_SB = OFF_MOBA + MOBA_IN
OFF_SSD = OFF_SB + SB_IN
IN_WIDTH = OFF_SSD + SSD_IN

kernel_name = "hybrid_gated_parallel_mixers"


def rms_norm(x, gain):
    xf = x.astype(jnp.float32)
    y = xf * lax.rsqrt(jnp.mean(xf * xf, axis=-1, keepdims=True) + EPS)
    return (y * gain.astype(jnp.float32)).astype(x.dtype)


def l2_normalize(x):
    return x * lax.rsqrt(jnp.sum(x * x, axis=-1, keepdims=True) + EPS)


def causal_dwconv(x, w):
    return lax.conv_general_dilated(
        x, w.astype(jnp.float32)[:, None, :], window_strides=(1,),
        padding=[(CONV_WIDTH - 1, 0)], dimension_numbers=('NWC', 'WIO', 'NWC'),
        feature_group_count=x.shape[-1])


def chunk_gated_delta_rule(q, k, v, g, beta):
    Bsz, T, H, D = q.shape
    C = GDN_CHUNK
    NC = T // C
    q = l2_normalize(q) * (D ** -0.5)
    k = l2_normalize(k)

    def to_chunks(a):
        return a.reshape(Bsz, NC, C, H, D).transpose(0, 3, 1, 2, 4)

    q, k, v = to_chunks(q), to_chunks(k), to_chunks(v)
    g = g.reshape(Bsz, NC, C, H).transpose(0, 3, 1, 2)
    beta = beta.reshape(Bsz, NC, C, H).transpose(0, 3, 1, 2)
    gc = jnp.cumsum(g, axis=-1)
    idx = jnp.arange(C)
    incl = idx[:, None] >= idx[None, :]
    strict = idx[:, None] > idx[None, :]
    decay = jnp.exp(jnp.where(incl, gc[..., :, None] - gc[..., None, :], -jnp.inf))
    kb = k * beta[..., None]
    vb = v * beta[..., None]
    a_kk = jnp.where(strict, jnp.einsum('bhncd,bhnsd->bhncs', kb, k) * decay, 0.0)
    tmat = a_kk + jnp.eye(C, dtype=jnp.float32)
    u = lax.linalg.triangular_solve(tmat, vb, left_side=True, lower=True)
    w = lax.linalg.triangular_solve(tmat, kb * jnp.exp(gc)[..., None], left_side=True, lower=True)
    a_qk = jnp.einsum('bhncd,bhnsd->bhncs', q, k) * decay
    q_dec = q * jnp.exp(gc)[..., None]
    k_dec = k * jnp.exp(gc[..., -1:] - gc)[..., None]
    g_tot = jnp.exp(gc[..., -1])

    def step(S, xs):
        u_c, w_c, q_c, k_c, a_c, gt = xs
        v_new = u_c - jnp.einsum('bhcd,bhde->bhce', w_c, S)
        o = jnp.einsum('bhcd,bhde->bhce', q_c, S) + jnp.einsum('bhcs,bhse->bhce', a_c, v_new)
        S = S * gt[..., None, None] + jnp.einsum('bhcd,bhce->bhde', k_c, v_new)
        return S, o

    xs = tuple(jnp.moveaxis(a, 2, 0) for a in (u, w, q_dec, k_dec, a_qk, g_tot))
    S0 = jnp.zeros((Bsz, H, D, D), jnp.float32)
    _, o = lax.scan(step, S0, xs)
    return o.transpose(1, 0, 3, 2, 4).reshape(Bsz, T, H, D)


def gdn_branch(p, conv_w, a_log, dt_bias, norm_w):
    Bsz, T, _ = p.shape
    W = BRANCH_WIDTH
    qkv = jax.nn.silu(causal_dwconv(p[..., :3 * W], conv_w))
    q = qkv[..., :W].reshape(Bsz, T, N_HEADS, HEAD_DIM)
    k = qkv[..., W:2 * W].reshape(Bsz, T, N_HEADS, HEAD_DIM)
    v = qkv[..., 2 * W:].reshape(Bsz, T, N_HEADS, HEAD_DIM)
    z = p[..., 3 * W:4 * W].reshape(Bsz, T, N_HEADS, HEAD_DIM)
    beta = jax.nn.sigmoid(p[..., 4 * W:4 * W + N_HEADS])
    g = -jnp.exp(a_log.astype(jnp.float32)) * jax.nn.softplus(
        p[..., 4 * W + N_HEADS:] + dt_bias.astype(jnp.float32))
    o = chunk_gated_delta_rule(q, k, v, g, beta)
    o = rms_norm(o, norm_w) * jax.nn.silu(z)
    return o.reshape(Bsz, T, W)


def alibi_slopes(n_heads):
    return jnp.asarray([2.0 ** (-8.0 * (i + 1) / n_heads) for i in range(n_heads)], jnp.float32)


def moba_attention(q, k, v):
    Bsz, T, H, D = q.shape
    BS = MOBA_BLOCK
    Tp = -(-T // BS) * BS
    pad = ((0, 0), (0, Tp - T), (0, 0), (0, 0))
    q, k, v = (jnp.pad(a, pad).transpose(0, 2, 1, 3) for a in (q, k, v))
    NB = Tp // BS
    scale = D ** -0.5
    slopes = alibi_slopes(H)
    kb = k.reshape(Bsz, H, NB, BS, D)
    vb = v.reshape(Bsz, H, NB, BS, D)
    kmean = jnp.mean(kb, axis=3)
    gate = jnp.einsum('bhtd,bhnd->bhtn', q, kmean)
    qblk = jnp.arange(Tp) // BS
    past = jnp.arange(NB)[None, :] < qblk[:, None]
    gate = jnp.where(past, gate, -jnp.inf)
    n_sel = min(MOBA_TOPK, NB)
    _, sel = lax.top_k(gate, n_sel)
    sel_valid = sel < qblk[:, None]

    QC = MOBA_QCHUNK
    nq = Tp // QC

    def chunked(a):
        return jnp.moveaxis(a.reshape(Bsz, H, nq, QC, *a.shape[3:]), 2, 0)

    bi = jnp.arange(Bsz)[:, None, None, None]
    hi = jnp.arange(H)[None, :, None, None]

    def step(args):
        qc, sc, vc, c = args
        t = c * QC + jnp.arange(QC)
        own = (c * QC) // BS
        ks = kb[bi, hi, sc]
        vs = vb[bi, hi, sc]
        s_pos = sc[..., None] * BS + jnp.arange(BS)
        dist = (t[None, None, :, None, None] - s_pos).astype(jnp.float32)
        s_sel = jnp.einsum('bhqd,bhqksd->bhqks', qc, ks) * scale - slopes[None, :, None, None, None] * dist
        s_sel = jnp.where(vc[..., None], s_sel, -jnp.inf)
        k_own = lax.dynamic_index_in_dim(kb, own, axis=2, keepdims=False)
        v_own = lax.dynamic_index_in_dim(vb, own, axis=2, keepdims=False)
        own_pos = own * BS + jnp.arange(BS)
        dist_own = (t[:, None] - own_pos[None, :]).astype(jnp.float32)
        s_own = jnp.einsum('bhqd,bhsd->bhqs', qc, k_own) * scale - slopes[None, :, None, None] * dist_own
        s_own = jnp.where(own_pos[None, :] <= t[:, None], s_own, -jnp.inf)
        scores = jnp.concatenate([s_sel.reshape(Bsz, H, QC, n_sel * BS), s_own], axis=-1)
        prob = jax.nn.softmax(scores, axis=-1)
        p_sel = prob[..., :n_sel * BS].reshape(Bsz, H, QC, n_sel, BS)
        p_own = prob[..., n_sel * BS:]
        return jnp.einsum('bhqks,bhqksd->bhqd', p_sel, vs) + jnp.einsum('bhqs,bhsd->bhqd', p_own, v_own)

    out = lax.map(step, (chunked(q), chunked(sel), chunked(sel_valid), jnp.arange(nq)))
    out = jnp.moveaxis(out, 0, 2).reshape(Bsz, H, Tp, D)[:, :, :T]
    return out.transpose(0, 2, 1, 3)


def moba_branch(p):
    Bsz, T, _ = p.shape
    W = BRANCH_WIDTH
    q, k, v = (p[..., i * W:(i + 1) * W].reshape(Bsz, T, N_HEADS, HEAD_DIM) for i in range(3))
    return moba_attention(q, k, v).reshape(Bsz, T, W)


def stick_breaking_attention(q, k, v):
    q, k, v = (a.transpose(0, 2, 1, 3) for a in (q, k, v))
    T = q.shape[2]
    scale = q.shape[-1] ** -0.5
    outs = []
    for i in range(T // SB_QBLOCK):
        t0, t1 = i * SB_QBLOCK, (i + 1) * SB_QBLOCK
        z = jnp.einsum('bhqd,bhsd->bhqs', q[:, :, t0:t1], k[:, :, :t1]) * scale
        causal = jnp.arange(t1)[None, :] < jnp.arange(t0, t1)[:, None]
        log_1m = jnp.where(causal, jax.nn.log_sigmoid(-z), 0.0)
        after = lax.cumsum(log_1m, axis=3, reverse=True) - log_1m
        weight = jnp.exp(jnp.where(causal, jax.nn.log_sigmoid(z) + after, -jnp.inf))
        outs.append(jnp.einsum('bhqs,bhsd->bhqd', weight, v[:, :, :t1]))
    return jnp.concatenate(outs, axis=2).transpose(0, 2, 1, 3)


def sb_branch(p):
    Bsz, T, _ = p.shape
    W = BRANCH_WIDTH
    q, k, v = (p[..., i * W:(i + 1) * W].reshape(Bsz, T, N_HEADS, HEAD_DIM) for i in range(3))
    return stick_breaking_attention(q, k, v).reshape(Bsz, T, W)


def ssd_chunked(xh, dt, a, bm, cm):
    Bsz, T, H, P = xh.shape
    N = bm.shape[-1]
    L = SSD_CHUNK
    NC = T // L
    xc = xh.reshape(Bsz, NC, L, H, P)
    dtc = dt.reshape(Bsz, NC, L, H)
    bc = bm.reshape(Bsz, NC, L, H, N)
    cc = cm.reshape(Bsz, NC, L, H, N)
    acs = jnp.cumsum(jnp.moveaxis(dtc * a, 3, 1), axis=-1)
    idx = jnp.arange(L)
    incl = idx[:, None] >= idx[None, :]
    lmat = jnp.exp(jnp.where(incl, acs[..., :, None] - acs[..., None, :], -jnp.inf))
    xdt = xc * dtc[..., None]
    scores = jnp.einsum('bclhn,bcshn->bhcls', cc, bc) * lmat
    y_diag = jnp.einsum('bhcls,bcshp->bclhp', scores, xdt)
    decay_states = jnp.exp(acs[..., -1:] - acs)
    states = jnp.einsum('bclhn,bhcl,bclhp->bchpn', bc, decay_states, xdt)
    chunk_decay = jnp.exp(acs[..., -1])

    def step(h, inp):
        st, dec = inp
        return h * dec[..., None, None] + st, h

    h0 = jnp.zeros((Bsz, H, P, N), jnp.float32)
    _, h_prev = lax.scan(step, h0, (jnp.moveaxis(states, 1, 0), jnp.moveaxis(chunk_decay, 2, 0)))
    h_prev = jnp.moveaxis(h_prev, 0, 1)
    y_off = jnp.einsum('bclhn,bchpn,bhcl->bclhp', cc, h_prev, jnp.exp(acs))
    return (y_diag + y_off).reshape(Bsz, T, H, P)


def ssd_branch(p, conv_w, conv_b, a_log, dt_bias, d_skip, norm_w):
    Bsz, T, _ = p.shape
    W = BRANCH_WIDTH
    GN = SSD_GROUPS * SSD_STATE
    z = p[..., :W]
    xbc = jax.nn.silu(causal_dwconv(p[..., W:W + SSD_CONV_DIM], conv_w) + conv_b.astype(jnp.float32))
    dt = jax.nn.softplus(p[..., W + SSD_CONV_DIM:] + dt_bias.astype(jnp.float32))
    xh = xbc[..., :W].reshape(Bsz, T, N_HEADS, HEAD_DIM)
    rep = N_HEADS // SSD_GROUPS
    bm = jnp.repeat(xbc[..., W:W + GN].reshape(Bsz, T, SSD_GROUPS, SSD_STATE), rep, axis=2)
    cm = jnp.repeat(xbc[..., W + GN:].reshape(Bsz, T, SSD_GROUPS, SSD_STATE), rep, axis=2)
    a = -jnp.exp(a_log.astype(jnp.float32))
    y = ssd_chunked(xh, dt, a, bm, cm) + d_skip.astype(jnp.float32)[:, None] * xh
    return rms_norm(y.reshape(Bsz, T, W) * jax.nn.silu(z), norm_w)


def setup_inputs(seed: int = 0) -> dict:
    key = jax.random.key(seed)
    ks = jax.random.split(key, 24)
    f32 = jnp.float32
    nrm = lambda k, shape, s: jax.random.normal(k, shape, f32) * s
    gain = lambda k, shape: 1.0 + 0.05 * jax.random.normal(k, shape, f32)

    def dt_bias_init(k):
        dt0 = jnp.exp(jax.random.uniform(k, (DEPTH, N_HEADS), f32, math.log(1e-3), math.log(1e-1)))
        return dt0 + jnp.log(-jnp.expm1(-dt0))

    def a_log_init(k):
        return jnp.log(jax.random.uniform(k, (DEPTH, N_HEADS), f32, 1.0, 16.0))

    return {
        'x': jax.random.normal(ks[0], (BATCH, SEQ, D_MODEL), f32),
        'norm_mix_pre': gain(ks[1], (DEPTH, D_MODEL)),
        'norm_mix_post': gain(ks[2], (DEPTH, D_MODEL)),
        'norm_ffn_pre': gain(ks[3], (DEPTH, D_MODEL)),
        'norm_ffn_post': gain(ks[4], (DEPTH, D_MODEL)),
        'w_in': nrm(ks[5], (DEPTH, D_MODEL, IN_WIDTH), D_MODEL ** -0.5),
        'gdn_conv': nrm(ks[6], (DEPTH, CONV_WIDTH, 3 * BRANCH_WIDTH), CONV_WIDTH ** -0.5),
        'gdn_a_log': a_log_init(ks[7]),
        'gdn_dt_bias': dt_bias_init(ks[8]),
        'gdn_norm': gain(ks[9], (DEPTH, HEAD_DIM)),
        'ssd_conv': nrm(ks[10], (DEPTH, CONV_WIDTH, SSD_CONV_DIM), CONV_WIDTH ** -0.5),
        'ssd_conv_bias': nrm(ks[11], (DEPTH, SSD_CONV_DIM), 0.02),
        'ssd_a_log': a_log_init(ks[12]),
        'ssd_dt_bias': dt_bias_init(ks[13]),
        'ssd_d': gain(ks[14], (DEPTH, N_HEADS)),
        'ssd_norm': gain(ks[15], (DEPTH, BRANCH_WIDTH)),
        'w_gate': nrm(ks[16], (DEPTH, N_BRANCH, D_MODEL, D_MODEL), D_MODEL ** -0.5),
        'w_branch': nrm(ks[17], (DEPTH, N_BRANCH, BRANCH_WIDTH, D_MODEL), BRANCH_WIDTH ** -0.5),
        'w_out': nrm(ks[18], (DEPTH, D_MODEL, D_MODEL), D_MODEL ** -0.5),
        'w_up': nrm(ks[19], (DEPTH, D_MODEL, D_FF), D_MODEL ** -0.5),
        'w_down': nrm(ks[20], (DEPTH, D_FF, D_MODEL), D_FF ** -0.5),
    }


def reference(x, norm_mix_pre, norm_mix_post, norm_ffn_pre, norm_ffn_post, w_in,
              gdn_conv, gdn_a_log, gdn_dt_bias, gdn_norm,
              ssd_conv, ssd_conv_bias, ssd_a_log, ssd_dt_bias, ssd_d, ssd_norm,
              w_gate, w_branch, w_out, w_up, w_down):
    for l in range(DEPTH):
        h = rms_norm(x, norm_mix_pre[l])
        p = jnp.matmul(h, w_in[l]).astype(jnp.float32)
        y_a = gdn_branch(p[..., OFF_GDN:OFF_MOBA], gdn_conv[l], gdn_a_log[l], gdn_dt_bias[l], gdn_norm[l])
        y_b = moba_branch(p[..., OFF_MOBA:OFF_SB])
        y_c = sb_branch(p[..., OFF_SB:OFF_SSD])
        y_d = ssd_branch(p[..., OFF_SSD:IN_WIDTH], ssd_conv[l], ssd_conv_bias[l], ssd_a_log[l],
                         ssd_dt_bias[l], ssd_d[l], ssd_norm[l])
        branches = (y_a, y_b, y_c, y_d)
        merged = jax.nn.sigmoid(jnp.matmul(h, w_gate[l, 0])) * jnp.matmul(branches[0].astype(x.dtype), w_branch[l, 0])
        for g in range(1, N_BRANCH):
            gate = jax.nn.sigmoid(jnp.matmul(h, w_gate[l, g]))
            merged = merged + gate * jnp.matmul(branches[g].astype(x.dtype), w_branch[l, g])
        mix = jnp.matmul(merged, w_out[l])
        x = x + rms_norm(mix, norm_mix_post[l])
        h = rms_norm(x, norm_ffn_pre[l])
        f = jnp.matmul(jnp.square(jax.nn.relu(jnp.matmul(h, w_up[l]))), w_down[l])
        x = x + rms_norm(f, norm_ffn_post[l])
    return x
```

```python
import numpy as np
from contextlib import ExitStack, contextmanager
import concourse.bass as bass
import concourse.mybir as mybir
from concourse.bass_utils import run_bass_kernel_spmd

F32 = mybir.dt.float32
BF16 = mybir.dt.bfloat16
AF = mybir.ActivationFunctionType
ALU = mybir.AluOpType
AX = mybir.AxisListType

ENGINES = ("pe", "act", "dve", "pool", "sp")


class Res:
    __slots__ = ("name", "last_w", "readers", "slot", "t", "excl", "pe_last")

    def __init__(self, name, t=None, excl=False):
        self.name = name
        self.excl = excl
        self.pe_last = None
        self.last_w = None
        self.readers = []
        self.slot = None
        self.t = t

    def __getitem__(self, idx):
        return V(self, self.t[idx])


class V:
    __slots__ = ("res", "ap")

    def __init__(self, res, ap):
        self.res = res
        self.ap = ap

    def __getitem__(self, idx):
        return V(self.res, self.ap[idx])

    def r(self, pat, **kw):
        return V(self.res, self.ap.rearrange(pat, **kw))

    def un(self, axis):
        return V(self.res, self.ap.unsqueeze(axis))

    def bc(self, shape):
        return V(self.res, self.ap.broadcast_to(list(shape)))


def _ap(x):
    return x.ap if isinstance(x, V) else x


def _rs(*xs):
    return [x.res for x in xs if isinstance(x, V)]


class Op:
    __slots__ = ("eng", "fn", "seq", "waits", "signal", "is_dma", "slot", "cnt",
                 "clock", "semval", "kind")

    def __init__(self, eng, fn, is_dma=False, slot=None):
        self.eng = eng
        self.fn = fn
        self.is_dma = is_dma
        self.slot = slot
        self.waits = []
        self.signal = False
        self.clock = None
        self.semval = None
        self.cnt = 0


class Prog:
    def __init__(self, nc):
        self.nc = nc
        self.ops = {e: [] for e in ENGINES}
        self.clock = {e: {} for e in ENGINES}
        self.nslots = 0
        self.slot_last = {}
        self.slot_cnt = {}
        self.all_res = []
        self.stack = None
        self.n_wait = 0
        self.free_slots = []

    def sb(self, name, shape, dtype):
        self.uid = getattr(self, "uid", 0) + 1
        name = "%s_%d" % (name, self.uid)
        t = self.stack.enter_context(self.nc.sbuf_tensor(name, list(shape), dtype))
        r = Res(name, t)
        self.all_res.append(r)
        return r

    def ps(self, name, shape, dtype=F32):
        t = self.stack.enter_context(self.nc.psum_tensor(name, list(shape), dtype))
        r = Res(name, t, excl=True)
        self.all_res.append(r)
        return r

    def res(self, name):
        r = Res(name, None)
        self.all_res.append(r)
        return r

    def _key(self, op):
        return ("s", op.slot) if op.is_dma else op.eng

    def _val(self, op):
        return op.cnt if op.is_dma else op.seq

    def _add_dep(self, op, dep, raw, force=False):
        if dep is None or dep is op:
            return
        if not force and not dep.is_dma and not op.is_dma and dep.eng == op.eng:
            if op.eng == "pe":
                return
        k = self._key(dep)
        v = self._val(dep)
        ck = self.clock[op.eng]
        if ck.get(k, -1) >= v:
            return
        op.waits.append(dep)
        dep.signal = True
        for kk, vv in dep.clock.items():
            if ck.get(kk, -1) < vv:
                ck[kk] = vv
        ck[k] = v

    def add(self, eng, fn, reads=(), writes=(), is_dma=False, force_dep=None):
        op = Op(eng, fn, is_dma=is_dma)
        xr = [r for r in reads if r.excl]
        if xr:
            reads = [r for r in reads if not r.excl]
            writes = list(writes) + [r for r in xr if r not in writes]
        lst = self.ops[eng]
        op.seq = len(lst)
        if is_dma:
            tile = None
            for r in list(writes) + list(reads):
                if r.t is not None:
                    tile = r
                    break
            assert tile is not None, "dma needs an sbuf tile resource"
            if tile.slot is None:
                if self.free_slots:
                    tile.slot = self.free_slots.pop()
                else:
                    tile.slot = self.nslots
                    self.nslots += 1
            op.slot = tile.slot
            op.cnt = self.slot_cnt.get(op.slot, 0) + 1
            self.slot_cnt[op.slot] = op.cnt
        cand = []
        for r in reads:
            if r.last_w is not None:
                cand.append((r.last_w, True))
        for w in writes:
            if w.last_w is not None:
                cand.append((w.last_w, True))
            for rd in w.readers:
                cand.append((rd, False))
        cand.sort(key=lambda t: -self._val(t[0]))
        for d, raw in cand:
            self._add_dep(op, d, raw)
        if force_dep is not None:
            self._add_dep(op, force_dep, True, force=True)
        for r in reads:
            r.readers.append(op)
        for w in writes:
            w.last_w = op
            w.readers = []
        ck = dict(self.clock[eng])
        if not is_dma:
            ck[eng] = op.seq
        else:
            ck[("s", op.slot)] = op.cnt
        op.clock = ck
        lst.append(op)
        return op

    def dma(self, q, out, in_, reads=(), writes=()):
        o, i = _ap(out), _ap(in_)
        return self.add(q, lambda e: e.dma_start(out=o, in_=i), list(reads) + _rs(in_), list(writes) + _rs(out), is_dma=True)

    def _pe_rowgroup_dep(self, out, stat):
        bp = stat.ap.base_partition()
        bp = bp() if callable(bp) else bp
        k = stat.ap.shape[0]
        groups = set(range(bp // 32, (bp + k + 31) // 32))
        prev = out.res.pe_last
        force = prev[0] if (prev is not None and prev[1].isdisjoint(groups)) else None
        return groups, force

    def mm(self, out, lhsT, rhs, start=True, stop=True):
        o, l, r = out.ap, lhsT.ap, rhs.ap
        groups, force = self._pe_rowgroup_dep(out, lhsT)
        op = self.add("pe", lambda e: e.matmul(o, lhsT=l, rhs=r, start=start, stop=stop), _rs(lhsT, rhs), _rs(out), force_dep=force)
        out.res.pe_last = (op, groups)
        return op

    def tr(self, out, in_, ident):
        o, i, d = out.ap, in_.ap, ident.ap
        groups, force = self._pe_rowgroup_dep(out, in_)
        op = self.add("pe", lambda e: e.transpose(out=o, in_=i, identity=d), _rs(in_, ident), _rs(out), force_dep=force)
        out.res.pe_last = (op, groups)
        return op

    def act(self, out, in_, func, bias=0.0, scale=1.0, accum=None):
        o, i, b, sc = out.ap, in_.ap, _ap(bias), _ap(scale)
        if accum is None:
            return self.add("act", lambda e: e.activation(out=o, in_=i, func=func, bias=b, scale=sc), _rs(in_, bias, scale), _rs(out))
        a = accum.ap
        return self.add("act", lambda e: e.activation(out=o, in_=i, func=func, bias=b, scale=sc, accum_out=a), _rs(in_, bias, scale), _rs(out, accum))

    def tt(self, eng, out, in0, in1, op):
        o, a, b = out.ap, in0.ap, in1.ap
        return self.add(eng, lambda e: e.tensor_tensor(out=o, in0=a, in1=b, op=op), _rs(in0, in1), _rs(out))

    def ts(self, eng, out, in0, s1, op0, s2=None, op1=None):
        o, a, x1, x2 = out.ap, in0.ap, _ap(s1), _ap(s2)
        if op1 is None:
            return self.add(eng, lambda e: e.tensor_scalar(out=o, in0=a, scalar1=x1, scalar2=None, op0=op0), _rs(in0, s1), _rs(out))
        return self.add(eng, lambda e: e.tensor_scalar(out=o, in0=a, scalar1=x1, scalar2=x2, op0=op0, op1=op1), _rs(in0, s1, s2), _rs(out))

    def stt(self, eng, out, in0, scalar, in1, op0, op1):
        o, a, sc, b = out.ap, in0.ap, _ap(scalar), in1.ap
        return self.add(eng, lambda e: e.scalar_tensor_tensor(out=o, in0=a, scalar=sc, in1=b, op0=op0, op1=op1), _rs(in0, scalar, in1), _rs(out))

    def copy(self, eng, out, in_):
        o, i = out.ap, in_.ap
        if eng == "act":
            return self.add("act", lambda e: e.copy(out=o, in_=i), _rs(in_), _rs(out))
        return self.add(eng, lambda e: e.tensor_copy(out=o, in_=i), _rs(in_), _rs(out))

    def memset(self, eng, out, val):
        o = out.ap
        return self.add(eng, lambda e: e.memset(o, val), [], _rs(out))

    def reduce(self, eng, out, in_, op=None):
        o, i = out.ap, in_.ap
        op = op or ALU.add
        return self.add(eng, lambda e: e.tensor_reduce(out=o, in_=i, axis=AX.X, op=op), _rs(in_), _rs(out))

    def max8(self, out, in_):
        o, i = out.ap, in_.ap
        return self.add("dve", lambda e: e.max(out=o, in_=i), _rs(in_), _rs(out))

    def recip(self, out, in_):
        o, i = out.ap, in_.ap
        return self.add("dve", lambda e: e.reciprocal(out=o, in_=i), _rs(in_), _rs(out))

    def barrier(self):
        lasts = []
        for e in ENGINES:
            for op in reversed(self.ops[e]):
                if not op.is_dma and op.fn is not None:
                    lasts.append(op)
                    break
        dmas = [op for e in ENGINES for op in self.ops[e] if op.is_dma and self.slot_cnt[op.slot] == op.cnt]
        for e in ENGINES:
            op = Op(e, None)
            op.seq = len(self.ops[e])
            for d in lasts:
                if d.eng != e:
                    self._add_dep(op, d, True)
            for d in dmas:
                self._add_dep(op, d, True)
            ck = dict(self.clock[e])
            op.clock = ck
            op.seq = -1
            self.ops[e].append(op)

    @contextmanager
    def scope(self):
        old = self.stack
        mark = len(self.all_res)
        with ExitStack() as st:
            self.stack = st
            yield
            self.barrier()
            for r in self.all_res[mark:]:
                if r.slot is not None:
                    self.free_slots.append(r.slot)
                    r.slot = None
            del self.all_res[mark:]
        self.stack = old

    def emit(self, final_wait_ops=()):
        nc = self.nc
        from contextlib import ExitStack
        with ExitStack() as st:
            esem = {e: st.enter_context(nc.semaphore("sem_" + e)) for e in ENGINES}
            ssem = {s: st.enter_context(nc.semaphore("dsem%d" % s)) for s in range(self.nslots)}
            for e in ENGINES:
                v = 0
                for op in self.ops[e]:
                    if op.fn is None or op.is_dma:
                        continue
                    if op.signal:
                        v += 1
                    op.semval = v
            block = st.enter_context(nc.Block())

            def run(e, eng):
                for op in self.ops[e]:
                    for d in op.waits:
                        if d.is_dma:
                            eng.wait_ge(ssem[d.slot], 16 * d.cnt)
                        else:
                            eng.wait_ge(esem[d.eng], d.semval)
                        self.n_wait += 1
                    if op.fn is None:
                        continue
                    ins = op.fn(eng)
                    if op.is_dma:
                        ins.then_inc(ssem[op.slot], 16)
                    elif op.signal:
                        ins.then_inc(esem[e], 1)

            @block.tensor
            def _(eng):
                run("pe", eng)

            @block.scalar
            def _(eng):
                run("act", eng)

            @block.vector
            def _(eng):
                run("dve", eng)

            @block.gpsimd
            def _(eng):
                run("pool", eng)

            @block.sync
            def _(eng):
                run("sp", eng)

D = 1024
T = 2048
NH = 4
HD = 64
BW = 256
DFF = 4096
OFF_GDN, OFF_MOBA, OFF_SB, OFF_SSD, INW = 0, 1032, 1800, 2568, 3596
EPS = 1e-6
NEG = -30000.0


def _const_table():
    p = np.arange(128)[:, None]
    f = np.arange(128)[None, :]
    c = {}
    c["ident"] = (p == f)
    c["ones"] = np.ones((128, 128))
    c["blk64"] = (p // 64 == f // 64)
    c["tri128"] = (p <= f)
    c["neg_ssd"] = np.where(f >= p, 0.0, NEG)
    same = (p // 64 == f // 64)
    c["tri_bd"] = same & (p <= f)
    c["neg_incl_bd"] = np.where(same & (f >= p), 0.0, NEG)
    c["neg_strict_bd"] = np.where(same & (f > p), 0.0, NEG)
    c["m_strict"] = (f > p)
    c["m_incl"] = (f >= p)
    c["u_incl"] = (p >= f)
    i = np.arange(16)[:, None, None]
    n = np.arange(8)[None, None, :]
    h = np.arange(4)[None, :, None]
    past = np.broadcast_to(n < i // 2, (16, 4, 8))
    own = np.broadcast_to(n == i // 2, (16, 4, 8))
    c["pastbias"] = np.broadcast_to(np.where(past, 0.0, -1e30).reshape(1, 512), (128, 512))
    c["pastm"] = np.broadcast_to(past.reshape(1, 512).astype(np.float64), (128, 512))
    c["ownm"] = np.broadcast_to(own.reshape(1, 512).astype(np.float64), (128, 512))
    slopes = np.array([2.0 ** (-8.0 * (k + 1) / 4) for k in range(4)])
    dl = np.arange(16)[None, None, :] - 12
    c["alibi"] = (slopes[None, :, None] * (dl * 128 + np.arange(128)[:, None, None])).reshape(128, 64)
    v = np.zeros((128, 32))
    for r in range(32):
        v[r, r] = 1.0
    for hh in range(4):
        v[32, hh * 8:(hh + 1) * 8] = slopes[hh]
        v[33, hh * 8:(hh + 1) * 8] = slopes[hh]
    c["selv"] = v
    t = np.arange(512)
    a = np.zeros((128, 512))
    a[32] = -128.0 * (t // 128)
    a[33] = -(t % 128).astype(np.float64)
    c["augrows"] = a
    offs = {}
    cols = []
    o = 0
    for k, val in c.items():
        val = np.asarray(val, dtype=np.float32)
        offs[k] = (o, val.shape[1])
        cols.append(val)
        o += val.shape[1]
    return np.ascontiguousarray(np.concatenate(cols, axis=1)), offs


CONST_ARR, CONST_OFF = _const_table()
NCONST = CONST_ARR.shape[1]

LP_OFF = {}
_o = 0
for _k, _w in (("g_mix_post", 1024), ("g_ffn_post", 1024), ("gdn_conv", 24), ("ssd_conv", 24), ("ssd_cb", 6),
               ("gdn_alog", 4), ("gdn_dtb", 4), ("ssd_alog", 4), ("ssd_dtb", 4), ("ssd_d", 4),
               ("gdn_norm", 64), ("ssd_norm", 256), ("rowg", 16)):
    LP_OFF[_k] = (_o, _w)
    _o += _w
NLP = _o


def _layer_params(inp, l):
    lp = np.zeros((128, NLP), np.float32)

    def put(k, arr):
        o, w = LP_OFF[k]
        lp[:, o:o + w] = arr
    put("g_mix_post", np.broadcast_to(inp["norm_mix_post"][l][None, :], (128, 1024)))
    put("g_ffn_post", np.broadcast_to(inp["norm_ffn_post"][l][None, :], (128, 1024)))
    put("gdn_conv", inp["gdn_conv"][l].reshape(4, 6, 128).transpose(2, 1, 0).reshape(128, 24))
    put("ssd_conv", inp["ssd_conv"][l].reshape(4, 6, 128).transpose(2, 1, 0).reshape(128, 24))
    put("ssd_cb", inp["ssd_conv_bias"][l].reshape(6, 128).T)
    for k, src in (("gdn_alog", "gdn_a_log"), ("gdn_dtb", "gdn_dt_bias"), ("ssd_alog", "ssd_a_log"),
                   ("ssd_dtb", "ssd_dt_bias"), ("ssd_d", "ssd_d")):
        put(k, np.broadcast_to(inp[src][l][None, :], (128, 4)))
    put("gdn_norm", np.broadcast_to(inp["gdn_norm"][l][None, :], (128, 64)))
    put("ssd_norm", np.broadcast_to(inp["ssd_norm"][l][None, :], (128, 256)))
    rg = np.concatenate([inp["norm_mix_pre"][l].reshape(8, 128).T, inp["norm_ffn_pre"][l].reshape(8, 128).T], axis=1)
    put("rowg", rg)
    return lp


class K:
    def __init__(self, n_seq=4, n_layers=2, units=("gdn", "moba", "sb", "ssd", "merge"), dbg=None):
        self.n_seq, self.n_layers, self.units, self.dbg = n_seq, n_layers, units, (dbg or {})
        nc = bass.Bass("TRN2", target_bir_lowering=False)
        self.nc = nc
        self.P = Prog(nc)
        NL = n_layers
        di = lambda name, shape, dt=F32: nc.dram_tensor(name, list(shape), dt, kind="ExternalInput").ap()
        dn = lambda name, shape, dt=BF16: nc.dram_tensor(name, list(shape), dt).ap()
        self.x = di("x", [n_seq * T, D])
        self.w_in = di("w_in", [NL, D, INW])
        self.w_gate = di("w_gate", [NL, 4, D, D])
        self.w_branch = di("w_branch", [NL, 4, BW, D])
        self.w_out = di("w_out", [NL, D, D])
        self.w_up = di("w_up", [NL, D, DFF])
        self.w_down = di("w_down", [NL, DFF, D])
        self.consts = di("consts", [128, NCONST])
        self.lpd = di("lp", [NL, 128, NLP])
        self.out = nc.dram_tensor("out", [n_seq * T, D], F32, kind="ExternalOutput").ap()
        self.xmid = dn("xmid", [n_seq * T, D], F32)
        self.wb_in = dn("wb_in", [NL, D, INW])
        self.wb_gate = dn("wb_gate", [NL, 8, 128, 4, 8, 128])
        self.wb_br = dn("wb_br", [NL, 8, 128, 4, 2, 128])
        self.wb_out = dn("wb_out", [NL, D, D])
        self.wb_up = dn("wb_up", [NL, 32, 128, 8, 128])
        self.wb_down = dn("wb_down", [NL, DFF, D])
        if "inject_yT" in self.dbg:
            self.dbg_yT = di("dbg_yT", [4, BW, T])
        if "dump_yT" in self.dbg:
            self.dbg_yT_out = nc.dram_tensor("dbg_yT_out", [4, BW, T], F32, kind="ExternalOutput").ap()
        if "dump_hT" in self.dbg:
            self.dbg_hT_out = nc.dram_tensor("dbg_hT_out", [D, T], F32, kind="ExternalOutput").ap()
        self._q = 0

    def q(self):
        self._q += 1
        return ("sp", "act")[self._q % 2]

    def c(self, name):
        o, w = CONST_OFF[name]
        return self.cst[:, o:o + w]

    def lpv(self, name):
        o, w = LP_OFF[name]
        return self.lp[:, o:o + w]

    def build(self):
        P = self.P
        with ExitStack() as st:
            P.stack = st
            self.pb = [P.ps("pb%d" % i, [128, 512], F32) for i in range(8)]
            self.cst = P.sb("cst", [128, NCONST], F32)
            self.lp = P.sb("lp_sb", [128, NLP], F32)
            self.ones_bf = P.sb("ones_bf", [128, 128], BF16)
            self.uincl_bf = P.sb("uincl_bf", [128, 128], BF16)
            self.sel_bf = P.sb("sel_bf", [128, 32, 128], BF16)
            self.hT = P.sb("hT", [128, 8, T], BF16)
            self.yT = [P.sb("yT%d" % g, [128, 2, T], BF16) for g in range(4)]
            P.dma("sp", self.cst[:, :], self.consts[:, :])
            P.copy("dve", self.ones_bf[:, :], self.c("ones"))
            P.copy("dve", self.uincl_bf[:, :], self.c("u_incl"))
            P.copy("pool", self.sel_bf[0:34, :, :], self.c("selv")[0:34, :].un(2).bc([34, 32, 128]))
            if "dump_yT" in self.dbg:
                for g in range(4):
                    P.memset("pool", self.yT[g][:, :, :], 0.0)
            for l in range(self.n_layers):
                P.dma("sp", self.lp[:, :], self.lpd[l, :, :])
                if "noconvert" not in self.dbg:
                    self.convert_weights(l)
                xsrc = self.x if l == 0 else self.xmid
                xdst = self.out if l == self.n_layers - 1 else self.xmid
                for s in range(self.n_seq):
                    self.u_norm(l, s, xsrc)
                    if "dump_hT" in self.dbg:
                        self.dump_hT()
                    if "inject_yT" in self.dbg:
                        self.inject_yT()
                    if "sb" in self.units:
                        self.u_sb(l, s)
                    if "moba" in self.units:
                        self.u_moba(l, s)
                    if "ssd" in self.units:
                        self.u_ssd(l, s)
                    if "gdn" in self.units:
                        self.u_gdn(l, s)
                    if "dump_yT" in self.dbg:
                        self.dump_yT()
                    if "merge" in self.units:
                        self.u_merge(l, s, xsrc, xdst)
            P.barrier()
            P.emit()
        return self.nc

    def dump_hT(self):
        P = self.P
        with P.scope():
            t = P.sb("dh", [128, 8, T], F32)
            P.copy("dve", t[:, :, :], self.hT[:, :, :])
            P.dma("sp", self.dbg_hT_out.rearrange("(k p) t -> p k t", p=128), t[:, :, :])

    def dump_yT(self):
        P = self.P
        with P.scope():
            for g in range(4):
                t = P.sb("dy%d" % g, [128, 2, T], F32)
                P.copy("dve", t[:, :, :], self.yT[g][:, :, :])
                P.dma("sp", self.dbg_yT_out[g].rearrange("(k p) t -> p k t", p=128), t[:, :, :])

    def inject_yT(self):
        P = self.P
        with P.scope():
            for g in range(4):
                t = P.sb("iy%d" % g, [128, 2, T], F32)
                P.dma("sp", t[:, :, :], self.dbg_yT[g].rearrange("(k p) t -> p k t", p=128))
                P.copy("dve", self.yT[g][:, :, :], t[:, :, :])

    def convert_weights(self, l):
        P = self.P
        with P.scope():
            st32 = [P.sb("cv32_%d" % i, [128, 4096], F32) for i in range(2)]
            st16 = [P.sb("cv16_%d" % i, [128, 4096], BF16) for i in range(2)]
            cnt = [0]
            rowg = self.lpv("rowg")

            def cv(src, w, scales, stores):
                b = cnt[0] % 2
                cnt[0] += 1
                s32, s16 = st32[b], st16[b]
                P.dma(self.q(), s32[:, 0:w] if len(src.shape) == 2 else s32[:, 0:w].r("p (k c) -> p k c", k=src.shape[1]), src)
                eng = ("dve", "pool")[cnt[0] % 2]
                if scales is None:
                    P.copy(("act", "dve", "pool")[cnt[0] % 3], s16[:, 0:w], s32[:, 0:w])
                else:
                    for (a, bnd, col) in scales:
                        P.ts(eng, s16[:, a:bnd], s32[:, a:bnd], rowg[:, col:col + 1], ALU.mult)
                for (dst, a, bnd, pat, kw) in stores:
                    v = s16[:, a:bnd]
                    if pat:
                        v = v.r(pat, **kw)
                    P.dma(self.q(), dst, v)

            for kt in range(8):
                cv(self.w_in[l, kt * 128:(kt + 1) * 128, :], INW, [(0, INW, kt)],
                   [(self.wb_in[l, kt * 128:(kt + 1) * 128, :], 0, INW, None, None)])
            for g in range(4):
                for half in range(2):
                    src = self.w_gate[l, g, half * 512:(half + 1) * 512, :].rearrange("(k p) c -> p k c", p=128)
                    cv(src, 4096, [(k * 1024, (k + 1) * 1024, half * 4 + k) for k in range(4)],
                       [(self.wb_gate[l, :, :, g, half * 4 + k, :].rearrange("c p j -> p c j"), k * 1024, (k + 1) * 1024,
                         "p (c j) -> p c j", dict(j=128)) for k in range(4)])
            for g in range(4):
                src = self.w_branch[l, g].rearrange("(k p) c -> p k c", p=128)
                cv(src, 2048, None,
                   [(self.wb_br[l, :, :, g, k, :].rearrange("c p j -> p c j"), k * 1024, (k + 1) * 1024,
                     "p (c j) -> p c j", dict(j=128)) for k in range(2)])
            for half in range(2):
                src = self.w_out[l, half * 512:(half + 1) * 512, :].rearrange("(k p) c -> p k c", p=128)
                cv(src, 4096, None,
                   [(self.wb_out[l, half * 512:(half + 1) * 512, :].rearrange("(k p) c -> p k c", p=128), 0, 4096,
                     "p (k c) -> p k c", dict(k=4))])
            for kt in range(8):
                cv(self.w_up[l, kt * 128:(kt + 1) * 128, :], 4096, [(0, 4096, 8 + kt)],
                   [(self.wb_up[l, f0:f0 + 8, :, kt, :].rearrange("f p j -> p f j"), f0 * 128, (f0 + 8) * 128,
                     "p (f j) -> p f j", dict(j=128)) for f0 in range(0, 32, 8)])
            for qd in range(8):
                src = self.w_down[l, qd * 512:(qd + 1) * 512, :].rearrange("(k p) c -> p k c", p=128)
                cv(src, 4096, None,
                   [(self.wb_down[l, qd * 512:(qd + 1) * 512, :].rearrange("(k p) c -> p k c", p=128), 0, 4096,
                     "p (k c) -> p k c", dict(k=4))])

    def u_norm(self, l, s, xsrc):
        P = self.P
        ident = self.c("ident")
        with P.scope():
            xt = [P.sb("nx%d" % i, [128, D], F32) for i in range(2)]
            hh = [P.sb("nh%d" % i, [128, D], F32) for i in range(2)]
            junk = P.sb("njunk", [128, D], F32)
            ss = [P.sb("nss%d" % i, [128, 1], F32) for i in range(2)]
            rs = [P.sb("nrs%d" % i, [128, 1], F32) for i in range(2)]
            for i in range(16):
                b = i % 2
                r0 = s * T + i * 128
                P.dma(self.q(), xt[b][:, :], xsrc[r0:r0 + 128, :])
                P.act(junk[:, :], xt[b][:, :], AF.Square, accum=ss[b][:, :])
                P.act(rs[b][:, :], ss[b][:, :], AF.Ln, bias=EPS, scale=1.0 / D)
                P.act(rs[b][:, :], rs[b][:, :], AF.Exp, scale=-0.5)
                P.ts("dve", hh[b][:, :], xt[b][:, :], rs[b][:, 0:1], ALU.mult)
                for g in range(2):
                    bank = self.pb[(2 * i + g) % 4]
                    for k in range(4):
                        kk = g * 4 + k
                        P.tr(bank[:, k * 128:(k + 1) * 128], hh[b][:, kk * 128:(kk + 1) * 128], ident)
                    P.copy("act" if g == 0 else "dve", self.hT[:, g * 4:(g + 1) * 4, i * 128:(i + 1) * 128],
                           bank[:, :].r("p (k n) -> p k n", k=4))

    def u_merge(self, l, s, xsrc, xdst):
        P = self.P
        ident = self.c("ident")
        g_post, g_fpost = self.lpv("g_mix_post"), self.lpv("g_ffn_post")
        pb = self.pb
        with P.scope():
            ws = [P.sb("mw%d" % i, [128, 4096], BF16) for i in range(3)]
            wbr = [P.sb("mwb%d" % i, [128, 4, 2, 128], BF16) for i in range(2)]
            xs = P.sb("mx", [128, 4, D], F32)
            acc = P.sb("macc", [128, 512], F32)
            sg = [P.sb("msg%d" % i, [128, 512], F32) for i in range(2)]
            mT = P.sb("mmT", [128, 8, 512], BF16)
            tmp = [P.sb("mtmp%d" % i, [128, D], F32) for i in range(2)]
            h2T = P.sb("mh2T", [128, 8, 512], BF16)
            aT = P.sb("maT", [128, 32, 512], BF16)
            rl = [P.sb("mrl%d" % i, [128, 512], F32) for i in range(2)]
            ss = P.sb("mss", [128, 8], F32)
            rs = P.sb("mrs", [128, 4], F32)
            wi = [0]

            def wload(src, pat=None, **kw):
                t = ws[wi[0] % 3]
                wi[0] += 1
                P.dma(self.q(), t[:, :].r(pat, **kw) if pat else t[:, :], src)
                return t

            for tb in range(4):
                t0 = tb * 512
                r0 = s * T + t0
                P.dma(self.q(), xs[:, :, :], xsrc[r0:r0 + 512, :].rearrange("(j p) c -> p j c", p=128))
                for cc in range(8):
                    wg = wload(self.wb_gate[l, cc].rearrange("p g k j -> p (g k j)"))
                    wb_ = wbr[cc % 2]
                    P.dma(self.q(), wb_[:, :, :, :], self.wb_br[l, cc])
                    for g in range(4):
                        gp, bp = pb[(2 * g) % 4], pb[(2 * g + 1) % 4]
                        for k in range(8):
                            P.mm(gp[:, :], wg[:, (g * 8 + k) * 128:(g * 8 + k + 1) * 128], self.hT[:, k, t0 + s * 0:t0 + 512],
                                 start=(k == 0), stop=(k == 7))
                        for k in range(2):
                            P.mm(bp[:, :], wb_[:, g, k, :], self.yT[g][:, k, t0:t0 + 512], start=(k == 0), stop=(k == 1))
                        sgt = sg[g % 2]
                        P.act(sgt[:, :], gp[:, :], AF.Sigmoid)
                        if g == 0:
                            P.tt("dve", acc[:, :], sgt[:, :], bp[:, :], ALU.mult)
                        else:
                            P.tt("dve", sgt[:, :], sgt[:, :], bp[:, :], ALU.mult)
                            if g < 3:
                                P.tt("pool", acc[:, :], acc[:, :], sgt[:, :], ALU.add)
                            else:
                                P.tt("pool", mT[:, cc, :], acc[:, :], sgt[:, :], ALU.add)
                for half in range(2):
                    wo = wload(self.wb_out[l, :, half * 512:(half + 1) * 512].rearrange("(k p) c -> p k c", p=128), "p (k c) -> p k c", k=8)
                    for j in range(4):
                        bank = pb[4 + half * 2 + j % 2] if False else pb[half * 4 + j]
                        for k in range(8):
                            P.mm(bank[:, :], mT[:, k, j * 128:(j + 1) * 128], wo[:, k * 512:(k + 1) * 512],
                                 start=(k == 0), stop=(k == 7))
                for j in range(4):
                    for half in range(2):
                        P.act(tmp[0][:, half * 512:(half + 1) * 512], pb[half * 4 + j][:, :], AF.Square,
                              accum=ss[:, half:half + 1])
                    P.tt("dve", ss[:, 2:3], ss[:, 0:1], ss[:, 1:2], ALU.add)
                    P.act(rs[:, 0:1], ss[:, 2:3], AF.Ln, bias=EPS, scale=1.0 / D)
                    P.act(rs[:, 0:1], rs[:, 0:1], AF.Exp, scale=-0.5)
                    for half in range(2):
                        cs = slice(half * 512, (half + 1) * 512)
                        P.stt("dve", tmp[1][:, cs], pb[half * 4 + j][:, :], rs[:, 0:1], g_post[:, cs], ALU.mult, ALU.mult)
                    P.tt("pool", xs[:, j, :], xs[:, j, :], tmp[1][:, :], ALU.add)
                    P.act(tmp[0][:, :], xs[:, j, :], AF.Square, accum=ss[:, 3:4])
                    P.act(rs[:, 1:2], ss[:, 3:4], AF.Ln, bias=EPS, scale=1.0 / D)
                    P.act(rs[:, 1:2], rs[:, 1:2], AF.Exp, scale=-0.5)
                    P.ts("dve", tmp[1][:, :], xs[:, j, :], rs[:, 1:2], ALU.mult)
                    for g in range(2):
                        bank = pb[half * 0 + g * 4 + j]
                        for k in range(4):
                            kk = g * 4 + k
                            P.tr(bank[:, k * 128:(k + 1) * 128], tmp[1][:, kk * 128:(kk + 1) * 128], ident)
                        P.copy("act" if g == 0 else "dve", h2T[:, g * 4:(g + 1) * 4, j * 128:(j + 1) * 128],
                               bank[:, :].r("p (k n) -> p k n", k=4))
                for f0 in range(0, 32, 4):
                    wu = wload(self.wb_up[l, f0:f0 + 4].rearrange("f p k j -> p f (k j)"), "p (f c) -> p f c", f=4)
                    for fi in range(4):
                        f = f0 + fi
                        bank = pb[f % 4]
                        for k in range(8):
                            P.mm(bank[:, :], wu[:, (fi * 8 + k) * 128:(fi * 8 + k + 1) * 128], h2T[:, k, :],
                                 start=(k == 0), stop=(k == 7))
                        r = rl[f % 2]
                        P.act(r[:, :], bank[:, :], AF.Relu)
                        P.tt("pool", aT[:, f, :], r[:, :], r[:, :], ALU.mult)
                for k0 in range(0, 32, 4):
                    wd = wload(self.wb_down[l, k0 * 128:(k0 + 4) * 128, :].rearrange("(k p) c -> p k c", p=128), "p (k c) -> p k c", k=4)
                    for ki in range(4):
                        k = k0 + ki
                        for j in range(4):
                            for half in range(2):
                                P.mm(pb[half * 4 + j][:, :], aT[:, k, j * 128:(j + 1) * 128],
                                     wd[:, ki * 1024 + half * 512:ki * 1024 + (half + 1) * 512],
                                     start=(k == 0), stop=(k == 31))
                for j in range(4):
                    for half in range(2):
                        P.act(tmp[0][:, half * 512:(half + 1) * 512], pb[half * 4 + j][:, :], AF.Square,
                              accum=ss[:, 4 + half:5 + half])
                    P.tt("dve", ss[:, 6:7], ss[:, 4:5], ss[:, 5:6], ALU.add)
                    P.act(rs[:, 2:3], ss[:, 6:7], AF.Ln, bias=EPS, scale=1.0 / D)
                    P.act(rs[:, 2:3], rs[:, 2:3], AF.Exp, scale=-0.5)
                    for half in range(2):
                        cs = slice(half * 512, (half + 1) * 512)
                        P.stt("dve", tmp[1][:, cs], pb[half * 4 + j][:, :], rs[:, 2:3], g_fpost[:, cs], ALU.mult, ALU.mult)
                    P.tt("pool", xs[:, j, :], xs[:, j, :], tmp[1][:, :], ALU.add)
                P.dma(self.q(), xdst[r0:r0 + 512, :].rearrange("(j p) c -> p j c", p=128), xs[:, :, :])

    def _proj_fm(self, wt, col0, out_writer, nchunks=4):
        P = self.P
        for c in range(nchunks):
            bank = self.pb[self._pj % 4]
            self._pj += 1
            for k in range(8):
                P.mm(bank[:, :], wt[:, k, col0:col0 + 128], self.hT[:, k, c * 512:(c + 1) * 512], start=(k == 0), stop=(k == 7))
            out_writer(c, bank[:, :])

    def _proj_tm(self, wt, col0, ncol, i, bank):
        P = self.P
        for k in range(8):
            P.mm(bank[:, 0:ncol], self.hT[:, k, i * 128:(i + 1) * 128], wt[:, k, col0:col0 + ncol], start=(k == 0), stop=(k == 7))

    def u_sb(self, l, s):
        P = self.P
        pb = self.pb
        self._pj = 0
        with P.scope():
            wt = P.sb("sw", [128, 8, 768], BF16)
            qT = P.sb("sq", [128, 2, T], BF16)
            kT = P.sb("sk", [128, 2, T], BF16)
            vv = P.sb("sv", [128, 16, 256], BF16)
            E = [P.sb("sE%d" % i, [128, 512], F32) for i in range(2)]
            Lp = [P.sb("sL%d" % i, [128, 512], BF16) for i in range(2)]
            G = [P.sb("sG%d" % i, [128, 512], F32) for i in range(2)]
            Wt = [P.sb("sW%d" % i, [128, 512], BF16) for i in range(2)]
            Pacc = P.sb("sPacc", [128, 512], BF16)
            P.dma("sp", wt[:, :, :], self.wb_in[l, :, OFF_SB:OFF_SB + 768].rearrange("(k p) c -> p k c", p=128))
            for hp in range(2):
                self._proj_fm(wt, hp * 128, lambda c, ps, hp=hp: P.ts("dve", qT[:, hp, c * 512:(c + 1) * 512], ps, 0.125, ALU.mult))
                self._proj_fm(wt, 256 + hp * 128, lambda c, ps, hp=hp: P.copy("act", kT[:, hp, c * 512:(c + 1) * 512], ps))
            for i in range(16):
                bank = pb[4 + i % 2]
                self._proj_tm(wt, 512, 256, i, bank)
                P.copy("act" if i % 2 else "dve", vv[:, i, :], bank[:, 0:256])
            it = 0
            for h in range(4):
                hp, r = h // 2, slice((h % 2) * 64, (h % 2) * 64 + 64)
                for qb in range(4):
                    q0 = qb * 512
                    P.memset("pool", Pacc[:, :], 0.0)
                    accb = pb[6 + (h * 4 + qb) % 2]
                    nk = 4 * qb + 4
                    for kt in range(nk - 1, -1, -1):
                        c0 = max(0, kt - 4 * qb) * 128
                        b = it % 2
                        it += 1
                        zb, sb_ = pb[b], pb[2 + b]
                        P.mm(zb[:, c0:512], kT[r, hp, kt * 128:(kt + 1) * 128], qT[r, hp, q0 + c0:q0 + 512])
                        P.act(E[b][:, c0:512], zb[:, c0:512], AF.Exp)
                        if kt >= 4 * qb:
                            P.tt("pool", E[b][:, c0:c0 + 128], E[b][:, c0:c0 + 128], self.c("m_strict"), ALU.mult)
                        P.act(Lp[b][:, c0:512], E[b][:, c0:512], AF.Ln, bias=1.0)
                        first = (kt == nk - 1)
                        P.mm(sb_[:, c0:512], self.uincl_bf[:, :], Lp[b][:, c0:512], start=True, stop=first)
                        if not first:
                            P.mm(sb_[:, c0:512], self.ones_bf[:, :], Pacc[:, c0:512], start=False, stop=True)
                        P.act(G[b][:, c0:512], sb_[:, c0:512], AF.Exp, scale=-1.0)
                        if c0 > 0:
                            P.memset("pool", Wt[b][:, 0:c0], 0.0)
                        P.tt("dve", Wt[b][:, c0:512], E[b][:, c0:512], G[b][:, c0:512], ALU.mult)
                        P.mm(accb[:, :], vv[:, kt, hp * 128:(hp + 1) * 128], Wt[b][:, :], start=first, stop=(kt == 0))
                        if kt > 0:
                            P.tt("pool", Pacc[:, c0:512], Pacc[:, c0:512], Lp[b][:, c0:512], ALU.add)
                    P.copy("act", self.yT[2][r, hp, q0:q0 + 512], accb[r, :])

    def u_moba(self, l, s):
        P = self.P
        pb = self.pb
        self._pj = 0
        ident = self.c("ident")
        with P.scope():
            wt = P.sb("bw", [128, 8, 768], BF16)
            q32 = P.sb("bq32", [128, 2, T], F32)
            qT = P.sb("bq", [128, 2, T], BF16)
            kT = P.sb("bk", [128, 2, T], BF16)
            vv = P.sb("bv", [128, 16, 256], BF16)
            ksum = P.sb("bks", [128, 2, 8], F32)
            gm = P.sb("bgm", [128, 64, 8], F32)
            thr = P.sb("bthr", [128, 64, 8], F32)
            sel = P.sb("bsel", [128, 512], F32)
            aug = P.sb("baug", [128, T], BF16)
            Pt = [P.sb("bP%d" % i, [128, 512], BF16) for i in range(2)]
            rden = P.sb("brd", [128, 512], F32)
            P.dma("sp", wt[:, :, :], self.wb_in[l, :, OFF_MOBA:OFF_MOBA + 768].rearrange("(k p) c -> p k c", p=128))
            P.copy("pool", aug[32:34, :].r("p (a t) -> p a t", a=4), self.c("augrows")[32:34, :].un(1).bc([2, 4, 512]))
            for hp in range(2):
                def wq(c, ps, hp=hp):
                    P.ts("dve", q32[:, hp, c * 512:(c + 1) * 512], ps, 0.125, ALU.mult)
                    P.copy("pool", qT[:, hp, c * 512:(c + 1) * 512], q32[:, hp, c * 512:(c + 1) * 512])
                self._proj_fm(wt, hp * 128, wq)

                def wk(c, ps, hp=hp):
                    P.copy("act", kT[:, hp, c * 512:(c + 1) * 512], ps)
                    P.reduce("dve", ksum[:, hp, 2 * c:2 * c + 2], ps.r("p (n t) -> p n t", n=2))
                self._proj_fm(wt, 256 + hp * 128, wk)
            for i in range(16):
                bank = pb[4 + i % 2]
                self._proj_tm(wt, 512, 256, i, bank)
                P.copy("act" if i % 2 else "dve", vv[:, i, :], bank[:, 0:256])
            if "moba_stop1" in self.dbg:
                return
            for hh in range(2):
                gb = pb[6 + hh]
                r = slice(hh * 64, hh * 64 + 64)
                for i in range(16):
                    for hp in range(2):
                        o = (i * 2 + hp) * 8
                        P.mm(gb[:, o:o + 8], q32[r, hp, i * 128:(i + 1) * 128], ksum[r, hp, :])
                P.tt("dve", gm[:, :, :].r("p (i hp hh) n -> p i hp hh n", hp=2, hh=2)[:, :, :, hh, :],
                     gb[:, 0:256].r("p (i hp n) -> p i hp n", hp=2, n=8),
                     self.c("pastbias").r("p (i hp hh n) -> p i hp hh n", hp=2, hh=2, n=8)[:, :, :, hh, :], ALU.add)
            for a in range(64):
                P.max8(thr[:, a, :], gm[:, a, :])
            P.tt("dve", sel[:, :].r("p (a n) -> p a n", n=8), gm[:, :, :], thr[:, :, 2:3].bc([128, 64, 8]), ALU.is_ge)
            P.tt("dve", sel[:, :], sel[:, :], self.c("pastm"), ALU.mult)
            P.tt("dve", sel[:, :], sel[:, :], self.c("ownm"), ALU.add)
            P.ts("dve", sel[:, :], sel[:, :], -1.0, ALU.add, 1000.0, ALU.mult)
            if "moba_stop2" in self.dbg:
                return
            for g4 in range(4):
                tb_ = pb[g4 % 2]
                for j in range(4):
                    i = g4 * 4 + j
                    P.tr(tb_[0:32, j * 128:(j + 1) * 128], sel[:, i * 32:(i + 1) * 32], ident)
                P.copy("act", aug[0:32, g4 * 512:(g4 + 1) * 512], tb_[0:32, :])
            it = 0
            alibi = self.c("alibi")
            for h in range(0 if "moba_noattn" not in self.dbg else 4, 4):
                hp, r = h // 2, slice((h % 2) * 64, (h % 2) * 64 + 64)
                for qb in range(4):
                    q0 = qb * 512
                    numb, denb = pb[4 + 2 * ((h * 4 + qb) % 2)], pb[5 + 2 * ((h * 4 + qb) % 2)]
                    nk = 4 * qb + 4
                    for kt in range(nk):
                        c0 = max(0, kt - 4 * qb) * 128
                        b = it % 2
                        it += 1
                        sb_ = pb[b]
                        n = kt // 2
                        P.mm(sb_[:, c0:512], kT[r, hp, kt * 128:(kt + 1) * 128], qT[r, hp, q0 + c0:q0 + 512], start=True, stop=False)
                        P.mm(sb_[:, c0:512], self.sel_bf[0:34, h * 8 + n, :], aug[0:34, q0 + c0:q0 + 512], start=False, stop=True)
                        dl = kt - 4 * qb + 12
                        P.act(Pt[b][:, c0:512], sb_[:, c0:512], AF.Exp, bias=alibi[:, h * 16 + dl:h * 16 + dl + 1])
                        if kt >= 4 * qb:
                            P.tt("pool", Pt[b][:, c0:c0 + 128], Pt[b][:, c0:c0 + 128], self.c("m_incl"), ALU.mult)
                            if c0 > 0:
                                P.memset("pool", Pt[b][:, 0:c0], 0.0)
                        P.mm(numb[:, :], vv[:, kt, hp * 128:(hp + 1) * 128], Pt[b][:, :], start=(kt == 0), stop=(kt == nk - 1))
                        P.mm(denb[:, :], self.ones_bf[:, :], Pt[b][:, :], start=(kt == 0), stop=(kt == nk - 1))
                    P.recip(rden[r, :], denb[r, :])
                    P.tt("dve", self.yT[1][r, hp, q0:q0 + 512], numb[r, :], rden[r, :], ALU.mult)

    def _conv_chunk(self, wt, col0, m, c, pre, acc, cw, out_view, bias, eng):
        P = self.P
        bank = self.pb[self._pj % 4]
        self._pj += 1
        for k in range(8):
            P.mm(bank[:, :], wt[:, k, col0:col0 + 128], self.hT[:, k, c * 512:(c + 1) * 512], start=(k == 0), stop=(k == 7))
        if c == 0:
            P.memset(eng, pre[:, m, 0:3], 0.0)
        else:
            P.copy(eng, pre[:, m, 0:3], pre[:, m, 512:515])
        P.copy("act", pre[:, m, 3:515], bank[:, :])
        P.ts(eng, acc[:, :], pre[:, m, 3:515], cw[:, m * 4 + 3:m * 4 + 4], ALU.mult)
        for j in range(1, 4):
            P.stt("dve", acc[:, :], pre[:, m, 3 - j:515 - j], cw[:, m * 4 + 3 - j:m * 4 + 4 - j], acc[:, :], ALU.mult, ALU.add)
        if bias is None:
            P.act(out_view, acc[:, :], AF.Silu)
        else:
            P.act(out_view, acc[:, :], AF.Silu, bias=bias)

    def u_ssd(self, l, s):
        P = self.P
        pb = self.pb
        self._pj = 0
        ident = self.c("ident")
        ones = self.c("ones")
        with P.scope():
            wt = P.sb("dw", [128, 8, 1028], BF16)
            pre = P.sb("dpre", [128, 6, 515], F32)
            accs = [P.sb("dacc%d" % i, [128, 512], F32) for i in range(2)]
            xbc = [P.sb("dxbc%d" % i, [128, 6, 512], F32) for i in range(2)]
            zs = P.sb("dzs", [128, 4, 256], F32)
            dt = P.sb("ddt", [128, 16, 4], F32)
            gg = P.sb("dgg", [128, 16, 4], F32)
            aneg = P.sb("daneg", [128, 4], F32)
            tok = [P.sb("dtok%d" % i, [128, 512], F32) for i in range(2)]
            gtri = P.sb("dgtri", [128, 4, 128], F32)
            Dm = P.sb("dD", [128, 4, 128], F32)
            lm = P.sb("dlm", [128, 4, 128], F32)
            Mm = P.sb("dM", [128, 4, 128], F32)
            sm = P.sb("dsm", [128, 32], F32)
            xw = P.sb("dxw", [128, 256], F32)
            hst = P.sb("dhst", [128, 256], F32)
            yb = [P.sb("dy%d" % i, [128, 256], F32) for i in range(2)]
            junk = P.sb("djunk", [128, 256], F32)
            P.dma("sp", wt[:, :, :], self.wb_in[l, :, OFF_SSD:OFF_SSD + 1028].rearrange("(k p) c -> p k c", p=128))
            cw, cb = self.lpv("ssd_conv"), self.lpv("ssd_cb")
            dtb = pb[7]
            for i in range(16):
                for k in range(8):
                    P.mm(dtb[:, i * 4:(i + 1) * 4], self.hT[:, k, i * 128:(i + 1) * 128], wt[:, k, 1024:1028], start=(k == 0), stop=(k == 7))
            P.tt("dve", dt[:, :, :], dtb[:, 0:64].r("p (i h) -> p i h", h=4), self.lpv("ssd_dtb").un(1).bc([128, 16, 4]), ALU.add)
            P.act(dt[:, :, :], dt[:, :, :], AF.Exp)
            P.act(dt[:, :, :], dt[:, :, :], AF.Ln, bias=1.0)
            P.act(aneg[:, :], self.lpv("ssd_alog"), AF.Exp)
            P.stt("dve", gg[:, :, :], dt[:, :, :], -1.0, aneg[:, :].un(1).bc([128, 16, 4]), ALU.mult, ALU.mult)
            P.memset("pool", hst[:, :], 0.0)
            for c in range(4):
                xb = xbc[c % 2]
                for m in range(6):
                    self._conv_chunk(wt, 256 + m * 128, m, c, pre, accs[m % 2], cw, xb[:, m, :], cb[:, m:m + 1],
                                     "dve" if m % 2 == 0 else "pool")
                for j in range(4):
                    i = c * 4 + j
                    bank = pb[4 + j % 2]
                    self._proj_tm(wt, 0, 256, i, bank)
                    P.act(zs[:, j, :], bank[:, 0:256], AF.Silu)
                for j in range(4):
                    i = c * 4 + j
                    cs = slice(j * 128, (j + 1) * 128)
                    tk = tok[i % 2]
                    tb_ = pb[4]
                    for m in range(4):
                        P.tr(tb_[:, m * 128:(m + 1) * 128], xb[:, m, cs], ident)
                    P.copy("act", tk[:, :], tb_[:, :])
                    sb_ = pb[5]
                    P.mm(sb_[:, 0:4], self.c("tri128"), gg[:, i, :])
                    P.copy("dve", sm[:, 0:4], sb_[:, 0:4])
                    P.tt("pool", gtri[:, :, :], self.c("tri128").un(1).bc([128, 4, 128]), gg[:, i, :].un(2).bc([128, 4, 128]), ALU.mult)
                    ab = pb[6]
                    P.mm(ab[:, :], ones, gtri[:, :, :].r("p h l -> p (h l)"))
                    P.copy("dve", sm[:, 4:8], ab[:, :].r("p (h l) -> p h l", h=4)[:, :, 127])
                    P.tt("dve", Dm[:, :, :], ab[:, :].r("p (h l) -> p h l", h=4), sm[:, 0:4].un(2).bc([128, 4, 128]), ALU.subtract)
                    P.tt("pool", Dm[:, :, :], Dm[:, :, :], self.c("neg_ssd").un(1).bc([128, 4, 128]), ALU.add)
                    P.act(lm[:, :, :], Dm[:, :, :], AF.Exp)
                    P.act(sm[:, 8:12], sm[:, 0:4], AF.Exp)
                    P.act(sm[:, 12:16], sm[:, 4:8], AF.Exp)
                    P.tt("dve", sm[:, 16:20], sm[:, 4:8], sm[:, 0:4], ALU.subtract)
                    P.act(sm[:, 16:20], sm[:, 16:20], AF.Exp)
                    P.tt("dve", sm[:, 16:20], sm[:, 16:20], dt[:, i, :], ALU.mult)
                    scb = pb[0]
                    for g in range(2):
                        P.mm(scb[:, g * 128:(g + 1) * 128], xb[:, 2 + g, cs], xb[:, 4 + g, cs])
                    for h in range(4):
                        P.stt("dve", Mm[:, h, :], lm[:, h, :], dt[:, i, h:h + 1], scb[:, (h // 2) * 128:(h // 2 + 1) * 128], ALU.mult, ALU.mult)
                    yb_ = pb[1]
                    for h in range(4):
                        P.mm(yb_[:, h * 64:(h + 1) * 64], Mm[:, h, :], tk[:, h * 64:(h + 1) * 64])
                    for h in range(4):
                        P.mm(yb_[:, 256 + h * 64:256 + (h + 1) * 64], xb[:, 4 + h // 2, cs], hst[:, h * 64:(h + 1) * 64])
                    y = yb[i % 2]
                    P.tt("dve", y[:, :].r("p (h e) -> p h e", h=4), yb_[:, 256:512].r("p (h e) -> p h e", h=4),
                         sm[:, 8:12].un(2).bc([128, 4, 64]), ALU.mult)
                    P.tt("dve", y[:, :], y[:, :], yb_[:, 0:256], ALU.add)
                    P.tt("pool", xw[:, :].r("p (h e) -> p h e", h=4), tk[:, 0:256].r("p (h e) -> p h e", h=4),
                         self.lpv("ssd_d").un(2).bc([128, 4, 64]), ALU.mult)
                    P.tt("pool", y[:, :], y[:, :], xw[:, :], ALU.add)
                    P.tt("pool", xw[:, :].r("p (h e) -> p h e", h=4), tk[:, 0:256].r("p (h e) -> p h e", h=4),
                         sm[:, 16:20].un(2).bc([128, 4, 64]), ALU.mult)
                    hb = pb[2]
                    for g in range(2):
                        P.mm(hb[:, g * 128:(g + 1) * 128], tk[:, 256 + g * 128:256 + (g + 1) * 128], xw[:, g * 128:(g + 1) * 128])
                    P.tt("pool", hst[:, :].r("p (h e) -> p h e", h=4), hst[:, :].r("p (h e) -> p h e", h=4),
                         sm[:, 12:16].un(2).bc([128, 4, 64]), ALU.mult)
                    P.tt("dve", hst[:, :], hst[:, :], hb[:, 0:256], ALU.add)
                    P.tt("pool", y[:, :], y[:, :], zs[:, j, :], ALU.mult)
                    P.act(junk[:, :], y[:, :], AF.Square, accum=sm[:, 20:21])
                    P.act(sm[:, 21:22], sm[:, 20:21], AF.Ln, bias=EPS, scale=1.0 / 256)
                    P.act(sm[:, 21:22], sm[:, 21:22], AF.Exp, scale=-0.5)
                    P.stt("dve", y[:, :], y[:, :], sm[:, 21:22], self.lpv("ssd_norm"), ALU.mult, ALU.mult)
                    ob = pb[3]
                    for k in range(2):
                        P.tr(ob[:, k * 128:(k + 1) * 128], y[:, k * 128:(k + 1) * 128], ident)
                    P.copy("act", self.yT[3][:, :, i * 128:(i + 1) * 128], ob[:, 0:256].r("p (k t) -> p k t", k=2))

    def u_gdn(self, l, s):
        P = self.P
        pb = self.pb
        self._pj = 0
        ident = self.c("ident")
        ones = self.c("ones")
        with P.scope():
            wt = P.sb("gw", [128, 8, 1032], BF16)
            pre = P.sb("gpre", [128, 6, 515], F32)
            accs = [P.sb("gacc%d" % i, [128, 512], F32) for i in range(2)]
            qkv = [P.sb("gqkv%d" % i, [128, 6, 512], F32) for i in range(2)]
            sq = P.sb("gsq", [128, 512], F32)
            rsn = P.sb("grsn", [128, 512], F32)
            zs = P.sb("gzs", [128, 4, 256], F32)
            bg = P.sb("gbg", [128, 16, 8], F32)
            beta = P.sb("gbeta", [128, 16, 4], F32)
            lnb = P.sb("glnb", [128, 16, 4], F32)
            gg = P.sb("ggg", [128, 16, 4], F32)
            aneg = P.sb("ganeg", [128, 4], F32)
            kv = [P.sb("gkv%d" % i, [128, 512], F32) for i in range(2)]
            r1 = P.sb("gr1", [128, 4, 128], F32)
            r2 = P.sb("gr2", [128, 4, 128], F32)
            X2 = P.sb("gX2", [128, 4, 128], F32)
            X3 = P.sb("gX3", [128, 4, 128], F32)
            egB = P.sb("gegB", [128, 4, 128], F32)
            sm = P.sb("gsm", [128, 40], F32)
            qd = [P.sb("gqd%d" % i, [128, 2, 128], F32) for i in range(2)]
            kdec = [P.sb("gkd%d" % i, [128, 256], F32) for i in range(2)]
            kbg = P.sb("gkbg", [128, 256], F32)
            vb = P.sb("gvb", [128, 256], F32)
            NT = [P.sb("gNT%d" % h, [128, 128], F32) for h in range(4)]
            Nn = [P.sb("gN%d" % h, [128, 128], F32) for h in range(4)]
            Ma = [P.sb("gMa%d" % h, [128, 128], F32) for h in range(4)]
            MTa = [P.sb("gMTa%d" % h, [128, 128], F32) for h in range(4)]
            PT = [P.sb("gPT%d" % h, [128, 128], F32) for h in range(4)]
            Aq = [[P.sb("gAq%d_%d" % (b, h), [128, 128], F32) for h in range(4)] for b in range(2)]
            us = [P.sb("gu%d" % i, [128, 4, 64], F32) for i in range(2)]
            wTs = [P.sb("gwT%d" % i, [128, 2, 128], F32) for i in range(2)]
            gts = [P.sb("ggt%d" % i, [128, 4, 2], F32) for i in range(2)]
            S = P.sb("gS", [128, 2, 64], F32)
            vnew = P.sb("gvn", [128, 4, 64], F32)
            ot = P.sb("got", [128, 4, 64], F32)
            on = P.sb("gon", [128, 256], F32)
            P.dma("sp", wt[:, :, :], self.wb_in[l, :, OFF_GDN:OFF_GDN + 1032].rearrange("(k p) c -> p k c", p=128))
            cw = self.lpv("gdn_conv")
            bgb = pb[7]
            for i in range(16):
                for k in range(8):
                    P.mm(bgb[:, i * 8:(i + 1) * 8], self.hT[:, k, i * 128:(i + 1) * 128], wt[:, k, 1024:1032], start=(k == 0), stop=(k == 7))
            P.copy("dve", bg[:, :, :], bgb[:, 0:128].r("p (i c) -> p i c", c=8))
            P.act(lnb[:, :, :], bg[:, :, 0:4], AF.Exp, scale=-1.0)
            P.act(lnb[:, :, :], lnb[:, :, :], AF.Ln, bias=1.0)
            P.ts("dve", lnb[:, :, :], lnb[:, :, :], -1.0, ALU.mult)
            P.act(beta[:, :, :], lnb[:, :, :], AF.Exp)
            P.tt("dve", gg[:, :, :], bg[:, :, 4:8], self.lpv("gdn_dtb").un(1).bc([128, 16, 4]), ALU.add)
            P.act(gg[:, :, :], gg[:, :, :], AF.Exp)
            P.act(gg[:, :, :], gg[:, :, :], AF.Ln, bias=1.0)
            P.act(aneg[:, :], self.lpv("gdn_alog"), AF.Exp)
            P.stt("dve", gg[:, :, :], gg[:, :, :], -1.0, aneg[:, :].un(1).bc([128, 16, 4]), ALU.mult, ALU.mult)
            P.memset("pool", S[:, :, :], 0.0)

            def prep(i, qk):
                j = i % 4
                cs = slice(j * 128, (j + 1) * 128)
                b = i % 2
                kvt = kv[b]
                tb_ = pb[4]
                for m in range(4):
                    P.tr(tb_[:, m * 128:(m + 1) * 128], qk[:, 2 + m, cs], ident)
                P.copy("act", kvt[:, :], tb_[:, :])
                sb_ = pb[3]
                P.mm(sb_[:, 0:4], self.c("tri_bd"), gg[:, i, :])
                P.copy("dve", sm[:, 0:4], sb_[:, 0:4])
                P.tt("pool", r1[:, :, :], self.c("tri_bd").un(1).bc([128, 4, 128]), gg[:, i, :].un(2).bc([128, 4, 128]), ALU.mult)
                P.tt("pool", r2[:, :, :], self.c("ident").un(1).bc([128, 4, 128]), lnb[:, i, :].un(2).bc([128, 4, 128]), ALU.mult)
                P.tt("pool", r2[:, :, :], r2[:, :, :], r1[:, :, :], ALU.add)
                gcB, gcbB = pb[0], pb[1]
                P.mm(gcB[:, :], ones, r1[:, :, :].r("p h c -> p (h c)"))
                P.mm(gcbB[:, :], ones, r2[:, :, :].r("p h c -> p (h c)"))
                gc_b = sm[:, 0:4].un(2).bc([128, 4, 128])
                P.tt("dve", X2[:, :, :], gcB[:, :].r("p (h c) -> p h c", h=4), gc_b, ALU.subtract)
                P.tt("pool", X2[:, :, :], X2[:, :, :], self.c("neg_incl_bd").un(1).bc([128, 4, 128]), ALU.add)
                P.act(X2[:, :, :], X2[:, :, :], AF.Exp)
                P.tt("dve", X3[:, :, :], gcbB[:, :].r("p (h c) -> p h c", h=4), gc_b, ALU.subtract)
                P.tt("pool", X3[:, :, :], X3[:, :, :], self.c("neg_strict_bd").un(1).bc([128, 4, 128]), ALU.add)
                P.act(X3[:, :, :], X3[:, :, :], AF.Exp)
                P.act(egB[:, :, :], gcB[:, :].r("p (h c) -> p h c", h=4), AF.Exp)
                gt = gts[b]
                P.act(gt[:, :, :], gcB[:, :].r("p (h c) -> p h c", h=4)[:, :, 63:128:64], AF.Exp)
                P.copy("dve", sm[0:64, 4:8], gcB[0:64, :].r("p (h c) -> p h c", h=4)[:, :, 63])
                P.copy("dve", sm[64:128, 4:8], gcB[64:128, :].r("p (h c) -> p h c", h=4)[:, :, 127])
                P.act(sm[:, 8:12], sm[:, 0:4], AF.Exp)
                P.tt("dve", sm[:, 12:16], sm[:, 4:8], sm[:, 0:4], ALU.subtract)
                P.act(sm[:, 12:16], sm[:, 12:16], AF.Exp)
                P.tt("dve", sm[:, 16:20], sm[:, 8:12], beta[:, i, :], ALU.mult)
                kd = kdec[b]
                k4 = kvt[:, 0:256].r("p (h d) -> p h d", h=4)
                P.tt("pool", kd[:, :].r("p (h d) -> p h d", h=4), k4, sm[:, 12:16].un(2).bc([128, 4, 64]), ALU.mult)
                P.tt("pool", kbg[:, :].r("p (h d) -> p h d", h=4), k4, sm[:, 16:20].un(2).bc([128, 4, 64]), ALU.mult)
                P.tt("pool", vb[:, :].r("p (h d) -> p h d", h=4), kvt[:, 256:512].r("p (h d) -> p h d", h=4),
                     beta[:, i, :].un(2).bc([128, 4, 64]), ALU.mult)
                qdt = qd[b]
                for hp in range(2):
                    for hh in range(2):
                        r = slice(hh * 64, hh * 64 + 64)
                        P.tt("pool", qdt[r, hp, :], qk[r, hp, cs], egB[r, hp * 2 + hh, :], ALU.mult)
                for h in range(4):
                    hp, r = h // 2, slice((h % 2) * 64, (h % 2) * 64 + 64)
                    gb_ = pb[2]
                    P.mm(gb_[:, 0:128], qk[r, 2 + hp, cs], qk[r, 2 + hp, cs])
                    P.mm(gb_[:, 128:256], qk[r, 2 + hp, cs], qk[r, hp, cs])
                    P.stt("dve", NT[h][:, :], gb_[:, 0:128], -1.0, X3[:, h, :], ALU.mult, ALU.mult)
                    P.tt("dve", Aq[b][h][:, :], gb_[:, 128:256], X2[:, h, :], ALU.mult)
                    P.tr(gb_[:, 256:384], NT[h][:, :], ident)
                    P.copy("act", Nn[h][:, :], gb_[:, 256:384])
                    P.tt("pool", PT[h][:, :], NT[h][:, :], ident, ALU.add)
                    M, MT = Nn[h], NT[h]
                    for lvl in range(5):
                        db = pb[2] if lvl % 2 else pb[3]
                        P.mm(db[:, 0:128], MT[:, :], M[:, :])
                        if lvl < 4:
                            P.mm(db[:, 128:256], M[:, :], MT[:, :])
                        M2 = Ma[h] if lvl % 2 == 0 else Nn[h]
                        MT2 = MTa[h] if lvl % 2 == 0 else NT[h]
                        P.copy("act", M2[:, :], db[:, 0:128])
                        if lvl < 4:
                            P.copy("dve", MT2[:, :], db[:, 128:256])
                        P.mm(db[:, 256:384], M2[:, :], PT[h][:, :])
                        P.tt("dve", PT[h][:, :], PT[h][:, :], db[:, 256:384], ALU.add)
                        M, MT = M2, MT2
                    ub = pb[4]
                    P.mm(ub[:, 0:64], PT[h][:, :], vb[:, h * 64:(h + 1) * 64])
                    P.mm(ub[:, 128:256], kbg[:, hp * 128:(hp + 1) * 128], PT[h][:, :])
                    P.copy("act", us[b][:, h, :], ub[:, 0:64])
                    P.copy("dve", wTs[b][r, hp, :], ub[r, 128:256])

            def scan(i):
                b = i % 2
                j4 = i % 4
                for jc in range(2):
                    rj = slice(jc * 64, jc * 64 + 64)
                    wsb, ob, snb = pb[5], pb[6], pb[7]
                    for h in range(4):
                        hp, r = h // 2, slice((h % 2) * 64, (h % 2) * 64 + 64)
                        P.mm(wsb[:, h * 64:(h + 1) * 64], wTs[b][r, hp, :], S[r, hp, :])
                    for h in range(4):
                        P.tt("dve", vnew[rj, h, :], us[b][rj, h, :], wsb[rj, h * 64:(h + 1) * 64], ALU.subtract)
                    for h in range(4):
                        hp, r = h // 2, slice((h % 2) * 64, (h % 2) * 64 + 64)
                        P.mm(ob[:, h * 64:(h + 1) * 64], qd[b][r, hp, :], S[r, hp, :], start=True, stop=False)
                        P.mm(ob[:, h * 64:(h + 1) * 64], Aq[b][h][rj, :], vnew[rj, h, :], start=False, stop=True)
                        P.mm(snb[:, h * 64:(h + 1) * 64], kdec[b][rj, hp * 128:(hp + 1) * 128], vnew[rj, h, :])
                    for h in range(4):
                        hp, r = h // 2, slice((h % 2) * 64, (h % 2) * 64 + 64)
                        P.stt("dve", S[r, hp, :], S[r, hp, :], gts[b][r, h, jc:jc + 1], snb[r, h * 64:(h + 1) * 64], ALU.mult, ALU.add)
                    P.copy("act", ot[rj, :, :], ob[rj, 0:256].r("p (h e) -> p h e", h=4))
                P.tt("pool", on[:, :], ot[:, :, :].r("p h e -> p (h e)"), ot[:, :, :].r("p h e -> p (h e)"), ALU.mult)
                P.reduce("dve", sm[:, 20:24], on[:, :].r("p (h e) -> p h e", h=4))
                P.act(sm[:, 24:28], sm[:, 20:24], AF.Ln, bias=EPS, scale=1.0 / 64)
                P.act(sm[:, 24:28], sm[:, 24:28], AF.Exp, scale=-0.5)
                P.tt("pool", on[:, :].r("p (h e) -> p h e", h=4), ot[:, :, :], sm[:, 24:28].un(2).bc([128, 4, 64]), ALU.mult)
                P.tt("pool", on[:, :].r("p (h e) -> p h e", h=4), on[:, :].r("p (h e) -> p h e", h=4),
                     self.lpv("gdn_norm").un(1).bc([128, 4, 64]), ALU.mult)
                P.tt("pool", on[:, :], on[:, :], zs[:, j4, :], ALU.mult)
                tb2 = pb[4]
                for k in range(2):
                    P.tr(tb2[:, 256 + k * 128:256 + (k + 1) * 128], on[:, k * 128:(k + 1) * 128], ident)
                P.copy("act", self.yT[0][:, :, i * 128:(i + 1) * 128], tb2[:, 256:512].r("p (k t) -> p k t", k=2))

            for c in range(4):
                qk = qkv[c % 2]
                for m in range(6):
                    self._conv_chunk(wt, m * 128, m, c, pre, accs[m % 2], cw, qk[:, m, :], None, "dve" if m % 2 == 0 else "pool")
                for m in range(4):
                    P.act(sq[:, :], qk[:, m, :], AF.Square)
                    nb = pb[m % 2]
                    P.mm(nb[:, :], self.c("blk64"), sq[:, :])
                    P.act(rsn[:, :], nb[:, :], AF.Ln, bias=EPS)
                    P.act(rsn[:, :], rsn[:, :], AF.Exp, scale=-0.5)
                    if m < 2:
                        P.stt("dve", qk[:, m, :], qk[:, m, :], 0.125, rsn[:, :], ALU.mult, ALU.mult)
                    else:
                        P.tt("pool", qk[:, m, :], qk[:, m, :], rsn[:, :], ALU.mult)
                for j in range(4):
                    i = c * 4 + j
                    bank = pb[4 + j % 2]
                    self._proj_tm(wt, 768, 256, i, bank)
                    P.act(zs[:, j, :], bank[:, 0:256], AF.Silu)
                for j in range(4):
                    i = c * 4 + j
                    prep(i, qk)
                    scan(i)


def _prep_inputs(inputs, n_layers=2):
    f = lambda k: np.ascontiguousarray(np.asarray(inputs[k], dtype=np.float32)[:n_layers])
    shared = {"w_in": f("w_in"), "w_gate": f("w_gate"), "w_branch": f("w_branch"), "w_out": f("w_out"),
              "w_up": f("w_up"), "w_down": f("w_down"), "consts": CONST_ARR,
              "lp": np.stack([_layer_params(inputs, l) for l in range(n_layers)])}
    return shared


def kernel(**inputs):
    x = np.ascontiguousarray(np.asarray(inputs["x"], dtype=np.float32))
    n_cores = 8
    per = x.shape[0] // n_cores
    shared = _prep_inputs(inputs)
    nc = K(n_seq=per).build()
    in_maps = []
    for c in range(n_cores):
        m = dict(shared)
        m["x"] = x[c * per:(c + 1) * per].reshape(per * T, D)
        in_maps.append(m)
    res = run_bass_kernel_spmd(nc, in_maps, core_ids=list(range(n_cores)))
    out = np.stack([np.asarray(r["out"]).reshape(per, T, D) for r in res.results], axis=0)
    return out.reshape(x.shape).astype(np.float32)
```

```python
import numpy as np
from contextlib import ExitStack, contextmanager
import concourse.bass as bass
import concourse.mybir as mybir
from concourse.bass_utils import run_bass_kernel_spmd

F32 = mybir.dt.float32
BF16 = mybir.dt.bfloat16
AF = mybir.ActivationFunctionType
ALU = mybir.AluOpType
AX = mybir.AxisListType

ENGINES = ("pe", "act", "dve", "pool", "sp")


class Res:
    __slots__ = ("name", "last_w", "readers", "slot", "t", "excl", "pe_last")

    def __init__(self, name, t=None, excl=False):
        self.name = name
        self.excl = excl
        self.pe_last = None
        self.last_w = None
        self.readers = []
        self.slot = None
        self.t = t

    def __getitem__(self, idx):
        return V(self, self.t[idx])


class V:
    __slots__ = ("res", "ap")

    def __init__(self, res, ap):
        self.res = res
        self.ap = ap

    def __getitem__(self, idx):
        return V(self.res, self.ap[idx])

    def r(self, pat, **kw):
        return V(self.res, self.ap.rearrange(pat, **kw))

    def un(self, axis):
        return V(self.res, self.ap.unsqueeze(axis))

    def bc(self, shape):
        return V(self.res, self.ap.broadcast_to(list(shape)))


def _ap(x):
    return x.ap if isinstance(x, V) else x


def _rs(*xs):
    return [x.res for x in xs if isinstance(x, V)]


class Op:
    __slots__ = ("eng", "fn", "seq", "waits", "signal", "is_dma", "slot", "cnt",
                 "clock", "semval", "kind")

    def __init__(self, eng, fn, is_dma=False, slot=None):
        self.eng = eng
        self.fn = fn
        self.is_dma = is_dma
        self.slot = slot
        self.waits = []
        self.signal = False
        self.clock = None
        self.semval = None
        self.cnt = 0


class Prog:
    def __init__(self, nc):
        self.nc = nc
        self.ops = {e: [] for e in ENGINES}
        self.clock = {e: {} for e in ENGINES}
        self.nslots = 0
        self.slot_last = {}
        self.slot_cnt = {}
        self.all_res = []
        self.stack = None
        self.n_wait = 0
        self.free_slots = []

    def sb(self, name, shape, dtype):
        self.uid = getattr(self, "uid", 0) + 1
        name = "%s_%d" % (name, self.uid)
        t = self.stack.enter_context(self.nc.sbuf_tensor(name, list(shape), dtype))
        r = Res(name, t)
        self.all_res.append(r)
        return r

    def ps(self, name, shape, dtype=F32):
        t = self.stack.enter_context(self.nc.psum_tensor(name, list(shape), dtype))
        r = Res(name, t, excl=True)
        self.all_res.append(r)
        return r

    def res(self, name):
        r = Res(name, None)
        self.all_res.append(r)
        return r

    def _key(self, op):
        return ("s", op.slot) if op.is_dma else op.eng

    def _val(self, op):
        return op.cnt if op.is_dma else op.seq

    def _add_dep(self, op, dep, raw, force=False):
        if dep is None or dep is op:
            return
        if not force and not dep.is_dma and not op.is_dma and dep.eng == op.eng:
            if op.eng == "pe":
                return
        k = self._key(dep)
        v = self._val(dep)
        ck = self.clock[op.eng]
        if ck.get(k, -1) >= v:
            return
        op.waits.append(dep)
        dep.signal = True
        for kk, vv in dep.clock.items():
            if ck.get(kk, -1) < vv:
                ck[kk] = vv
        ck[k] = v

    def add(self, eng, fn, reads=(), writes=(), is_dma=False, force_dep=None):
        op = Op(eng, fn, is_dma=is_dma)
        xr = [r for r in reads if r.excl]
        if xr:
            reads = [r for r in reads if not r.excl]
            writes = list(writes) + [r for r in xr if r not in writes]
        lst = self.ops[eng]
        op.seq = len(lst)
        if is_dma:
            tile = None
            for r in list(writes) + list(reads):
                if r.t is not None:
                    tile = r
                    break
            assert tile is not None, "dma needs an sbuf tile resource"
            if tile.slot is None:
                if self.free_slots:
                    tile.slot = self.free_slots.pop()
                else:
                    tile.slot = self.nslots
                    self.nslots += 1
            op.slot = tile.slot
            op.cnt = self.slot_cnt.get(op.slot, 0) + 1
            self.slot_cnt[op.slot] = op.cnt
        cand = []
        for r in reads:
            if r.last_w is not None:
                cand.append((r.last_w, True))
        for w in writes:
            if w.last_w is not None:
                cand.append((w.last_w, True))
            for rd in w.readers:
                cand.append((rd, False))
        cand.sort(key=lambda t: -self._val(t[0]))
        for d, raw in cand:
            self._add_dep(op, d, raw)
        if force_dep is not None:
            self._add_dep(op, force_dep, True, force=True)
        for r in reads:
            r.readers.append(op)
        for w in writes:
            w.last_w = op
            w.readers = []
        ck = dict(self.clock[eng])
        if not is_dma:
            ck[eng] = op.seq
        else:
            ck[("s", op.slot)] = op.cnt
        op.clock = ck
        lst.append(op)
        return op

    def dma(self, q, out, in_, reads=(), writes=()):
        o, i = _ap(out), _ap(in_)
        return self.add(q, lambda e: e.dma_start(out=o, in_=i), list(reads) + _rs(in_), list(writes) + _rs(out), is_dma=True)

    def _pe_rowgroup_dep(self, out, stat):
        bp = stat.ap.base_partition()
        bp = bp() if callable(bp) else bp
        k = stat.ap.shape[0]
        groups = set(range(bp // 32, (bp + k + 31) // 32))
        prev = out.res.pe_last
        force = prev[0] if (prev is not None and prev[1].isdisjoint(groups)) else None
        return groups, force

    def mm(self, out, lhsT, rhs, start=True, stop=True):
        o, l, r = out.ap, lhsT.ap, rhs.ap
        groups, force = self._pe_rowgroup_dep(out, lhsT)
        op = self.add("pe", lambda e: e.matmul(o, lhsT=l, rhs=r, start=start, stop=stop), _rs(lhsT, rhs), _rs(out), force_dep=force)
        out.res.pe_last = (op, groups)
        return op

    def tr(self, out, in_, ident):
        o, i, d = out.ap, in_.ap, ident.ap
        groups, force = self._pe_rowgroup_dep(out, in_)
        op = self.add("pe", lambda e: e.transpose(out=o, in_=i, identity=d), _rs(in_, ident), _rs(out), force_dep=force)
        out.res.pe_last = (op, groups)
        return op

    def act(self, out, in_, func, bias=0.0, scale=1.0, accum=None):
        o, i, b, sc = out.ap, in_.ap, _ap(bias), _ap(scale)
        if accum is None:
            return self.add("act", lambda e: e.activation(out=o, in_=i, func=func, bias=b, scale=sc), _rs(in_, bias, scale), _rs(out))
        a = accum.ap
        return self.add("act", lambda e: e.activation(out=o, in_=i, func=func, bias=b, scale=sc, accum_out=a), _rs(in_, bias, scale), _rs(out, accum))

    def tt(self, eng, out, in0, in1, op):
        o, a, b = out.ap, in0.ap, in1.ap
        return self.add(eng, lambda e: e.tensor_tensor(out=o, in0=a, in1=b, op=op), _rs(in0, in1), _rs(out))

    def ts(self, eng, out, in0, s1, op0, s2=None, op1=None):
        o, a, x1, x2 = out.ap, in0.ap, _ap(s1), _ap(s2)
        if op1 is None:
            return self.add(eng, lambda e: e.tensor_scalar(out=o, in0=a, scalar1=x1, scalar2=None, op0=op0), _rs(in0, s1), _rs(out))
        return self.add(eng, lambda e: e.tensor_scalar(out=o, in0=a, scalar1=x1, scalar2=x2, op0=op0, op1=op1), _rs(in0, s1, s2), _rs(out))

    def stt(self, eng, out, in0, scalar, in1, op0, op1):
        o, a, sc, b = out.ap, in0.ap, _ap(scalar), in1.ap
        return self.add(eng, lambda e: e.scalar_tensor_tensor(out=o, in0=a, scalar=sc, in1=b, op0=op0, op1=op1), _rs(in0, scalar, in1), _rs(out))

    def copy(self, eng, out, in_):
        o, i = out.ap, in_.ap
        if eng == "act":
            return self.add("act", lambda e: e.copy(out=o, in_=i), _rs(in_), _rs(out))
        return self.add(eng, lambda e: e.tensor_copy(out=o, in_=i), _rs(in_), _rs(out))

    def memset(self, eng, out, val):
        o = out.ap
        return self.add(eng, lambda e: e.memset(o, val), [], _rs(out))

    def reduce(self, eng, out, in_, op=None):
        o, i = out.ap, in_.ap
        op = op or ALU.add
        return self.add(eng, lambda e: e.tensor_reduce(out=o, in_=i, axis=AX.X, op=op), _rs(in_), _rs(out))

    def max8(self, out, in_):
        o, i = out.ap, in_.ap
        return self.add("dve", lambda e: e.max(out=o, in_=i), _rs(in_), _rs(out))

    def recip(self, out, in_):
        o, i = out.ap, in_.ap
        return self.add("dve", lambda e: e.reciprocal(out=o, in_=i), _rs(in_), _rs(out))

    def barrier(self):
        lasts = []
        for e in ENGINES:
            for op in reversed(self.ops[e]):
                if not op.is_dma and op.fn is not None:
                    lasts.append(op)
                    break
        dmas = [op for e in ENGINES for op in self.ops[e] if op.is_dma and self.slot_cnt[op.slot] == op.cnt]
        for e in ENGINES:
            op = Op(e, None)
            op.seq = len(self.ops[e])
            for d in lasts:
                if d.eng != e:
                    self._add_dep(op, d, True)
            for d in dmas:
                self._add_dep(op, d, True)
            ck = dict(self.clock[e])
            op.clock = ck
            op.seq = -1
            self.ops[e].append(op)

    @contextmanager
    def scope(self):
        old = self.stack
        mark = len(self.all_res)
        with ExitStack() as st:
            self.stack = st
            yield
            self.barrier()
            for r in self.all_res[mark:]:
                if r.slot is not None:
                    self.free_slots.append(r.slot)
                    r.slot = None
            del self.all_res[mark:]
        self.stack = old

    def emit(self, final_wait_ops=()):
        nc = self.nc
        from contextlib import ExitStack
        with ExitStack() as st:
            esem = {e: st.enter_context(nc.semaphore("sem_" + e)) for e in ENGINES}
            ssem = {s: st.enter_context(nc.semaphore("dsem%d" % s)) for s in range(self.nslots)}
            for e in ENGINES:
                v = 0
                for op in self.ops[e]:
                    if op.fn is None or op.is_dma:
                        continue
                    if op.signal:
                        v += 1
                    op.semval = v
            block = st.enter_context(nc.Block())

            def run(e, eng):
                for op in self.ops[e]:
                    for d in op.waits:
                        if d.is_dma:
                            eng.wait_ge(ssem[d.slot], 16 * d.cnt)
                        else:
                            eng.wait_ge(esem[d.eng], d.semval)
                        self.n_wait += 1
                    if op.fn is None:
                        continue
                    ins = op.fn(eng)
                    if op.is_dma:
                        ins.then_inc(ssem[op.slot], 16)
                    elif op.signal:
                        ins.then_inc(esem[e], 1)

            @block.tensor
            def _(eng):
                run("pe", eng)

            @block.scalar
            def _(eng):
                run("act", eng)

            @block.vector
            def _(eng):
                run("dve", eng)

            @block.gpsimd
            def _(eng):
                run("pool", eng)

            @block.sync
            def _(eng):
                run("sp", eng)

D = 1024
T = 2048
NH = 4
HD = 64
BW = 256
DFF = 4096
OFF_GDN, OFF_MOBA, OFF_SB, OFF_SSD, INW = 0, 1032, 1800, 2568, 3596
EPS = 1e-6
NEG = -30000.0


def _const_table():
    p = np.arange(128)[:, None]
    f = np.arange(128)[None, :]
    c = {}
    c["ident"] = (p == f)
    c["ones"] = np.ones((128, 128))
    c["blk64"] = (p // 64 == f // 64)
    c["tri128"] = (p <= f)
    c["neg_ssd"] = np.where(f >= p, 0.0, NEG)
    same = (p // 64 == f // 64)
    c["tri_bd"] = same & (p <= f)
    c["neg_incl_bd"] = np.where(same & (f >= p), 0.0, NEG)
    c["neg_strict_bd"] = np.where(same & (f > p), 0.0, NEG)
    c["m_strict"] = (f > p)
    c["m_incl"] = (f >= p)
    c["u_incl"] = (p >= f)
    i = np.arange(16)[:, None, None]
    n = np.arange(8)[None, None, :]
    h = np.arange(4)[None, :, None]
    past = np.broadcast_to(n < i // 2, (16, 4, 8))
    own = np.broadcast_to(n == i // 2, (16, 4, 8))
    c["pastbias"] = np.broadcast_to(np.where(past, 0.0, -1e30).reshape(1, 512), (128, 512))
    c["pastm"] = np.broadcast_to(past.reshape(1, 512).astype(np.float64), (128, 512))
    c["ownm"] = np.broadcast_to(own.reshape(1, 512).astype(np.float64), (128, 512))
    slopes = np.array([2.0 ** (-8.0 * (k + 1) / 4) for k in range(4)])
    dl = np.arange(16)[None, None, :] - 12
    c["alibi"] = (slopes[None, :, None] * (dl * 128 + np.arange(128)[:, None, None])).reshape(128, 64)
    v = np.zeros((128, 32))
    for r in range(32):
        v[r, r] = 1.0
    for hh in range(4):
        v[32, hh * 8:(hh + 1) * 8] = slopes[hh]
        v[33, hh * 8:(hh + 1) * 8] = slopes[hh]
    c["selv"] = v
    t = np.arange(512)
    a = np.zeros((128, 512))
    a[32] = -128.0 * (t // 128)
    a[33] = -(t % 128).astype(np.float64)
    c["augrows"] = a
    offs = {}
    cols = []
    o = 0
    for k, val in c.items():
        val = np.asarray(val, dtype=np.float32)
        offs[k] = (o, val.shape[1])
        cols.append(val)
        o += val.shape[1]
    return np.ascontiguousarray(np.concatenate(cols, axis=1)), offs


CONST_ARR, CONST_OFF = _const_table()
NCONST = CONST_ARR.shape[1]

LP_OFF = {}
_o = 0
for _k, _w in (("g_mix_post", 1024), ("g_ffn_post", 1024), ("gdn_conv", 24), ("ssd_conv", 24), ("ssd_cb", 6),
               ("gdn_alog", 4), ("gdn_dtb", 4), ("ssd_alog", 4), ("ssd_dtb", 4), ("ssd_d", 4),
               ("gdn_norm", 64), ("ssd_norm", 256), ("rowg", 16)):
    LP_OFF[_k] = (_o, _w)
    _o += _w
NLP = _o


def _layer_params(inp, l):
    lp = np.zeros((128, NLP), np.float32)

    def put(k, arr):
        o, w = LP_OFF[k]
        lp[:, o:o + w] = arr
    put("g_mix_post", np.broadcast_to(inp["norm_mix_post"][l][None, :], (128, 1024)))
    put("g_ffn_post", np.broadcast_to(inp["norm_ffn_post"][l][None, :], (128, 1024)))
    put("gdn_conv", inp["gdn_conv"][l].reshape(4, 6, 128).transpose(2, 1, 0).reshape(128, 24))
    put("ssd_conv", inp["ssd_conv"][l].reshape(4, 6, 128).transpose(2, 1, 0).reshape(128, 24))
    put("ssd_cb", inp["ssd_conv_bias"][l].reshape(6, 128).T)
    for k, src in (("gdn_alog", "gdn_a_log"), ("gdn_dtb", "gdn_dt_bias"), ("ssd_alog", "ssd_a_log"),
                   ("ssd_dtb", "ssd_dt_bias"), ("ssd_d", "ssd_d")):
        put(k, np.broadcast_to(inp[src][l][None, :], (128, 4)))
    put("gdn_norm", np.broadcast_to(inp["gdn_norm"][l][None, :], (128, 64)))
    put("ssd_norm", np.broadcast_to(inp["ssd_norm"][l][None, :], (128, 256)))
    rg = np.concatenate([inp["norm_mix_pre"][l].reshape(8, 128).T, inp["norm_ffn_pre"][l].reshape(8, 128).T], axis=1)
    put("rowg", rg)
    return lp


class K:
    def __init__(self, n_seq=4, n_layers=2, units=("gdn", "moba", "sb", "ssd", "merge"), dbg=None):
        self.n_seq, self.n_layers, self.units, self.dbg = n_seq, n_layers, units, (dbg or {})
        nc = bass.Bass("TRN2", target_bir_lowering=False)
        self.nc = nc
        self.P = Prog(nc)
        NL = n_layers
        di = lambda name, shape, dt=F32: nc.dram_tensor(name, list(shape), dt, kind="ExternalInput").ap()
        dn = lambda name, shape, dt=BF16: nc.dram_tensor(name, list(shape), dt).ap()
        self.x = di("x", [n_seq * T, D])
        self.w_in = di("w_in", [NL, D, INW])
        self.w_gate = di("w_gate", [NL, 4, D, D])
        self.w_branch = di("w_branch", [NL, 4, BW, D])
        self.w_out = di("w_out", [NL, D, D])
        self.w_up = di("w_up", [NL, D, DFF])
        self.w_down = di("w_down", [NL, DFF, D])
        self.consts = di("consts", [128, NCONST])
        self.lpd = di("lp", [NL, 128, NLP])
        self.out = nc.dram_tensor("out", [n_seq * T, D], F32, kind="ExternalOutput").ap()
        self.xmid = dn("xmid", [n_seq * T, D], F32)
        self.wb_in = dn("wb_in", [NL, D, INW])
        self.wb_gate = dn("wb_gate", [NL, 8, 128, 4, 8, 128])
        self.wb_br = dn("wb_br", [NL, 8, 128, 4, 2, 128])
        self.wb_out = dn("wb_out", [NL, D, D])
        self.wb_up = dn("wb_up", [NL, 32, 128, 8, 128])
        self.wb_down = dn("wb_down", [NL, DFF, D])
        if "inject_yT" in self.dbg:
            self.dbg_yT = di("dbg_yT", [4, BW, T])
        if "dump_yT" in self.dbg:
            self.dbg_yT_out = nc.dram_tensor("dbg_yT_out", [4, BW, T], F32, kind="ExternalOutput").ap()
        if "dump_hT" in self.dbg:
            self.dbg_hT_out = nc.dram_tensor("dbg_hT_out", [D, T], F32, kind="ExternalOutput").ap()
        self._q = 0

    def q(self):
        return "sp"

    def c(self, name):
        o, w = CONST_OFF[name]
        return self.cst[:, o:o + w]

    def lpv(self, name):
        o, w = LP_OFF[name]
        return self.lp[:, o:o + w]

    def build(self):
        P = self.P
        with ExitStack() as st:
            P.stack = st
            self.pb = [P.ps("pb%d" % i, [128, 512], F32) for i in range(8)]
            self.cst = P.sb("cst", [128, NCONST], F32)
            self.lp = P.sb("lp_sb", [128, NLP], F32)
            self.ones_bf = P.sb("ones_bf", [128, 128], BF16)
            self.uincl_bf = P.sb("uincl_bf", [128, 128], BF16)
            self.sel_bf = P.sb("sel_bf", [128, 32, 128], BF16)
            self.hT = P.sb("hT", [128, 8, T], BF16)
            self.yT = [P.sb("yT%d" % g, [128, 2, T], BF16) for g in range(4)]
            P.dma("sp", self.cst[:, :], self.consts[:, :])
            P.copy("dve", self.ones_bf[:, :], self.c("ones"))
            P.copy("dve", self.uincl_bf[:, :], self.c("u_incl"))
            P.copy("pool", self.sel_bf[0:34, :, :], self.c("selv")[0:34, :].un(2).bc([34, 32, 128]))
            if "dump_yT" in self.dbg:
                for g in range(4):
                    P.memset("pool", self.yT[g][:, :, :], 0.0)
            for l in range(self.n_layers):
                P.dma("sp", self.lp[:, :], self.lpd[l, :, :])
                if "noconvert" not in self.dbg:
                    self.convert_weights(l)
                xsrc = self.x if l == 0 else self.xmid
                xdst = self.out if l == self.n_layers - 1 else self.xmid
                for s in range(self.n_seq):
                    self.u_norm(l, s, xsrc)
                    if "dump_hT" in self.dbg:
                        self.dump_hT()
                    if "inject_yT" in self.dbg:
                        self.inject_yT()
                    if "sb" in self.units:
                        self.u_sb(l, s)
                    if "moba" in self.units:
                        self.u_moba(l, s)
                    if "ssd" in self.units:
                        self.u_ssd(l, s)
                    if "gdn" in self.units:
                        self.u_gdn(l, s)
                    if "dump_yT" in self.dbg:
                        self.dump_yT()
                    if "merge" in self.units:
                        self.u_merge(l, s, xsrc, xdst)
            P.barrier()
            P.emit()
        return self.nc

    def dump_hT(self):
        P = self.P
        with P.scope():
            t = P.sb("dh", [128, 8, T], F32)
            P.copy("dve", t[:, :, :], self.hT[:, :, :])
            P.dma("sp", self.dbg_hT_out.rearrange("(k p) t -> p k t", p=128), t[:, :, :])

    def dump_yT(self):
        P = self.P
        with P.scope():
            for g in range(4):
                t = P.sb("dy%d" % g, [128, 2, T], F32)
                P.copy("dve", t[:, :, :], self.yT[g][:, :, :])
                P.dma("sp", self.dbg_yT_out[g].rearrange("(k p) t -> p k t", p=128), t[:, :, :])

    def inject_yT(self):
        P = self.P
        with P.scope():
            for g in range(4):
                t = P.sb("iy%d" % g, [128, 2, T], F32)
                P.dma("sp", t[:, :, :], self.dbg_yT[g].rearrange("(k p) t -> p k t", p=128))
                P.copy("dve", self.yT[g][:, :, :], t[:, :, :])

    def convert_weights(self, l):
        P = self.P
        with P.scope():
            st32 = [P.sb("cv32_%d" % i, [128, 4096], F32) for i in range(2)]
            st16 = [P.sb("cv16_%d" % i, [128, 4096], BF16) for i in range(2)]
            cnt = [0]
            rowg = self.lpv("rowg")

            def cv(src, w, scales, stores):
                b = cnt[0] % 2
                cnt[0] += 1
                s32, s16 = st32[b], st16[b]
                P.dma(self.q(), s32[:, 0:w] if len(src.shape) == 2 else s32[:, 0:w].r("p (k c) -> p k c", k=src.shape[1]), src)
                eng = "dve"
                if scales is None:
                    P.copy(("act", "act", "dve")[cnt[0] % 3], s16[:, 0:w], s32[:, 0:w])
                else:
                    for (a, bnd, col) in scales:
                        P.ts(eng, s16[:, a:bnd], s32[:, a:bnd], rowg[:, col:col + 1], ALU.mult)
                for (dst, a, bnd, pat, kw) in stores:
                    v = s16[:, a:bnd]
                    if pat:
                        v = v.r(pat, **kw)
                    P.dma(self.q(), dst, v)

            for kt in range(8):
                cv(self.w_in[l, kt * 128:(kt + 1) * 128, :], INW, [(0, INW, kt)],
                   [(self.wb_in[l, kt * 128:(kt + 1) * 128, :], 0, INW, None, None)])
            for g in range(4):
                for half in range(2):
                    src = self.w_gate[l, g, half * 512:(half + 1) * 512, :].rearrange("(k p) c -> p k c", p=128)
                    cv(src, 4096, [(k * 1024, (k + 1) * 1024, half * 4 + k) for k in range(4)],
                       [(self.wb_gate[l, :, :, g, half * 4 + k, :].rearrange("c p j -> p c j"), k * 1024, (k + 1) * 1024,
                         "p (c j) -> p c j", dict(j=128)) for k in range(4)])
            for g in range(4):
                src = self.w_branch[l, g].rearrange("(k p) c -> p k c", p=128)
                cv(src, 2048, None,
                   [(self.wb_br[l, :, :, g, k, :].rearrange("c p j -> p c j"), k * 1024, (k + 1) * 1024,
                     "p (c j) -> p c j", dict(j=128)) for k in range(2)])
            for half in range(2):
                src = self.w_out[l, half * 512:(half + 1) * 512, :].rearrange("(k p) c -> p k c", p=128)
                cv(src, 4096, None,
                   [(self.wb_out[l, half * 512:(half + 1) * 512, :].rearrange("(k p) c -> p k c", p=128), 0, 4096,
                     "p (k c) -> p k c", dict(k=4))])
            for kt in range(8):
                cv(self.w_up[l, kt * 128:(kt + 1) * 128, :], 4096, [(0, 4096, 8 + kt)],
                   [(self.wb_up[l, f0:f0 + 8, :, kt, :].rearrange("f p j -> p f j"), f0 * 128, (f0 + 8) * 128,
                     "p (f j) -> p f j", dict(j=128)) for f0 in range(0, 32, 8)])
            for qd in range(8):
                src = self.w_down[l, qd * 512:(qd + 1) * 512, :].rearrange("(k p) c -> p k c", p=128)
                cv(src, 4096, None,
                   [(self.wb_down[l, qd * 512:(qd + 1) * 512, :].rearrange("(k p) c -> p k c", p=128), 0, 4096,
                     "p (k c) -> p k c", dict(k=4))])

    def u_norm(self, l, s, xsrc):
        P = self.P
        ident = self.c("ident")
        with P.scope():
            xt = [P.sb("nx%d" % i, [128, D], F32) for i in range(2)]
            hh = [P.sb("nh%d" % i, [128, D], F32) for i in range(2)]
            junk = P.sb("njunk", [128, D], F32)
            ss = [P.sb("nss%d" % i, [128, 1], F32) for i in range(2)]
            rs = [P.sb("nrs%d" % i, [128, 1], F32) for i in range(2)]
            for i in range(16):
                b = i % 2
                r0 = s * T + i * 128
                P.dma(self.q(), xt[b][:, :], xsrc[r0:r0 + 128, :])
                P.act(junk[:, :], xt[b][:, :], AF.Square, accum=ss[b][:, :])
                P.act(rs[b][:, :], ss[b][:, :], AF.Ln, bias=EPS, scale=1.0 / D)
                P.act(rs[b][:, :], rs[b][:, :], AF.Exp, scale=-0.5)
                P.ts("dve", hh[b][:, :], xt[b][:, :], rs[b][:, 0:1], ALU.mult)
                for g in range(2):
                    bank = self.pb[(2 * i + g) % 4]
                    for k in range(4):
                        kk = g * 4 + k
                        P.tr(bank[:, k * 128:(k + 1) * 128], hh[b][:, kk * 128:(kk + 1) * 128], ident)
                    P.copy("act" if g == 0 else "dve", self.hT[:, g * 4:(g + 1) * 4, i * 128:(i + 1) * 128],
                           bank[:, :].r("p (k n) -> p k n", k=4))

    def u_merge(self, l, s, xsrc, xdst):
        P = self.P
        ident = self.c("ident")
        g_post, g_fpost = self.lpv("g_mix_post"), self.lpv("g_ffn_post")
        pb = self.pb
        with P.scope():
            ws = [P.sb("mw%d" % i, [128, 4096], BF16) for i in range(3)]
            wbr = [P.sb("mwb%d" % i, [128, 4, 2, 128], BF16) for i in range(2)]
            xs = P.sb("mx", [128, 4, D], F32)
            acc = P.sb("macc", [128, 512], F32)
            sg = [P.sb("msg%d" % i, [128, 512], F32) for i in range(2)]
            mT = P.sb("mmT", [128, 8, 512], BF16)
            tmp = [P.sb("mtmp%d" % i, [128, D], F32) for i in range(2)]
            h2T = P.sb("mh2T", [128, 8, 512], BF16)
            aT = P.sb("maT", [128, 32, 512], BF16)
            rl = [P.sb("mrl%d" % i, [128, 512], F32) for i in range(2)]
            ss = P.sb("mss", [128, 8], F32)
            rs = P.sb("mrs", [128, 4], F32)
            wi = [0]

            def wload(src, pat=None, **kw):
                t = ws[wi[0] % 3]
                wi[0] += 1
                P.dma(self.q(), t[:, :].r(pat, **kw) if pat else t[:, :], src)
                return t

            for tb in range(4):
                t0 = tb * 512
                r0 = s * T + t0
                P.dma(self.q(), xs[:, :, :], xsrc[r0:r0 + 512, :].rearrange("(j p) c -> p j c", p=128))
                for cc in range(8):
                    wg = wload(self.wb_gate[l, cc].rearrange("p g k j -> p (g k j)"))
                    wb_ = wbr[cc % 2]
                    P.dma(self.q(), wb_[:, :, :, :], self.wb_br[l, cc])
                    for g in range(4):
                        gp, bp = pb[(2 * g) % 4], pb[(2 * g + 1) % 4]
                        for k in range(8):
                            P.mm(gp[:, :], wg[:, (g * 8 + k) * 128:(g * 8 + k + 1) * 128], self.hT[:, k, t0 + s * 0:t0 + 512],
                                 start=(k == 0), stop=(k == 7))
                        for k in range(2):
                            P.mm(bp[:, :], wb_[:, g, k, :], self.yT[g][:, k, t0:t0 + 512], start=(k == 0), stop=(k == 1))
                        sgt = sg[g % 2]
                        P.act(sgt[:, :], gp[:, :], AF.Sigmoid)
                        if g == 0:
                            P.tt("dve", acc[:, :], sgt[:, :], bp[:, :], ALU.mult)
                        else:
                            P.tt("dve", sgt[:, :], sgt[:, :], bp[:, :], ALU.mult)
                            if g < 3:
                                P.tt("pool", acc[:, :], acc[:, :], sgt[:, :], ALU.add)
                            else:
                                P.tt("pool", mT[:, cc, :], acc[:, :], sgt[:, :], ALU.add)
                for half in range(2):
                    wo = wload(self.wb_out[l, :, half * 512:(half + 1) * 512].rearrange("(k p) c -> p k c", p=128), "p (k c) -> p k c", k=8)
                    for j in range(4):
                        bank = pb[4 + half * 2 + j % 2] if False else pb[half * 4 + j]
                        for k in range(8):
                            P.mm(bank[:, :], mT[:, k, j * 128:(j + 1) * 128], wo[:, k * 512:(k + 1) * 512],
                                 start=(k == 0), stop=(k == 7))
                for j in range(4):
                    for half in range(2):
                        P.act(tmp[0][:, half * 512:(half + 1) * 512], pb[half * 4 + j][:, :], AF.Square,
                              accum=ss[:, half:half + 1])
                    P.tt("dve", ss[:, 2:3], ss[:, 0:1], ss[:, 1:2], ALU.add)
                    P.act(rs[:, 0:1], ss[:, 2:3], AF.Ln, bias=EPS, scale=1.0 / D)
                    P.act(rs[:, 0:1], rs[:, 0:1], AF.Exp, scale=-0.5)
                    for half in range(2):
                        cs = slice(half * 512, (half + 1) * 512)
                        P.stt("dve", tmp[1][:, cs], pb[half * 4 + j][:, :], rs[:, 0:1], g_post[:, cs], ALU.mult, ALU.mult)
                    P.tt("pool", xs[:, j, :], xs[:, j, :], tmp[1][:, :], ALU.add)
                    P.act(tmp[0][:, :], xs[:, j, :], AF.Square, accum=ss[:, 3:4])
                    P.act(rs[:, 1:2], ss[:, 3:4], AF.Ln, bias=EPS, scale=1.0 / D)
                    P.act(rs[:, 1:2], rs[:, 1:2], AF.Exp, scale=-0.5)
                    P.ts("dve", tmp[1][:, :], xs[:, j, :], rs[:, 1:2], ALU.mult)
                    for g in range(2):
                        bank = pb[half * 0 + g * 4 + j]
                        for k in range(4):
                            kk = g * 4 + k
                            P.tr(bank[:, k * 128:(k + 1) * 128], tmp[1][:, kk * 128:(kk + 1) * 128], ident)
                        P.copy("act" if g == 0 else "dve", h2T[:, g * 4:(g + 1) * 4, j * 128:(j + 1) * 128],
                               bank[:, :].r("p (k n) -> p k n", k=4))
                for f0 in range(0, 32, 4):
                    wu = wload(self.wb_up[l, f0:f0 + 4].rearrange("f p k j -> p f (k j)"), "p (f c) -> p f c", f=4)
                    for fi in range(4):
                        f = f0 + fi
                        bank = pb[f % 4]
                        for k in range(8):
                            P.mm(bank[:, :], wu[:, (fi * 8 + k) * 128:(fi * 8 + k + 1) * 128], h2T[:, k, :],
                                 start=(k == 0), stop=(k == 7))
                        r = rl[f % 2]
                        P.act(r[:, :], bank[:, :], AF.Relu)
                        P.tt("pool", aT[:, f, :], r[:, :], r[:, :], ALU.mult)
                for k0 in range(0, 32, 4):
                    wd = wload(self.wb_down[l, k0 * 128:(k0 + 4) * 128, :].rearrange("(k p) c -> p k c", p=128), "p (k c) -> p k c", k=4)
                    for ki in range(4):
                        k = k0 + ki
                        for j in range(4):
                            for half in range(2):
                                P.mm(pb[half * 4 + j][:, :], aT[:, k, j * 128:(j + 1) * 128],
                                     wd[:, ki * 1024 + half * 512:ki * 1024 + (half + 1) * 512],
                                     start=(k == 0), stop=(k == 31))
                for j in range(4):
                    for half in range(2):
                        P.act(tmp[0][:, half * 512:(half + 1) * 512], pb[half * 4 + j][:, :], AF.Square,
                              accum=ss[:, 4 + half:5 + half])
                    P.tt("dve", ss[:, 6:7], ss[:, 4:5], ss[:, 5:6], ALU.add)
                    P.act(rs[:, 2:3], ss[:, 6:7], AF.Ln, bias=EPS, scale=1.0 / D)
                    P.act(rs[:, 2:3], rs[:, 2:3], AF.Exp, scale=-0.5)
                    for half in range(2):
                        cs = slice(half * 512, (half + 1) * 512)
                        P.stt("dve", tmp[1][:, cs], pb[half * 4 + j][:, :], rs[:, 2:3], g_fpost[:, cs], ALU.mult, ALU.mult)
                    P.tt("pool", xs[:, j, :], xs[:, j, :], tmp[1][:, :], ALU.add)
                P.dma(self.q(), xdst[r0:r0 + 512, :].rearrange("(j p) c -> p j c", p=128), xs[:, :, :])

    def _proj_fm(self, wt, col0, out_writer, nchunks=4):
        P = self.P
        for c in range(nchunks):
            bank = self.pb[self._pj % 4]
            self._pj += 1
            for k in range(8):
                P.mm(bank[:, :], wt[:, k, col0:col0 + 128], self.hT[:, k, c * 512:(c + 1) * 512], start=(k == 0), stop=(k == 7))
            out_writer(c, bank[:, :])

    def _proj_tm(self, wt, col0, ncol, i, bank):
        P = self.P
        for k in range(8):
            P.mm(bank[:, 0:ncol], self.hT[:, k, i * 128:(i + 1) * 128], wt[:, k, col0:col0 + ncol], start=(k == 0), stop=(k == 7))

    def u_sb(self, l, s):
        P = self.P
        pb = self.pb
        self._pj = 0
        with P.scope():
            wt = P.sb("sw", [128, 8, 768], BF16)
            qT = P.sb("sq", [128, 2, T], BF16)
            kT = P.sb("sk", [128, 2, T], BF16)
            vv = P.sb("sv", [128, 16, 256], BF16)
            NS = 4
            E = [P.sb("sE%d" % i, [128, 512], F32) for i in range(NS)]
            Lp = [P.sb("sL%d" % i, [128, 512], BF16) for i in range(NS)]
            G = [P.sb("sG%d" % i, [128, 512], F32) for i in range(NS)]
            Wt = [P.sb("sW%d" % i, [128, 512], BF16) for i in range(NS)]
            Pacc = [P.sb("sPacc%d" % i, [128, 512], BF16) for i in range(NS)]
            P.dma("sp", wt[:, :, :], self.wb_in[l, :, OFF_SB:OFF_SB + 768].rearrange("(k p) c -> p k c", p=128))
            for hp in range(2):
                self._proj_fm(wt, hp * 128, lambda c, ps, hp=hp: P.ts("dve", qT[:, hp, c * 512:(c + 1) * 512], ps, 0.125, ALU.mult))
                self._proj_fm(wt, 256 + hp * 128, lambda c, ps, hp=hp: P.copy("act", kT[:, hp, c * 512:(c + 1) * 512], ps))
            for i in range(16):
                bank = pb[4 + i % 2]
                self._proj_tm(wt, 512, 256, i, bank)
                P.copy("act" if i % 2 else "dve", vv[:, i, :], bank[:, 0:256])

            def stream(h, qb, sl):
                hp, r = h // 2, slice((h % 2) * 64, (h % 2) * 64 + 64)
                q0 = qb * 512
                wb_, accb = pb[sl], pb[4 + sl]
                Es, Ls, Gs, Ws, Pa = E[sl], Lp[sl], G[sl], Wt[sl], Pacc[sl]
                P.memset("pool", Pa[:, :], 0.0)
                nk = 4 * qb + 4
                for kt in range(nk - 1, -1, -1):
                    c0 = max(0, kt - 4 * qb) * 128
                    first = (kt == nk - 1)
                    P.mm(wb_[:, c0:512], kT[r, hp, kt * 128:(kt + 1) * 128], qT[r, hp, q0 + c0:q0 + 512])
                    yield
                    P.act(Es[:, c0:512], wb_[:, c0:512], AF.Exp)
                    yield
                    if kt >= 4 * qb:
                        P.tt("pool", Es[:, c0:c0 + 128], Es[:, c0:c0 + 128], self.c("m_strict"), ALU.mult)
                        yield
                    P.act(Ls[:, c0:512], Es[:, c0:512], AF.Ln, bias=1.0)
                    yield
                    P.mm(wb_[:, c0:512], self.uincl_bf[:, :], Ls[:, c0:512], start=True, stop=first)
                    if not first:
                        P.mm(wb_[:, c0:512], self.ones_bf[:, :], Pa[:, c0:512], start=False, stop=True)
                    yield
                    P.act(Gs[:, c0:512], wb_[:, c0:512], AF.Exp, scale=-1.0)
                    if c0 > 0:
                        P.memset("pool", Ws[:, 0:c0], 0.0)
                    yield
                    P.tt("dve", Ws[:, c0:512], Es[:, c0:512], Gs[:, c0:512], ALU.mult)
                    if kt > 0:
                        P.tt("pool", Pa[:, c0:512], Pa[:, c0:512], Ls[:, c0:512], ALU.add)
                    yield
                    P.mm(accb[:, :], vv[:, kt, hp * 128:(hp + 1) * 128], Ws[:, :], start=first, stop=(kt == 0))
                    yield
                P.copy("act", self.yT[2][r, hp, q0:q0 + 512], accb[r, :])
                yield

            todo = sorted([(h, qb) for h in range(4) for qb in range(4)], key=lambda t: -t[1])
            active = {}
            while todo or active:
                for sl in range(NS):
                    if sl not in active and todo:
                        h, qb = todo.pop(0)
                        active[sl] = stream(h, qb, sl)
                for sl in list(active):
                    try:
                        next(active[sl])
                    except StopIteration:
                        del active[sl]

    def u_moba(self, l, s):
        P = self.P
        pb = self.pb
        self._pj = 0
        ident = self.c("ident")
        with P.scope():
            wt = P.sb("bw", [128, 8, 768], BF16)
            q32 = P.sb("bq32", [128, 2, T], F32)
            qT = P.sb("bq", [128, 2, T], BF16)
            kT = P.sb("bk", [128, 2, T], BF16)
            vv = P.sb("bv", [128, 4, 16, 128], BF16)
            ksum = P.sb("bks", [128, 2, 8], F32)
            gm = P.sb("bgm", [128, 64, 8], F32)
            thr = P.sb("bthr", [128, 64, 8], F32)
            sel = P.sb("bsel", [128, 512], F32)
            aug = P.sb("baug", [128, T], BF16)
            NS = 4
            Pt = [P.sb("bP%d" % i, [128, 512], BF16) for i in range(NS)]
            rden = [P.sb("brd%d" % i, [128, 512], F32) for i in range(NS)]
            P.memset("pool", vv[:, :, :, :], 1.0)
            P.dma("sp", wt[:, :, :], self.wb_in[l, :, OFF_MOBA:OFF_MOBA + 768].rearrange("(k p) c -> p k c", p=128))
            P.copy("pool", aug[32:34, :].r("p (a t) -> p a t", a=4), self.c("augrows")[32:34, :].un(1).bc([2, 4, 512]))
            for hp in range(2):
                def wq(c, ps, hp=hp):
                    P.ts("dve", q32[:, hp, c * 512:(c + 1) * 512], ps, 0.125, ALU.mult)
                    P.copy("pool", qT[:, hp, c * 512:(c + 1) * 512], q32[:, hp, c * 512:(c + 1) * 512])
                self._proj_fm(wt, hp * 128, wq)

                def wk(c, ps, hp=hp):
                    P.copy("act", kT[:, hp, c * 512:(c + 1) * 512], ps)
                    P.reduce("dve", ksum[:, hp, 2 * c:2 * c + 2], ps.r("p (n t) -> p n t", n=2))
                self._proj_fm(wt, 256 + hp * 128, wk)
            for i in range(16):
                bank = pb[4 + i % 2]
                self._proj_tm(wt, 512, 256, i, bank)
                v4 = bank[:, 0:256].r("p (h d) -> p h d", h=4)
                P.copy("act", vv[:, 0:4:2, i, 0:64], v4[:, 0:4:2, :])
                P.copy("dve", vv[:, 1:4:2, i, 64:128], v4[:, 1:4:2, :])
            if "moba_stop1" in self.dbg:
                return
            for hh in range(2):
                gb = pb[6 + hh]
                r = slice(hh * 64, hh * 64 + 64)
                for i in range(16):
                    for hp in range(2):
                        o = (i * 2 + hp) * 8
                        P.mm(gb[:, o:o + 8], q32[r, hp, i * 128:(i + 1) * 128], ksum[r, hp, :])
                P.tt("dve", gm[:, :, :].r("p (i hp hh) n -> p i hp hh n", hp=2, hh=2)[:, :, :, hh, :],
                     gb[:, 0:256].r("p (i hp n) -> p i hp n", hp=2, n=8),
                     self.c("pastbias").r("p (i hp hh n) -> p i hp hh n", hp=2, hh=2, n=8)[:, :, :, hh, :], ALU.add)
            for a in range(64):
                P.max8(thr[:, a, :], gm[:, a, :])
            P.tt("dve", sel[:, :].r("p (a n) -> p a n", n=8), gm[:, :, :], thr[:, :, 2:3].bc([128, 64, 8]), ALU.is_ge)
            P.tt("dve", sel[:, :], sel[:, :], self.c("pastm"), ALU.mult)
            P.tt("dve", sel[:, :], sel[:, :], self.c("ownm"), ALU.add)
            P.ts("dve", sel[:, :], sel[:, :], -1.0, ALU.add, 1000.0, ALU.mult)
            if "moba_stop2" in self.dbg:
                return
            for g4 in range(4):
                tb_ = pb[g4 % 2]
                for j in range(4):
                    i = g4 * 4 + j
                    P.tr(tb_[0:32, j * 128:(j + 1) * 128], sel[:, i * 32:(i + 1) * 32], ident)
                P.copy("act", aug[0:32, g4 * 512:(g4 + 1) * 512], tb_[0:32, :])
            alibi = self.c("alibi")

            def stream(h, qb, sl):
                hp, r = h // 2, slice((h % 2) * 64, (h % 2) * 64 + 64)
                ro = slice(64 - (h % 2) * 64, 128 - (h % 2) * 64)
                q0 = qb * 512
                sb_, accb = pb[sl], pb[4 + sl]
                Ps = Pt[sl]
                nk = 4 * qb + 4
                for kt in range(nk):
                    c0 = max(0, kt - 4 * qb) * 128
                    n = kt // 2
                    P.mm(sb_[:, c0:512], kT[r, hp, kt * 128:(kt + 1) * 128], qT[r, hp, q0 + c0:q0 + 512], start=True, stop=False)
                    P.mm(sb_[:, c0:512], self.sel_bf[0:34, h * 8 + n, :], aug[0:34, q0 + c0:q0 + 512], start=False, stop=True)
                    yield
                    dl = kt - 4 * qb + 12
                    P.act(Ps[:, c0:512], sb_[:, c0:512], AF.Exp, bias=alibi[:, h * 16 + dl:h * 16 + dl + 1])
                    yield
                    if kt >= 4 * qb:
                        P.tt("pool", Ps[:, c0:c0 + 128], Ps[:, c0:c0 + 128], self.c("m_incl"), ALU.mult)
                        if c0 > 0:
                            P.memset("pool", Ps[:, 0:c0], 0.0)
                        yield
                    P.mm(accb[:, :], vv[:, h, kt, :], Ps[:, :], start=(kt == 0), stop=(kt == nk - 1))
                    yield
                P.recip(rden[sl][r, :], accb[ro, :])
                yield
                P.tt("dve", self.yT[1][r, hp, q0:q0 + 512], accb[r, :], rden[sl][r, :], ALU.mult)
                yield

            todo = sorted([(h, qb) for h in range(0 if "moba_noattn" not in self.dbg else 4, 4) for qb in range(4)], key=lambda t: -t[1])
            active = {}
            while todo or active:
                for sl in range(NS):
                    if sl not in active and todo:
                        h, qb = todo.pop(0)
                        active[sl] = stream(h, qb, sl)
                for sl in list(active):
                    try:
                        next(active[sl])
                    except StopIteration:
                        del active[sl]

    def _conv_chunk(self, wt, col0, m, c, pre, acc, cw, out_view, bias, eng):
        P = self.P
        bank = self.pb[self._pj % 4]
        self._pj += 1
        for k in range(8):
            P.mm(bank[:, :], wt[:, k, col0:col0 + 128], self.hT[:, k, c * 512:(c + 1) * 512], start=(k == 0), stop=(k == 7))
        if c == 0:
            P.memset(eng, pre[:, m, 0:3], 0.0)
        else:
            P.copy(eng, pre[:, m, 0:3], pre[:, m, 512:515])
        P.copy("act", pre[:, m, 3:515], bank[:, :])
        P.ts(eng, acc[:, :], pre[:, m, 3:515], cw[:, m * 4 + 3:m * 4 + 4], ALU.mult)
        for j in range(1, 4):
            P.stt("dve", acc[:, :], pre[:, m, 3 - j:515 - j], cw[:, m * 4 + 3 - j:m * 4 + 4 - j], acc[:, :], ALU.mult, ALU.add)
        if bias is None:
            P.act(out_view, acc[:, :], AF.Silu)
        else:
            P.act(out_view, acc[:, :], AF.Silu, bias=bias)

    def u_ssd(self, l, s):
        P = self.P
        pb = self.pb
        self._pj = 0
        ident = self.c("ident")
        ones = self.c("ones")
        with P.scope():
            wt = P.sb("dw", [128, 8, 1028], BF16)
            pre = P.sb("dpre", [128, 6, 515], F32)
            accs = [P.sb("dacc%d" % i, [128, 512], F32) for i in range(2)]
            xbc = [P.sb("dxbc%d" % i, [128, 6, 512], F32) for i in range(2)]
            zs = P.sb("dzs", [128, 4, 256], F32)
            dt = P.sb("ddt", [128, 16, 4], F32)
            gg = P.sb("dgg", [128, 16, 4], F32)
            aneg = P.sb("daneg", [128, 4], F32)
            tok = [P.sb("dtok%d" % i, [128, 512], F32) for i in range(2)]
            gtri = P.sb("dgtri", [128, 4, 128], F32)
            Dm = P.sb("dD", [128, 4, 128], F32)
            lm = P.sb("dlm", [128, 4, 128], F32)
            Mm = P.sb("dM", [128, 4, 128], F32)
            sm = P.sb("dsm", [128, 32], F32)
            xw = P.sb("dxw", [128, 256], F32)
            hst = P.sb("dhst", [128, 256], F32)
            yb = [P.sb("dy%d" % i, [128, 256], F32) for i in range(2)]
            junk = P.sb("djunk", [128, 256], F32)
            P.dma("sp", wt[:, :, :], self.wb_in[l, :, OFF_SSD:OFF_SSD + 1028].rearrange("(k p) c -> p k c", p=128))
            cw, cb = self.lpv("ssd_conv"), self.lpv("ssd_cb")
            dtb = pb[7]
            for i in range(16):
                for k in range(8):
                    P.mm(dtb[:, i * 4:(i + 1) * 4], self.hT[:, k, i * 128:(i + 1) * 128], wt[:, k, 1024:1028], start=(k == 0), stop=(k == 7))
            P.tt("dve", dt[:, :, :], dtb[:, 0:64].r("p (i h) -> p i h", h=4), self.lpv("ssd_dtb").un(1).bc([128, 16, 4]), ALU.add)
            P.act(dt[:, :, :], dt[:, :, :], AF.Exp)
            P.act(dt[:, :, :], dt[:, :, :], AF.Ln, bias=1.0)
            P.act(aneg[:, :], self.lpv("ssd_alog"), AF.Exp)
            P.stt("dve", gg[:, :, :], dt[:, :, :], -1.0, aneg[:, :].un(1).bc([128, 16, 4]), ALU.mult, ALU.mult)
            P.memset("pool", hst[:, :], 0.0)
            for c in range(4):
                xb = xbc[c % 2]
                for m in range(6):
                    self._conv_chunk(wt, 256 + m * 128, m, c, pre, accs[m % 2], cw, xb[:, m, :], cb[:, m:m + 1],
                                     "dve" if m % 2 == 0 else "pool")
                for j in range(4):
                    i = c * 4 + j
                    bank = pb[4 + j % 2]
                    self._proj_tm(wt, 0, 256, i, bank)
                    P.act(zs[:, j, :], bank[:, 0:256], AF.Silu)
                for j in range(4):
                    i = c * 4 + j
                    cs = slice(j * 128, (j + 1) * 128)
                    tk = tok[i % 2]
                    tb_ = pb[4]
                    for m in range(4):
                        P.tr(tb_[:, m * 128:(m + 1) * 128], xb[:, m, cs], ident)
                    P.copy("act", tk[:, :], tb_[:, :])
                    sb_ = pb[5]
                    P.mm(sb_[:, 0:4], self.c("tri128"), gg[:, i, :])
                    P.copy("dve", sm[:, 0:4], sb_[:, 0:4])
                    P.tt("pool", gtri[:, :, :], self.c("tri128").un(1).bc([128, 4, 128]), gg[:, i, :].un(2).bc([128, 4, 128]), ALU.mult)
                    ab = pb[6]
                    P.mm(ab[:, :], ones, gtri[:, :, :].r("p h l -> p (h l)"))
                    P.copy("dve", sm[:, 4:8], ab[:, :].r("p (h l) -> p h l", h=4)[:, :, 127])
                    P.tt("dve", Dm[:, :, :], ab[:, :].r("p (h l) -> p h l", h=4), sm[:, 0:4].un(2).bc([128, 4, 128]), ALU.subtract)
                    P.tt("pool", Dm[:, :, :], Dm[:, :, :], self.c("neg_ssd").un(1).bc([128, 4, 128]), ALU.add)
                    P.act(lm[:, :, :], Dm[:, :, :], AF.Exp)
                    P.act(sm[:, 8:12], sm[:, 0:4], AF.Exp)
                    P.act(sm[:, 12:16], sm[:, 4:8], AF.Exp)
                    P.tt("dve", sm[:, 16:20], sm[:, 4:8], sm[:, 0:4], ALU.subtract)
                    P.act(sm[:, 16:20], sm[:, 16:20], AF.Exp)
                    P.tt("dve", sm[:, 16:20], sm[:, 16:20], dt[:, i, :], ALU.mult)
                    scb = pb[0]
                    for g in range(2):
                        P.mm(scb[:, g * 128:(g + 1) * 128], xb[:, 2 + g, cs], xb[:, 4 + g, cs])
                    for h in range(4):
                        P.stt("dve", Mm[:, h, :], lm[:, h, :], dt[:, i, h:h + 1], scb[:, (h // 2) * 128:(h // 2 + 1) * 128], ALU.mult, ALU.mult)
                    yb_ = pb[1]
                    for h in range(4):
                        P.mm(yb_[:, h * 64:(h + 1) * 64], Mm[:, h, :], tk[:, h * 64:(h + 1) * 64])
                    for h in range(4):
                        P.mm(yb_[:, 256 + h * 64:256 + (h + 1) * 64], xb[:, 4 + h // 2, cs], hst[:, h * 64:(h + 1) * 64])
                    y = yb[i % 2]
                    P.tt("dve", y[:, :].r("p (h e) -> p h e", h=4), yb_[:, 256:512].r("p (h e) -> p h e", h=4),
                         sm[:, 8:12].un(2).bc([128, 4, 64]), ALU.mult)
                    P.tt("dve", y[:, :], y[:, :], yb_[:, 0:256], ALU.add)
                    P.tt("pool", xw[:, :].r("p (h e) -> p h e", h=4), tk[:, 0:256].r("p (h e) -> p h e", h=4),
                         self.lpv("ssd_d").un(2).bc([128, 4, 64]), ALU.mult)
                    P.tt("pool", y[:, :], y[:, :], xw[:, :], ALU.add)
                    P.tt("pool", xw[:, :].r("p (h e) -> p h e", h=4), tk[:, 0:256].r("p (h e) -> p h e", h=4),
                         sm[:, 16:20].un(2).bc([128, 4, 64]), ALU.mult)
                    hb = pb[2]
                    for g in range(2):
                        P.mm(hb[:, g * 128:(g + 1) * 128], tk[:, 256 + g * 128:256 + (g + 1) * 128], xw[:, g * 128:(g + 1) * 128])
                    P.tt("pool", hst[:, :].r("p (h e) -> p h e", h=4), hst[:, :].r("p (h e) -> p h e", h=4),
                         sm[:, 12:16].un(2).bc([128, 4, 64]), ALU.mult)
                    P.tt("dve", hst[:, :], hst[:, :], hb[:, 0:256], ALU.add)
                    P.tt("pool", y[:, :], y[:, :], zs[:, j, :], ALU.mult)
                    P.act(junk[:, :], y[:, :], AF.Square, accum=sm[:, 20:21])
                    P.act(sm[:, 21:22], sm[:, 20:21], AF.Ln, bias=EPS, scale=1.0 / 256)
                    P.act(sm[:, 21:22], sm[:, 21:22], AF.Exp, scale=-0.5)
                    P.stt("dve", y[:, :], y[:, :], sm[:, 21:22], self.lpv("ssd_norm"), ALU.mult, ALU.mult)
                    ob = pb[3]
                    for k in range(2):
                        P.tr(ob[:, k * 128:(k + 1) * 128], y[:, k * 128:(k + 1) * 128], ident)
                    P.copy("act", self.yT[3][:, :, i * 128:(i + 1) * 128], ob[:, 0:256].r("p (k t) -> p k t", k=2))

    def u_gdn(self, l, s):
        P = self.P
        pb = self.pb
        self._pj = 0
        ident = self.c("ident")
        ones = self.c("ones")
        with P.scope():
            wt = P.sb("gw", [128, 8, 1032], BF16)
            pre = P.sb("gpre", [128, 6, 515], F32)
            accs = [P.sb("gacc%d" % i, [128, 512], F32) for i in range(2)]
            qkv = [P.sb("gqkv%d" % i, [128, 6, 512], F32) for i in range(2)]
            sq = P.sb("gsq", [128, 512], F32)
            rsn = P.sb("grsn", [128, 512], F32)
            bg = P.sb("gbg", [128, 16, 8], F32)
            beta = P.sb("gbeta", [128, 16, 4], F32)
            lnb = P.sb("glnb", [128, 16, 4], F32)
            gg = P.sb("ggg", [128, 16, 4], F32)
            aneg = P.sb("ganeg", [128, 4], F32)
            kv = [P.sb("gkv%d" % i, [128, 512], F32) for i in range(2)]
            r1 = P.sb("gr1", [128, 4, 128], F32)
            r2 = P.sb("gr2", [128, 4, 128], F32)
            X2 = P.sb("gX2", [128, 4, 128], F32)
            X3 = P.sb("gX3", [128, 4, 128], F32)
            egB = P.sb("gegB", [128, 4, 128], F32)
            smps = [P.sb("gsmp%d" % i, [128, 20], F32) for i in range(2)]
            sms = P.sb("gsms", [128, 8], F32)
            zsb = [P.sb("gzs%d" % i, [128, 4, 256], F32) for i in range(2)]
            qd = [P.sb("gqd%d" % i, [128, 2, 128], F32) for i in range(2)]
            kdec = [P.sb("gkd%d" % i, [128, 256], F32) for i in range(2)]
            kbg = P.sb("gkbg", [128, 256], F32)
            vb = P.sb("gvb", [128, 256], F32)
            NT = [P.sb("gNT%d" % h, [128, 128], F32) for h in range(4)]
            Nn = [P.sb("gN%d" % h, [128, 128], F32) for h in range(4)]
            Ma = [P.sb("gMa%d" % h, [128, 128], F32) for h in range(4)]
            MTa = [P.sb("gMTa%d" % h, [128, 128], F32) for h in range(4)]
            PT = [P.sb("gPT%d" % h, [128, 128], F32) for h in range(4)]
            Aq = [[P.sb("gAq%d_%d" % (b, h), [128, 128], F32) for h in range(4)] for b in range(2)]
            us = [P.sb("gu%d" % i, [128, 4, 64], F32) for i in range(2)]
            wTs = [P.sb("gwT%d" % i, [128, 2, 128], F32) for i in range(2)]
            gts = [P.sb("ggt%d" % i, [128, 4, 2], F32) for i in range(2)]
            S = P.sb("gS", [128, 2, 64], F32)
            vnew = P.sb("gvn", [128, 4, 64], F32)
            ot = P.sb("got", [128, 4, 64], F32)
            on = P.sb("gon", [128, 256], F32)
            P.dma("sp", wt[:, :, :], self.wb_in[l, :, OFF_GDN:OFF_GDN + 1032].rearrange("(k p) c -> p k c", p=128))
            cw = self.lpv("gdn_conv")
            bgb = pb[7]
            for i in range(16):
                for k in range(8):
                    P.mm(bgb[:, i * 8:(i + 1) * 8], self.hT[:, k, i * 128:(i + 1) * 128], wt[:, k, 1024:1032], start=(k == 0), stop=(k == 7))
            P.copy("dve", bg[:, :, :], bgb[:, 0:128].r("p (i c) -> p i c", c=8))
            P.act(lnb[:, :, :], bg[:, :, 0:4], AF.Exp, scale=-1.0)
            P.act(lnb[:, :, :], lnb[:, :, :], AF.Ln, bias=1.0)
            P.ts("dve", lnb[:, :, :], lnb[:, :, :], -1.0, ALU.mult)
            P.act(beta[:, :, :], lnb[:, :, :], AF.Exp)
            P.tt("dve", gg[:, :, :], bg[:, :, 4:8], self.lpv("gdn_dtb").un(1).bc([128, 16, 4]), ALU.add)
            P.act(gg[:, :, :], gg[:, :, :], AF.Exp)
            P.act(gg[:, :, :], gg[:, :, :], AF.Ln, bias=1.0)
            P.act(aneg[:, :], self.lpv("gdn_alog"), AF.Exp)
            P.stt("dve", gg[:, :, :], gg[:, :, :], -1.0, aneg[:, :].un(1).bc([128, 16, 4]), ALU.mult, ALU.mult)
            P.memset("pool", S[:, :, :], 0.0)

            def prep_common(i, qk):
                j = i % 4
                cs = slice(j * 128, (j + 1) * 128)
                b = i % 2
                kvt = kv[b]
                smp = smps[b]
                tb_ = pb[4]
                for m in range(4):
                    P.tr(tb_[:, m * 128:(m + 1) * 128], qk[:, 2 + m, cs], ident)
                P.copy("act", kvt[:, :], tb_[:, :])
                sb_ = pb[3]
                P.mm(sb_[:, 0:4], self.c("tri_bd"), gg[:, i, :])
                P.copy("dve", smp[:, 0:4], sb_[:, 0:4])
                P.tt("pool", r1[:, :, :], self.c("tri_bd").un(1).bc([128, 4, 128]), gg[:, i, :].un(2).bc([128, 4, 128]), ALU.mult)
                P.tt("pool", r2[:, :, :], self.c("ident").un(1).bc([128, 4, 128]), lnb[:, i, :].un(2).bc([128, 4, 128]), ALU.mult)
                P.tt("pool", r2[:, :, :], r2[:, :, :], r1[:, :, :], ALU.add)
                gcB, gcbB = pb[0], pb[1]
                P.mm(gcB[:, :], ones, r1[:, :, :].r("p h c -> p (h c)"))
                P.mm(gcbB[:, :], ones, r2[:, :, :].r("p h c -> p (h c)"))
                gc_b = smp[:, 0:4].un(2).bc([128, 4, 128])
                P.tt("dve", X2[:, :, :], gcB[:, :].r("p (h c) -> p h c", h=4), gc_b, ALU.subtract)
                P.tt("pool", X2[:, :, :], X2[:, :, :], self.c("neg_incl_bd").un(1).bc([128, 4, 128]), ALU.add)
                P.act(X2[:, :, :], X2[:, :, :], AF.Exp)
                P.tt("dve", X3[:, :, :], gcbB[:, :].r("p (h c) -> p h c", h=4), gc_b, ALU.subtract)
                P.tt("pool", X3[:, :, :], X3[:, :, :], self.c("neg_strict_bd").un(1).bc([128, 4, 128]), ALU.add)
                P.act(X3[:, :, :], X3[:, :, :], AF.Exp)
                P.act(egB[:, :, :], gcB[:, :].r("p (h c) -> p h c", h=4), AF.Exp)
                gt = gts[b]
                P.act(gt[:, :, :], gcB[:, :].r("p (h c) -> p h c", h=4)[:, :, 63:128:64], AF.Exp)
                P.copy("dve", smp[0:64, 4:8], gcB[0:64, :].r("p (h c) -> p h c", h=4)[:, :, 63])
                P.copy("dve", smp[64:128, 4:8], gcB[64:128, :].r("p (h c) -> p h c", h=4)[:, :, 127])
                P.act(smp[:, 8:12], smp[:, 0:4], AF.Exp)
                P.tt("dve", smp[:, 12:16], smp[:, 4:8], smp[:, 0:4], ALU.subtract)
                P.act(smp[:, 12:16], smp[:, 12:16], AF.Exp)
                P.tt("dve", smp[:, 16:20], smp[:, 8:12], beta[:, i, :], ALU.mult)
                kd = kdec[b]
                k4 = kvt[:, 0:256].r("p (h d) -> p h d", h=4)
                P.tt("pool", kd[:, :].r("p (h d) -> p h d", h=4), k4, smp[:, 12:16].un(2).bc([128, 4, 64]), ALU.mult)
                P.tt("pool", kbg[:, :].r("p (h d) -> p h d", h=4), k4, smp[:, 16:20].un(2).bc([128, 4, 64]), ALU.mult)
                P.tt("pool", vb[:, :].r("p (h d) -> p h d", h=4), kvt[:, 256:512].r("p (h d) -> p h d", h=4),
                     beta[:, i, :].un(2).bc([128, 4, 64]), ALU.mult)
                qdt = qd[b]
                for hp in range(2):
                    for hh in range(2):
                        r = slice(hh * 64, hh * 64 + 64)
                        P.tt("pool", qdt[r, hp, :], qk[r, hp, cs], egB[r, hp * 2 + hh, :], ALU.mult)

            def head_chain(i, qk, h):
                j = i % 4
                cs = slice(j * 128, (j + 1) * 128)
                b = i % 2
                hp, r = h // 2, slice((h % 2) * 64, (h % 2) * 64 + 64)
                hb = pb[h]
                P.mm(hb[:, 0:128], qk[r, 2 + hp, cs], qk[r, 2 + hp, cs])
                P.mm(hb[:, 128:256], qk[r, 2 + hp, cs], qk[r, hp, cs])
                yield
                P.stt("dve", NT[h][:, :], hb[:, 0:128], -1.0, X3[:, h, :], ALU.mult, ALU.mult)
                P.tt("dve", Aq[b][h][:, :], hb[:, 128:256], X2[:, h, :], ALU.mult)
                yield
                P.tr(hb[:, 256:384], NT[h][:, :], ident)
                P.tt("pool", PT[h][:, :], NT[h][:, :], ident, ALU.add)
                yield
                P.copy("act", Nn[h][:, :], hb[:, 256:384])
                yield
                M, MT = Nn[h], NT[h]
                for lvl in range(5):
                    P.mm(hb[:, 0:128], MT[:, :], M[:, :])
                    if lvl < 4:
                        P.mm(hb[:, 128:256], M[:, :], MT[:, :])
                    yield
                    M2 = Ma[h] if lvl % 2 == 0 else Nn[h]
                    MT2 = MTa[h] if lvl % 2 == 0 else NT[h]
                    P.copy("act", M2[:, :], hb[:, 0:128])
                    if lvl < 4:
                        P.copy("dve", MT2[:, :], hb[:, 128:256])
                    yield
                    P.mm(hb[:, 256:384], M2[:, :], PT[h][:, :])
                    yield
                    P.tt("dve", PT[h][:, :], PT[h][:, :], hb[:, 256:384], ALU.add)
                    yield
                    M, MT = M2, MT2
                P.mm(hb[:, 0:64], PT[h][:, :], vb[:, h * 64:(h + 1) * 64])
                P.mm(hb[:, 128:256], kbg[:, hp * 128:(hp + 1) * 128], PT[h][:, :])
                yield
                P.copy("act", us[b][:, h, :], hb[:, 0:64])
                P.copy("dve", wTs[b][r, hp, :], hb[r, 128:256])
                yield

            def scan(i):
                b = i % 2
                j4 = i % 4
                for jc in range(2):
                    rj = slice(jc * 64, jc * 64 + 64)
                    wsb, ob, snb = pb[5], pb[6], pb[7]
                    for hh in range(2):
                        for h in (hh, hh + 2):
                            hp, r = h // 2, slice((h % 2) * 64, (h % 2) * 64 + 64)
                            P.mm(wsb[:, h * 64:(h + 1) * 64], wTs[b][r, hp, :], S[r, hp, :])
                    yield
                    P.tt("dve", vnew[rj, :, :], us[b][rj, :, :], wsb[rj, 0:256].r("p (h e) -> p h e", h=4), ALU.subtract)
                    yield
                    for h in range(4):
                        hp, r = h // 2, slice((h % 2) * 64, (h % 2) * 64 + 64)
                        P.mm(ob[:, h * 64:(h + 1) * 64], qd[b][r, hp, :], S[r, hp, :], start=True, stop=False)
                        P.mm(ob[:, h * 64:(h + 1) * 64], Aq[b][h][rj, :], vnew[rj, h, :], start=False, stop=True)
                    for h in range(4):
                        hp = h // 2
                        P.mm(snb[:, h * 64:(h + 1) * 64], kdec[b][rj, hp * 128:(hp + 1) * 128], vnew[rj, h, :])
                    yield
                    for h in range(4):
                        hp, r = h // 2, slice((h % 2) * 64, (h % 2) * 64 + 64)
                        P.stt("dve", S[r, hp, :], S[r, hp, :], gts[b][r, h, jc:jc + 1], snb[r, h * 64:(h + 1) * 64], ALU.mult, ALU.add)
                    P.copy("act", ot[rj, :, :], ob[rj, 0:256].r("p (h e) -> p h e", h=4))
                    yield
                P.tt("pool", on[:, :], ot[:, :, :].r("p h e -> p (h e)"), ot[:, :, :].r("p h e -> p (h e)"), ALU.mult)
                P.reduce("dve", sms[:, 0:4], on[:, :].r("p (h e) -> p h e", h=4))
                yield
                P.act(sms[:, 4:8], sms[:, 0:4], AF.Ln, bias=EPS, scale=1.0 / 64)
                P.act(sms[:, 4:8], sms[:, 4:8], AF.Exp, scale=-0.5)
                yield
                P.tt("pool", on[:, :].r("p (h e) -> p h e", h=4), ot[:, :, :], sms[:, 4:8].un(2).bc([128, 4, 64]), ALU.mult)
                P.tt("pool", on[:, :].r("p (h e) -> p h e", h=4), on[:, :].r("p (h e) -> p h e", h=4),
                     self.lpv("gdn_norm").un(1).bc([128, 4, 64]), ALU.mult)
                P.tt("pool", on[:, :], on[:, :], zsb[(i // 4) % 2][:, j4, :], ALU.mult)
                yield
                tb2 = pb[4]
                for k in range(2):
                    P.tr(tb2[:, 256 + k * 128:256 + (k + 1) * 128], on[:, k * 128:(k + 1) * 128], ident)
                P.copy("act", self.yT[0][:, :, i * 128:(i + 1) * 128], tb2[:, 256:512].r("p (k t) -> p k t", k=2))
                yield

            def chunk_front(c):
                qk = qkv[c % 2]
                for m in range(6):
                    self._conv_chunk(wt, m * 128, m, c, pre, accs[m % 2], cw, qk[:, m, :], None, "dve" if m % 2 == 0 else "pool")
                for m in range(4):
                    P.act(sq[:, :], qk[:, m, :], AF.Square)
                    nb = pb[m % 2]
                    P.mm(nb[:, :], self.c("blk64"), sq[:, :])
                    P.act(rsn[:, :], nb[:, :], AF.Ln, bias=EPS)
                    P.act(rsn[:, :], rsn[:, :], AF.Exp, scale=-0.5)
                    if m < 2:
                        P.stt("dve", qk[:, m, :], qk[:, m, :], 0.125, rsn[:, :], ALU.mult, ALU.mult)
                    else:
                        P.tt("pool", qk[:, m, :], qk[:, m, :], rsn[:, :], ALU.mult)
                for j in range(4):
                    i = c * 4 + j
                    bank = pb[4 + j % 2]
                    self._proj_tm(wt, 768, 256, i, bank)
                    P.act(zsb[c % 2][:, j, :], bank[:, 0:256], AF.Silu)

            def run_rr(gens):
                gens = list(gens)
                while gens:
                    for g in list(gens):
                        try:
                            next(g)
                        except StopIteration:
                            gens.remove(g)

            prev = None
            for c in range(4):
                chunk_front(c)
                for j in range(4):
                    i = c * 4 + j
                    prep_common(i, qkv[c % 2])
                    gens = [head_chain(i, qkv[c % 2], h) for h in range(4)]
                    if prev is not None:
                        gens.append(scan(prev))
                    run_rr(gens)
                    prev = i
            run_rr([scan(prev)])


def _prep_inputs(inputs, n_layers=2):
    f = lambda k: np.ascontiguousarray(np.asarray(inputs[k], dtype=np.float32)[:n_layers])
    shared = {"w_in": f("w_in"), "w_gate": f("w_gate"), "w_branch": f("w_branch"), "w_out": f("w_out"),
              "w_up": f("w_up"), "w_down": f("w_down"), "consts": CONST_ARR,
              "lp": np.stack([_layer_params(inputs, l) for l in range(n_layers)])}
    return shared


def kernel(**inputs):
    x = np.ascontiguousarray(np.asarray(inputs["x"], dtype=np.float32))
    n_cores = 8
    per = x.shape[0] // n_cores
    shared = _prep_inputs(inputs)
    nc = K(n_seq=per).build()
    in_maps = []
    for c in range(n_cores):
        m = dict(shared)
        m["x"] = x[c * per:(c + 1) * per].reshape(per * T, D)
        in_maps.append(m)
    res = run_bass_kernel_spmd(nc, in_maps, core_ids=list(range(n_cores)))
    out = np.stack([np.asarray(r["out"]).reshape(per, T, D) for r in res.results], axis=0)
    return out.reshape(x.shape).astype(np.float32)
```

```python
import numpy as np
from contextlib import ExitStack, contextmanager
import concourse.bass as bass
import concourse.mybir as mybir
from concourse.bass_utils import run_bass_kernel_spmd

F32 = mybir.dt.float32
BF16 = mybir.dt.bfloat16
AF = mybir.ActivationFunctionType
ALU = mybir.AluOpType
AX = mybir.AxisListType

ENGINES = ("pe", "act", "dve", "pool", "sp")


class Res:
    __slots__ = ("name", "last_w", "readers", "slot", "t", "excl", "pe_last")

    def __init__(self, name, t=None, excl=False):
        self.name = name
        self.excl = excl
        self.pe_last = None
        self.last_w = None
        self.readers = []
        self.slot = None
        self.t = t

    def __getitem__(self, idx):
        return V(self, self.t[idx])


class V:
    __slots__ = ("res", "ap")

    def __init__(self, res, ap):
        self.res = res
        self.ap = ap

    def __getitem__(self, idx):
        return V(self.res, self.ap[idx])

    def r(self, pat, **kw):
        return V(self.res, self.ap.rearrange(pat, **kw))

    def un(self, axis):
        return V(self.res, self.ap.unsqueeze(axis))

    def bc(self, shape):
        return V(self.res, self.ap.broadcast_to(list(shape)))


def _ap(x):
    return x.ap if isinstance(x, V) else x


def _rs(*xs):
    return [x.res for x in xs if isinstance(x, V)]


class Op:
    __slots__ = ("eng", "fn", "seq", "waits", "signal", "is_dma", "slot", "cnt",
                 "clock", "semval", "kind")

    def __init__(self, eng, fn, is_dma=False, slot=None):
        self.eng = eng
        self.fn = fn
        self.is_dma = is_dma
        self.slot = slot
        self.waits = []
        self.signal = False
        self.clock = None
        self.semval = None
        self.cnt = 0


class Prog:
    def __init__(self, nc):
        self.nc = nc
        self.ops = {e: [] for e in ENGINES}
        self.clock = {e: {} for e in ENGINES}
        self.nslots = 0
        self.slot_last = {}
        self.slot_cnt = {}
        self.all_res = []
        self.stack = None
        self.n_wait = 0
        self.free_slots = []

    def sb(self, name, shape, dtype):
        self.uid = getattr(self, "uid", 0) + 1
        name = "%s_%d" % (name, self.uid)
        t = self.stack.enter_context(self.nc.sbuf_tensor(name, list(shape), dtype))
        r = Res(name, t)
        self.all_res.append(r)
        return r

    def ps(self, name, shape, dtype=F32):
        t = self.stack.enter_context(self.nc.psum_tensor(name, list(shape), dtype))
        r = Res(name, t, excl=True)
        self.all_res.append(r)
        return r

    def res(self, name):
        r = Res(name, None)
        self.all_res.append(r)
        return r

    def _key(self, op):
        return ("s", op.slot) if op.is_dma else op.eng

    def _val(self, op):
        return op.cnt if op.is_dma else op.seq

    def _add_dep(self, op, dep, raw, force=False):
        if dep is None or dep is op:
            return
        if not force and not dep.is_dma and not op.is_dma and dep.eng == op.eng:
            if op.eng == "pe":
                return
        k = self._key(dep)
        v = self._val(dep)
        ck = self.clock[op.eng]
        if ck.get(k, -1) >= v:
            return
        op.waits.append(dep)
        dep.signal = True
        for kk, vv in dep.clock.items():
            if ck.get(kk, -1) < vv:
                ck[kk] = vv
        ck[k] = v

    def add(self, eng, fn, reads=(), writes=(), is_dma=False, force_dep=None):
        op = Op(eng, fn, is_dma=is_dma)
        xr = [r for r in reads if r.excl]
        if xr:
            reads = [r for r in reads if not r.excl]
            writes = list(writes) + [r for r in xr if r not in writes]
        lst = self.ops[eng]
        op.seq = len(lst)
        if is_dma:
            tile = None
            for r in list(writes) + list(reads):
                if r.t is not None:
                    tile = r
                    break
            assert tile is not None, "dma needs an sbuf tile resource"
            if tile.slot is None:
                if self.free_slots:
                    tile.slot = self.free_slots.pop()
                else:
                    tile.slot = self.nslots
                    self.nslots += 1
            op.slot = tile.slot
            op.cnt = self.slot_cnt.get(op.slot, 0) + 1
            self.slot_cnt[op.slot] = op.cnt
        cand = []
        for r in reads:
            if r.last_w is not None:
                cand.append((r.last_w, True))
        for w in writes:
            if w.last_w is not None:
                cand.append((w.last_w, True))
            for rd in w.readers:
                cand.append((rd, False))
        cand.sort(key=lambda t: -self._val(t[0]))
        for d, raw in cand:
            self._add_dep(op, d, raw)
        if force_dep is not None:
            self._add_dep(op, force_dep, True, force=True)
        for r in reads:
            r.readers.append(op)
        for w in writes:
            w.last_w = op
            w.readers = []
        ck = dict(self.clock[eng])
        if not is_dma:
            ck[eng] = op.seq
        else:
            ck[("s", op.slot)] = op.cnt
        op.clock = ck
        lst.append(op)
        return op

    def dma(self, q, out, in_, reads=(), writes=()):
        o, i = _ap(out), _ap(in_)
        return self.add(q, lambda e: e.dma_start(out=o, in_=i), list(reads) + _rs(in_), list(writes) + _rs(out), is_dma=True)

    def _pe_rowgroup_dep(self, out, stat):
        bp = stat.ap.base_partition()
        bp = bp() if callable(bp) else bp
        k = stat.ap.shape[0]
        groups = set(range(bp // 32, (bp + k + 31) // 32))
        prev = out.res.pe_last
        force = prev[0] if (prev is not None and prev[1].isdisjoint(groups)) else None
        return groups, force

    def mm(self, out, lhsT, rhs, start=True, stop=True):
        o, l, r = out.ap, lhsT.ap, rhs.ap
        groups, force = self._pe_rowgroup_dep(out, lhsT)
        op = self.add("pe", lambda e: e.matmul(o, lhsT=l, rhs=r, start=start, stop=stop), _rs(lhsT, rhs), _rs(out), force_dep=force)
        out.res.pe_last = (op, groups)
        return op

    def tr(self, out, in_, ident):
        o, i, d = out.ap, in_.ap, ident.ap
        groups, force = self._pe_rowgroup_dep(out, in_)
        op = self.add("pe", lambda e: e.transpose(out=o, in_=i, identity=d), _rs(in_, ident), _rs(out), force_dep=force)
        out.res.pe_last = (op, groups)
        return op

    def act(self, out, in_, func, bias=0.0, scale=1.0, accum=None):
        o, i, b, sc = out.ap, in_.ap, _ap(bias), _ap(scale)
        if accum is None:
            return self.add("act", lambda e: e.activation(out=o, in_=i, func=func, bias=b, scale=sc), _rs(in_, bias, scale), _rs(out))
        a = accum.ap
        return self.add("act", lambda e: e.activation(out=o, in_=i, func=func, bias=b, scale=sc, accum_out=a), _rs(in_, bias, scale), _rs(out, accum))

    def tt(self, eng, out, in0, in1, op):
        o, a, b = out.ap, in0.ap, in1.ap
        return self.add(eng, lambda e: e.tensor_tensor(out=o, in0=a, in1=b, op=op), _rs(in0, in1), _rs(out))

    def ts(self, eng, out, in0, s1, op0, s2=None, op1=None):
        o, a, x1, x2 = out.ap, in0.ap, _ap(s1), _ap(s2)
        if op1 is None:
            return self.add(eng, lambda e: e.tensor_scalar(out=o, in0=a, scalar1=x1, scalar2=None, op0=op0), _rs(in0, s1), _rs(out))
        return self.add(eng, lambda e: e.tensor_scalar(out=o, in0=a, scalar1=x1, scalar2=x2, op0=op0, op1=op1), _rs(in0, s1, s2), _rs(out))

    def stt(self, eng, out, in0, scalar, in1, op0, op1):
        o, a, sc, b = out.ap, in0.ap, _ap(scalar), in1.ap
        return self.add(eng, lambda e: e.scalar_tensor_tensor(out=o, in0=a, scalar=sc, in1=b, op0=op0, op1=op1), _rs(in0, scalar, in1), _rs(out))

    def copy(self, eng, out, in_):
        o, i = out.ap, in_.ap
        if eng == "act":
            return self.add("act", lambda e: e.copy(out=o, in_=i), _rs(in_), _rs(out))
        return self.add(eng, lambda e: e.tensor_copy(out=o, in_=i), _rs(in_), _rs(out))

    def memset(self, eng, out, val):
        o = out.ap
        return self.add(eng, lambda e: e.memset(o, val), [], _rs(out))

    def reduce(self, eng, out, in_, op=None):
        o, i = out.ap, in_.ap
        op = op or ALU.add
        return self.add(eng, lambda e: e.tensor_reduce(out=o, in_=i, axis=AX.X, op=op), _rs(in_), _rs(out))

    def max8(self, out, in_):
        o, i = out.ap, in_.ap
        return self.add("dve", lambda e: e.max(out=o, in_=i), _rs(in_), _rs(out))

    def recip(self, out, in_):
        o, i = out.ap, in_.ap
        return self.add("dve", lambda e: e.reciprocal(out=o, in_=i), _rs(in_), _rs(out))

    def barrier(self):
        lasts = []
        for e in ENGINES:
            for op in reversed(self.ops[e]):
                if not op.is_dma and op.fn is not None:
                    lasts.append(op)
                    break
        dmas = [op for e in ENGINES for op in self.ops[e] if op.is_dma and self.slot_cnt[op.slot] == op.cnt]
        for e in ENGINES:
            op = Op(e, None)
            op.seq = len(self.ops[e])
            for d in lasts:
                if d.eng != e:
                    self._add_dep(op, d, True)
            for d in dmas:
                self._add_dep(op, d, True)
            ck = dict(self.clock[e])
            op.clock = ck
            op.seq = -1
            self.ops[e].append(op)

    @contextmanager
    def scope(self):
        old = self.stack
        mark = len(self.all_res)
        with ExitStack() as st:
            self.stack = st
            yield
            self.barrier()
            for r in self.all_res[mark:]:
                if r.slot is not None:
                    self.free_slots.append(r.slot)
                    r.slot = None
            del self.all_res[mark:]
        self.stack = old

    def emit(self, final_wait_ops=()):
        nc = self.nc
        from contextlib import ExitStack
        with ExitStack() as st:
            esem = {e: st.enter_context(nc.semaphore("sem_" + e)) for e in ENGINES}
            ssem = {s: st.enter_context(nc.semaphore("dsem%d" % s)) for s in range(self.nslots)}
            for e in ENGINES:
                v = 0
                for op in self.ops[e]:
                    if op.fn is None or op.is_dma:
                        continue
                    if op.signal:
                        v += 1
                    op.semval = v
            block = st.enter_context(nc.Block())

            def run(e, eng):
                for op in self.ops[e]:
                    for d in op.waits:
                        if d.is_dma:
                            eng.wait_ge(ssem[d.slot], 16 * d.cnt)
                        else:
                            eng.wait_ge(esem[d.eng], d.semval)
                        self.n_wait += 1
                    if op.fn is None:
                        continue
                    ins = op.fn(eng)
                    if op.is_dma:
                        ins.then_inc(ssem[op.slot], 16)
                    elif op.signal:
                        ins.then_inc(esem[e], 1)

            @block.tensor
            def _(eng):
                run("pe", eng)

            @block.scalar
            def _(eng):
                run("act", eng)

            @block.vector
            def _(eng):
                run("dve", eng)

            @block.gpsimd
            def _(eng):
                run("pool", eng)

            @block.sync
            def _(eng):
                run("sp", eng)

D = 1024
T = 2048
NH = 4
HD = 64
BW = 256
DFF = 4096
OFF_GDN, OFF_MOBA, OFF_SB, OFF_SSD, INW = 0, 1032, 1800, 2568, 3596
EPS = 1e-6
NEG = -30000.0


def _const_table():
    p = np.arange(128)[:, None]
    f = np.arange(128)[None, :]
    c = {}
    c["ident"] = (p == f)
    c["ones"] = np.ones((128, 128))
    c["blk64"] = (p // 64 == f // 64)
    c["tri128"] = (p <= f)
    c["neg_ssd"] = np.where(f >= p, 0.0, NEG)
    same = (p // 64 == f // 64)
    c["tri_bd"] = same & (p <= f)
    c["neg_incl_bd"] = np.where(same & (f >= p), 0.0, NEG)
    c["neg_strict_bd"] = np.where(same & (f > p), 0.0, NEG)
    c["m_strict"] = (f > p)
    c["m_incl"] = (f >= p)
    c["u_incl"] = (p >= f)
    i = np.arange(16)[:, None, None]
    n = np.arange(8)[None, None, :]
    h = np.arange(4)[None, :, None]
    past = np.broadcast_to(n < i // 2, (16, 4, 8))
    own = np.broadcast_to(n == i // 2, (16, 4, 8))
    slopes = np.array([2.0 ** (-8.0 * (k + 1) / 4) for k in range(4)])
    dl = np.arange(16)[None, None, :] - 12
    c["alibi"] = (slopes[None, :, None] * (dl * 128 + np.arange(128)[:, None, None])).reshape(128, 64)
    v = np.zeros((128, 32))
    for r in range(32):
        v[r, r] = 1.0
    for hh in range(4):
        v[32, hh * 8:(hh + 1) * 8] = slopes[hh]
        v[33, hh * 8:(hh + 1) * 8] = slopes[hh]
    c["selv"] = v
    t = np.arange(512)
    a = np.zeros((128, 512))
    a[32] = -128.0 * (t // 128)
    a[33] = -(t % 128).astype(np.float64)
    c["augrows"] = a
    c["pastbias"] = np.broadcast_to(np.where(past, 0.0, -1e30).reshape(1, 512), (128, 512))
    c["pastm"] = np.broadcast_to(past.reshape(1, 512).astype(np.float64), (128, 512))
    c["ownm"] = np.broadcast_to(own.reshape(1, 512).astype(np.float64), (128, 512))
    offs = {}
    cols = []
    o = 0
    for k, val in c.items():
        val = np.asarray(val, dtype=np.float32)
        offs[k] = (o, val.shape[1])
        cols.append(val)
        o += val.shape[1]
    return np.ascontiguousarray(np.concatenate(cols, axis=1)), offs


CONST_ARR, CONST_OFF = _const_table()
NCONST = CONST_ARR.shape[1]
NCONST_RES = CONST_OFF["pastbias"][0]

LP_OFF = {}
_o = 0
for _k, _w in (("g_mix_post", 1024), ("g_ffn_post", 1024), ("gdn_conv", 24), ("ssd_conv", 24), ("ssd_cb", 6),
               ("gdn_alog", 4), ("gdn_dtb", 4), ("ssd_alog", 4), ("ssd_dtb", 4), ("ssd_d", 4),
               ("gdn_norm", 64), ("ssd_norm", 256), ("rowg", 16)):
    LP_OFF[_k] = (_o, _w)
    _o += _w
NLP = _o


def _layer_params(inp, l):
    lp = np.zeros((128, NLP), np.float32)

    def put(k, arr):
        o, w = LP_OFF[k]
        lp[:, o:o + w] = arr
    put("g_mix_post", np.broadcast_to(inp["norm_mix_post"][l][None, :], (128, 1024)))
    put("g_ffn_post", np.broadcast_to(inp["norm_ffn_post"][l][None, :], (128, 1024)))
    put("gdn_conv", inp["gdn_conv"][l].reshape(4, 6, 128).transpose(2, 1, 0).reshape(128, 24))
    put("ssd_conv", inp["ssd_conv"][l].reshape(4, 6, 128).transpose(2, 1, 0).reshape(128, 24))
    put("ssd_cb", inp["ssd_conv_bias"][l].reshape(6, 128).T)
    for k, src in (("gdn_alog", "gdn_a_log"), ("gdn_dtb", "gdn_dt_bias"), ("ssd_alog", "ssd_a_log"),
                   ("ssd_dtb", "ssd_dt_bias"), ("ssd_d", "ssd_d")):
        put(k, np.broadcast_to(inp[src][l][None, :], (128, 4)))
    put("gdn_norm", np.broadcast_to(inp["gdn_norm"][l][None, :], (128, 64)))
    put("ssd_norm", np.broadcast_to(inp["ssd_norm"][l][None, :], (128, 256)))
    rg = np.concatenate([inp["norm_mix_pre"][l].reshape(8, 128).T, inp["norm_ffn_pre"][l].reshape(8, 128).T], axis=1)
    put("rowg", rg)
    return lp


class K:
    def __init__(self, n_seq=4, n_layers=2, units=("gdn", "moba", "sb", "ssd", "merge"), dbg=None):
        self.n_seq, self.n_layers, self.units, self.dbg = n_seq, n_layers, units, (dbg or {})
        nc = bass.Bass("TRN2", target_bir_lowering=False)
        self.nc = nc
        self.P = Prog(nc)
        NL = n_layers
        di = lambda name, shape, dt=F32: nc.dram_tensor(name, list(shape), dt, kind="ExternalInput").ap()
        dn = lambda name, shape, dt=BF16: nc.dram_tensor(name, list(shape), dt).ap()
        self.x = di("x", [n_seq * T, D])
        self.w_in = di("w_in", [NL, D, INW])
        self.w_gate = di("w_gate", [NL, 4, D, D])
        self.w_branch = di("w_branch", [NL, 4, BW, D])
        self.w_out = di("w_out", [NL, D, D])
        self.w_up = di("w_up", [NL, D, DFF])
        self.w_down = di("w_down", [NL, DFF, D])
        self.consts = di("consts", [128, NCONST])
        self.lpd = di("lp", [NL, 128, NLP])
        self.out = nc.dram_tensor("out", [n_seq * T, D], F32, kind="ExternalOutput").ap()
        self.xmid = dn("xmid", [n_seq * T, D], F32)
        self.wb_in = dn("wb_in", [NL, D, INW])
        self.wb_gate = dn("wb_gate", [NL, 8, 128, 4, 8, 128])
        self.wb_br = dn("wb_br", [NL, 8, 128, 4, 2, 128])
        self.wb_out = dn("wb_out", [NL, D, D])
        self.wb_up = dn("wb_up", [NL, 32, 128, 8, 128])
        self.wb_down = dn("wb_down", [NL, DFF, D])
        if "inject_yT" in self.dbg:
            self.dbg_yT = di("dbg_yT", [4, BW, T])
        if "dump_yT" in self.dbg:
            self.dbg_yT_out = nc.dram_tensor("dbg_yT_out", [4, BW, T], F32, kind="ExternalOutput").ap()
        if "dump_hT" in self.dbg:
            self.dbg_hT_out = nc.dram_tensor("dbg_hT_out", [D, T], F32, kind="ExternalOutput").ap()
        self._q = 0

    def q(self):
        return "sp"

    def c(self, name):
        o, w = CONST_OFF[name]
        if o >= NCONST_RES:
            return self.mcst[:, o - NCONST_RES:o - NCONST_RES + w]
        return self.cst[:, o:o + w]

    def lpv(self, name):
        o, w = LP_OFF[name]
        return self.lp[:, o:o + w]

    def build(self):
        P = self.P
        with ExitStack() as st:
            P.stack = st
            self.pb = [P.ps("pb%d" % i, [128, 512], F32) for i in range(8)]
            self.cst = P.sb("cst", [128, NCONST_RES], F32)
            self.lp = P.sb("lp_sb", [128, NLP], F32)
            self.ones_bf = P.sb("ones_bf", [128, 128], BF16)
            self.uincl_bf = P.sb("uincl_bf", [128, 128], BF16)
            self.sel_bf = P.sb("sel_bf", [128, 32, 128], BF16)
            self.hT = P.sb("hT", [128, 8, T], BF16)
            self.yT = [P.sb("yT%d" % g, [128, 2, T], BF16) for g in range(4)]
            P.dma("sp", self.cst[:, :], self.consts[:, 0:NCONST_RES])
            P.copy("dve", self.ones_bf[:, :], self.c("ones"))
            P.copy("dve", self.uincl_bf[:, :], self.c("u_incl"))
            P.copy("pool", self.sel_bf[0:34, :, :], self.c("selv")[0:34, :].un(2).bc([34, 32, 128]))
            if "dump_yT" in self.dbg:
                for g in range(4):
                    P.memset("pool", self.yT[g][:, :, :], 0.0)
            for l in range(self.n_layers):
                P.dma("sp", self.lp[:, :], self.lpd[l, :, :])
                if "noconvert" not in self.dbg:
                    self.convert_weights(l)
                xsrc = self.x if l == 0 else self.xmid
                xdst = self.out if l == self.n_layers - 1 else self.xmid
                for s in range(self.n_seq):
                    self.u_norm(l, s, xsrc)
                    if "dump_hT" in self.dbg:
                        self.dump_hT()
                    if "inject_yT" in self.dbg:
                        self.inject_yT()
                    if "sb" in self.units:
                        self.u_sb(l, s)
                    if "moba" in self.units:
                        self.u_moba(l, s)
                    if "ssd" in self.units:
                        self.u_ssd(l, s)
                    if "gdn" in self.units:
                        self.u_gdn(l, s)
                    if "dump_yT" in self.dbg:
                        self.dump_yT()
                    if "merge" in self.units:
                        self.u_merge(l, s, xsrc, xdst)
            P.barrier()
            P.emit()
        return self.nc

    def dump_hT(self):
        P = self.P
        with P.scope():
            t = P.sb("dh", [128, 8, T], F32)
            P.copy("dve", t[:, :, :], self.hT[:, :, :])
            P.dma("sp", self.dbg_hT_out.rearrange("(k p) t -> p k t", p=128), t[:, :, :])

    def dump_yT(self):
        P = self.P
        with P.scope():
            for g in range(4):
                t = P.sb("dy%d" % g, [128, 2, T], F32)
                P.copy("dve", t[:, :, :], self.yT[g][:, :, :])
                P.dma("sp", self.dbg_yT_out[g].rearrange("(k p) t -> p k t", p=128), t[:, :, :])

    def inject_yT(self):
        P = self.P
        with P.scope():
            for g in range(4):
                t = P.sb("iy%d" % g, [128, 2, T], F32)
                P.dma("sp", t[:, :, :], self.dbg_yT[g].rearrange("(k p) t -> p k t", p=128))
                P.copy("dve", self.yT[g][:, :, :], t[:, :, :])

    def convert_weights(self, l):
        P = self.P
        with P.scope():
            st32 = [P.sb("cv32_%d" % i, [128, 4096], F32) for i in range(2)]
            st16 = [P.sb("cv16_%d" % i, [128, 4096], BF16) for i in range(2)]
            cnt = [0]
            rowg = self.lpv("rowg")

            def cv(src, w, scales, stores):
                b = cnt[0] % 2
                cnt[0] += 1
                s32, s16 = st32[b], st16[b]
                P.dma(self.q(), s32[:, 0:w] if len(src.shape) == 2 else s32[:, 0:w].r("p (k c) -> p k c", k=src.shape[1]), src)
                eng = "dve"
                if scales is None:
                    P.copy(("act", "act", "dve")[cnt[0] % 3], s16[:, 0:w], s32[:, 0:w])
                else:
                    for (a, bnd, col) in scales:
                        P.ts(eng, s16[:, a:bnd], s32[:, a:bnd], rowg[:, col:col + 1], ALU.mult)
                for (dst, a, bnd, pat, kw) in stores:
                    v = s16[:, a:bnd]
                    if pat:
                        v = v.r(pat, **kw)
                    P.dma(self.q(), dst, v)

            for kt in range(8):
                cv(self.w_in[l, kt * 128:(kt + 1) * 128, :], INW, [(0, INW, kt)],
                   [(self.wb_in[l, kt * 128:(kt + 1) * 128, :], 0, INW, None, None)])
            for g in range(4):
                for half in range(2):
                    src = self.w_gate[l, g, half * 512:(half + 1) * 512, :].rearrange("(k p) c -> p k c", p=128)
                    cv(src, 4096, [(k * 1024, (k + 1) * 1024, half * 4 + k) for k in range(4)],
                       [(self.wb_gate[l, :, :, g, half * 4 + k, :].rearrange("c p j -> p c j"), k * 1024, (k + 1) * 1024,
                         "p (c j) -> p c j", dict(j=128)) for k in range(4)])
            for g in range(4):
                src = self.w_branch[l, g].rearrange("(k p) c -> p k c", p=128)
                cv(src, 2048, None,
                   [(self.wb_br[l, :, :, g, k, :].rearrange("c p j -> p c j"), k * 1024, (k + 1) * 1024,
                     "p (c j) -> p c j", dict(j=128)) for k in range(2)])
            for half in range(2):
                src = self.w_out[l, half * 512:(half + 1) * 512, :].rearrange("(k p) c -> p k c", p=128)
                cv(src, 4096, None,
                   [(self.wb_out[l, half * 512:(half + 1) * 512, :].rearrange("(k p) c -> p k c", p=128), 0, 4096,
                     "p (k c) -> p k c", dict(k=4))])
            for kt in range(8):
                cv(self.w_up[l, kt * 128:(kt + 1) * 128, :], 4096, [(0, 4096, 8 + kt)],
                   [(self.wb_up[l, f0:f0 + 8, :, kt, :].rearrange("f p j -> p f j"), f0 * 128, (f0 + 8) * 128,
                     "p (f j) -> p f j", dict(j=128)) for f0 in range(0, 32, 8)])
            for qd in range(8):
                src = self.w_down[l, qd * 512:(qd + 1) * 512, :].rearrange("(k p) c -> p k c", p=128)
                cv(src, 4096, None,
                   [(self.wb_down[l, qd * 512:(qd + 1) * 512, :].rearrange("(k p) c -> p k c", p=128), 0, 4096,
                     "p (k c) -> p k c", dict(k=4))])

    def u_norm(self, l, s, xsrc):
        P = self.P
        ident = self.c("ident")
        with P.scope():
            xt = [P.sb("nx%d" % i, [128, D], F32) for i in range(2)]
            hh = [P.sb("nh%d" % i, [128, D], F32) for i in range(2)]
            junk = P.sb("njunk", [128, D], F32)
            ss = [P.sb("nss%d" % i, [128, 1], F32) for i in range(2)]
            rs = [P.sb("nrs%d" % i, [128, 1], F32) for i in range(2)]
            for i in range(16):
                b = i % 2
                r0 = s * T + i * 128
                P.dma(self.q(), xt[b][:, :], xsrc[r0:r0 + 128, :])
                P.act(junk[:, :], xt[b][:, :], AF.Square, accum=ss[b][:, :])
                P.act(rs[b][:, :], ss[b][:, :], AF.Ln, bias=EPS, scale=1.0 / D)
                P.act(rs[b][:, :], rs[b][:, :], AF.Exp, scale=-0.5)
                P.ts("dve", hh[b][:, :], xt[b][:, :], rs[b][:, 0:1], ALU.mult)
                for g in range(2):
                    bank = self.pb[(2 * i + g) % 4]
                    for k in range(4):
                        kk = g * 4 + k
                        P.tr(bank[:, k * 128:(k + 1) * 128], hh[b][:, kk * 128:(kk + 1) * 128], ident)
                    P.copy("act" if g == 0 else "dve", self.hT[:, g * 4:(g + 1) * 4, i * 128:(i + 1) * 128],
                           bank[:, :].r("p (k n) -> p k n", k=4))

    def u_merge(self, l, s, xsrc, xdst):
        P = self.P
        ident = self.c("ident")
        g_post, g_fpost = self.lpv("g_mix_post"), self.lpv("g_ffn_post")
        pb = self.pb
        with P.scope():
            ws = [P.sb("mw%d" % i, [128, 4096], BF16) for i in range(2)]
            wgs = [P.sb("mwg%d" % i, [128, 8, 128], BF16) for i in range(3)]
            wbr = [P.sb("mwb%d" % i, [128, 4, 2, 128], BF16) for i in range(2)]
            xs = P.sb("mx", [128, 4, D], F32)
            acc = P.sb("macc", [128, 512], F32)
            sg = [P.sb("msg%d" % i, [128, 512], F32) for i in range(2)]
            mTs = [P.sb("mmT%d" % i, [128, 8, 512], BF16) for i in range(2)]
            junk = P.sb("mjunk", [128, D], BF16)
            tmp = P.sb("mtmp", [128, D], F32)
            h2T = P.sb("mh2T", [128, 8, 512], BF16)
            aT = P.sb("maT", [128, 32, 512], BF16)
            rl = [P.sb("mrl%d" % i, [128, 512], F32) for i in range(2)]
            ss = P.sb("mss", [128, 8], F32)
            rs = P.sb("mrs", [128, 4], F32)
            wi = [0]

            def wload(src, pat=None, **kw):
                t = ws[wi[0] % 2]
                wi[0] += 1
                P.dma("sp", t[:, :].r(pat, **kw) if pat else t[:, :], src)
                return t

            def gates(tb):
                t0 = tb * 512
                mT = mTs[tb % 2]
                gi = [0]

                def gload(cc, g):
                    t = wgs[gi[0] % 3]
                    gi[0] += 1
                    P.dma("sp", t[:, :, :], self.wb_gate[l, cc, :, g])
                    return t
                nxt = gload(0, 0)
                for cc in range(8):
                    wb_ = wbr[cc % 2]
                    P.dma("sp", wb_[:, :, :, :], self.wb_br[l, cc])
                    for g in range(4):
                        wg = nxt
                        if not (cc == 7 and g == 3):
                            nxt = gload(cc + (g + 1) // 4, (g + 1) % 4)
                        gp, bp = pb[(2 * g) % 4], pb[(2 * g + 1) % 4]
                        for k in range(8):
                            P.mm(gp[:, :], wg[:, k, :], self.hT[:, k, t0:t0 + 512],
                                 start=(k == 0), stop=(k == 7))
                        for k in range(2):
                            P.mm(bp[:, :], wb_[:, g, k, :], self.yT[g][:, k, t0:t0 + 512], start=(k == 0), stop=(k == 1))
                        sgt = sg[g % 2]
                        P.act(sgt[:, :], gp[:, :], AF.Sigmoid)
                        if g == 0:
                            P.tt("dve", acc[:, :], sgt[:, :], bp[:, :], ALU.mult)
                        else:
                            P.tt("dve", sgt[:, :], sgt[:, :], bp[:, :], ALU.mult)
                            if g < 3:
                                P.tt("pool", acc[:, :], acc[:, :], sgt[:, :], ALU.add)
                            else:
                                P.tt("pool", mT[:, cc, :], acc[:, :], sgt[:, :], ALU.add)
                        yield

            def rest(tb):
                t0 = tb * 512
                r0 = s * T + t0
                mT = mTs[tb % 2]
                P.dma("sp", xs[:, :, :], xsrc[r0:r0 + 512, :].rearrange("(j p) c -> p j c", p=128))
                for jp in range(2):
                    for half in range(2):
                        wo = wload(self.wb_out[l, :, half * 512:(half + 1) * 512].rearrange("(k p) c -> p k c", p=128),
                                   "p (k c) -> p k c", k=8)
                        for j in (2 * jp, 2 * jp + 1):
                            bank = pb[4 + half * 2 + j % 2]
                            for k in range(8):
                                P.mm(bank[:, :], mT[:, k, j * 128:(j + 1) * 128], wo[:, k * 512:(k + 1) * 512],
                                     start=(k == 0), stop=(k == 7))
                    yield
                    for j in (2 * jp, 2 * jp + 1):
                        bk = [pb[4 + half * 2 + j % 2] for half in range(2)]
                        for half in range(2):
                            P.act(junk[:, half * 512:(half + 1) * 512], bk[half][:, :], AF.Square, accum=ss[:, half:half + 1])
                        P.tt("dve", ss[:, 2:3], ss[:, 0:1], ss[:, 1:2], ALU.add)
                        yield
                        P.act(rs[:, 0:1], ss[:, 2:3], AF.Ln, bias=EPS, scale=1.0 / D)
                        P.act(rs[:, 0:1], rs[:, 0:1], AF.Exp, scale=-0.5)
                        yield
                        for half in range(2):
                            cs = slice(half * 512, (half + 1) * 512)
                            P.stt("dve", tmp[:, cs], bk[half][:, :], rs[:, 0:1], g_post[:, cs], ALU.mult, ALU.mult)
                        yield
                        P.tt("pool", xs[:, j, :], xs[:, j, :], tmp[:, :], ALU.add)
                        P.act(junk[:, :], xs[:, j, :], AF.Square, accum=ss[:, 3:4])
                        yield
                        P.act(rs[:, 1:2], ss[:, 3:4], AF.Ln, bias=EPS, scale=1.0 / D)
                        P.act(rs[:, 1:2], rs[:, 1:2], AF.Exp, scale=-0.5)
                        yield
                        P.ts("dve", tmp[:, :], xs[:, j, :], rs[:, 1:2], ALU.mult)
                        yield
                        for g in range(2):
                            bank = bk[g]
                            for k in range(4):
                                kk = g * 4 + k
                                P.tr(bank[:, k * 128:(k + 1) * 128], tmp[:, kk * 128:(kk + 1) * 128], ident)
                            P.copy("act" if g == 0 else "dve", h2T[:, g * 4:(g + 1) * 4, j * 128:(j + 1) * 128],
                                   bank[:, :].r("p (k n) -> p k n", k=4))
                        yield
                for f0 in range(0, 32, 4):
                    wu = wload(self.wb_up[l, f0:f0 + 4].rearrange("f p k j -> p f (k j)"), "p (f c) -> p f c", f=4)
                    for fi in range(4):
                        f = f0 + fi
                        bank = pb[4 + f % 4]
                        for k in range(8):
                            P.mm(bank[:, :], wu[:, (fi * 8 + k) * 128:(fi * 8 + k + 1) * 128], h2T[:, k, :],
                                 start=(k == 0), stop=(k == 7))
                        r = rl[f % 2]
                        P.act(r[:, :], bank[:, :], AF.Relu)
                        P.tt("pool", aT[:, f, :], r[:, :], r[:, :], ALU.mult)
                    yield

            def down(tb):
                t0 = tb * 512
                r0 = s * T + t0
                for k0 in range(0, 32, 4):
                    wd = wload(self.wb_down[l, k0 * 128:(k0 + 4) * 128, :].rearrange("(k p) c -> p k c", p=128), "p (k c) -> p k c", k=4)
                    for ki in range(4):
                        k = k0 + ki
                        for j in range(4):
                            for half in range(2):
                                P.mm(pb[half * 4 + j][:, :], aT[:, k, j * 128:(j + 1) * 128],
                                     wd[:, ki * 1024 + half * 512:ki * 1024 + (half + 1) * 512],
                                     start=(k == 0), stop=(k == 31))
                for j in range(4):
                    for half in range(2):
                        P.act(junk[:, half * 512:(half + 1) * 512], pb[half * 4 + j][:, :], AF.Square,
                              accum=ss[:, 4 + half:5 + half])
                    P.tt("dve", ss[:, 6:7], ss[:, 4:5], ss[:, 5:6], ALU.add)
                    P.act(rs[:, 2:3], ss[:, 6:7], AF.Ln, bias=EPS, scale=1.0 / D)
                    P.act(rs[:, 2:3], rs[:, 2:3], AF.Exp, scale=-0.5)
                    for half in range(2):
                        cs = slice(half * 512, (half + 1) * 512)
                        P.stt("dve", tmp[:, cs], pb[half * 4 + j][:, :], rs[:, 2:3], g_fpost[:, cs], ALU.mult, ALU.mult)
                    P.tt("pool", xs[:, j, :], xs[:, j, :], tmp[:, :], ALU.add)
                P.dma("sp", xdst[r0:r0 + 512, :].rearrange("(j p) c -> p j c", p=128), xs[:, :, :])

            def run_rr(gens):
                gens = list(gens)
                while gens:
                    for g in list(gens):
                        try:
                            next(g)
                        except StopIteration:
                            gens.remove(g)

            run_rr([gates(0)])
            for tb in range(4):
                gens = [rest(tb)]
                if tb + 1 < 4:
                    gens.append(gates(tb + 1))
                run_rr(gens)
                down(tb)

    def _proj_fm(self, wt, col0, out_writer, nchunks=4):
        P = self.P
        for c in range(nchunks):
            bank = self.pb[self._pj % 4]
            self._pj += 1
            for k in range(8):
                P.mm(bank[:, :], wt[:, k, col0:col0 + 128], self.hT[:, k, c * 512:(c + 1) * 512], start=(k == 0), stop=(k == 7))
            out_writer(c, bank[:, :])

    def _proj_tm(self, wt, col0, ncol, i, bank):
        P = self.P
        for k in range(8):
            P.mm(bank[:, 0:ncol], self.hT[:, k, i * 128:(i + 1) * 128], wt[:, k, col0:col0 + ncol], start=(k == 0), stop=(k == 7))

    def u_sb(self, l, s):
        P = self.P
        pb = self.pb
        self._pj = 0
        with P.scope():
            wt = P.sb("sw", [128, 8, 768], BF16)
            qT = P.sb("sq", [128, 2, T], BF16)
            kT = P.sb("sk", [128, 2, T], BF16)
            vv = P.sb("sv", [128, 16, 256], BF16)
            NS = 4
            E = [P.sb("sE%d" % i, [128, 512], F32) for i in range(NS)]
            Lp = [P.sb("sL%d" % i, [128, 512], BF16) for i in range(NS)]
            G = [P.sb("sG%d" % i, [128, 512], F32) for i in range(NS)]
            Wt = [P.sb("sW%d" % i, [128, 512], BF16) for i in range(NS)]
            Pacc = [P.sb("sPacc%d" % i, [128, 512], BF16) for i in range(NS)]
            P.dma("sp", wt[:, :, :], self.wb_in[l, :, OFF_SB:OFF_SB + 768].rearrange("(k p) c -> p k c", p=128))
            for hp in range(2):
                self._proj_fm(wt, hp * 128, lambda c, ps, hp=hp: P.ts("dve", qT[:, hp, c * 512:(c + 1) * 512], ps, 0.125, ALU.mult))
                self._proj_fm(wt, 256 + hp * 128, lambda c, ps, hp=hp: P.copy("act", kT[:, hp, c * 512:(c + 1) * 512], ps))
            for i in range(16):
                bank = pb[4 + i % 2]
                self._proj_tm(wt, 512, 256, i, bank)
                P.copy("act" if i % 2 else "dve", vv[:, i, :], bank[:, 0:256])

            def stream(h, qb, sl):
                hp, r = h // 2, slice((h % 2) * 64, (h % 2) * 64 + 64)
                q0 = qb * 512
                wb_, accb = pb[sl], pb[4 + sl]
                Es, Ls, Gs, Ws, Pa = E[sl], Lp[sl], G[sl], Wt[sl], Pacc[sl]
                P.memset("pool", Pa[:, :], 0.0)
                nk = 4 * qb + 4
                for kt in range(nk - 1, -1, -1):
                    c0 = max(0, kt - 4 * qb) * 128
                    first = (kt == nk - 1)
                    P.mm(wb_[:, c0:512], kT[r, hp, kt * 128:(kt + 1) * 128], qT[r, hp, q0 + c0:q0 + 512])
                    yield
                    P.act(Es[:, c0:512], wb_[:, c0:512], AF.Exp)
                    yield
                    if kt >= 4 * qb:
                        P.tt("pool", Es[:, c0:c0 + 128], Es[:, c0:c0 + 128], self.c("m_strict"), ALU.mult)
                        yield
                    P.act(Ls[:, c0:512], Es[:, c0:512], AF.Ln, bias=1.0)
                    yield
                    P.mm(wb_[:, c0:512], self.uincl_bf[:, :], Ls[:, c0:512], start=True, stop=first)
                    if not first:
                        P.mm(wb_[:, c0:512], self.ones_bf[:, :], Pa[:, c0:512], start=False, stop=True)
                    yield
                    P.act(Gs[:, c0:512], wb_[:, c0:512], AF.Exp, scale=-1.0)
                    if c0 > 0:
                        P.memset("pool", Ws[:, 0:c0], 0.0)
                    yield
                    P.tt("dve", Ws[:, c0:512], Es[:, c0:512], Gs[:, c0:512], ALU.mult)
                    if kt > 0:
                        P.tt("pool", Pa[:, c0:512], Pa[:, c0:512], Ls[:, c0:512], ALU.add)
                    yield
                    P.mm(accb[:, :], vv[:, kt, hp * 128:(hp + 1) * 128], Ws[:, :], start=first, stop=(kt == 0))
                    yield
                P.copy("act", self.yT[2][r, hp, q0:q0 + 512], accb[r, :])
                yield

            todo = sorted([(h, qb) for h in range(4) for qb in range(4)], key=lambda t: -t[1])
            active = {}
            while todo or active:
                for sl in range(NS):
                    if sl not in active and todo:
                        h, qb = todo.pop(0)
                        active[sl] = stream(h, qb, sl)
                for sl in list(active):
                    try:
                        next(active[sl])
                    except StopIteration:
                        del active[sl]

    def u_moba(self, l, s):
        P = self.P
        pb = self.pb
        self._pj = 0
        ident = self.c("ident")
        with P.scope():
            wt = P.sb("bw", [128, 8, 768], BF16)
            q32 = P.sb("bq32", [128, 2, T], F32)
            qT = P.sb("bq", [128, 2, T], BF16)
            kT = P.sb("bk", [128, 2, T], BF16)
            vv = P.sb("bv", [128, 4, 16, 128], BF16)
            ksum = P.sb("bks", [128, 2, 8], F32)
            gm = P.sb("bgm", [128, 64, 8], F32)
            thr = P.sb("bthr", [128, 64, 8], F32)
            sel = P.sb("bsel", [128, 512], F32)
            aug = P.sb("baug", [128, T], BF16)
            NS = 4
            Pt = [P.sb("bP%d" % i, [128, 512], BF16) for i in range(NS)]
            rden = [P.sb("brd%d" % i, [128, 512], F32) for i in range(NS)]
            P.memset("pool", vv[:, :, :, :], 1.0)
            self.mcst = P.sb("bmcst", [128, NCONST - NCONST_RES], F32)
            P.dma("sp", self.mcst[:, :], self.consts[:, NCONST_RES:NCONST])
            P.dma("sp", wt[:, :, :], self.wb_in[l, :, OFF_MOBA:OFF_MOBA + 768].rearrange("(k p) c -> p k c", p=128))
            P.copy("pool", aug[32:34, :].r("p (a t) -> p a t", a=4), self.c("augrows")[32:34, :].un(1).bc([2, 4, 512]))
            for hp in range(2):
                def wq(c, ps, hp=hp):
                    P.ts("dve", q32[:, hp, c * 512:(c + 1) * 512], ps, 0.125, ALU.mult)
                    P.copy("pool", qT[:, hp, c * 512:(c + 1) * 512], q32[:, hp, c * 512:(c + 1) * 512])
                self._proj_fm(wt, hp * 128, wq)

                def wk(c, ps, hp=hp):
                    P.copy("act", kT[:, hp, c * 512:(c + 1) * 512], ps)
                    P.reduce("dve", ksum[:, hp, 2 * c:2 * c + 2], ps.r("p (n t) -> p n t", n=2))
                self._proj_fm(wt, 256 + hp * 128, wk)
            for i in range(16):
                bank = pb[4 + i % 2]
                self._proj_tm(wt, 512, 256, i, bank)
                v4 = bank[:, 0:256].r("p (h d) -> p h d", h=4)
                P.copy("act", vv[:, 0:4:2, i, 0:64], v4[:, 0:4:2, :])
                P.copy("dve", vv[:, 1:4:2, i, 64:128], v4[:, 1:4:2, :])
            if "moba_stop1" in self.dbg:
                return
            for hh in range(2):
                gb = pb[6 + hh]
                r = slice(hh * 64, hh * 64 + 64)
                for i in range(16):
                    for hp in range(2):
                        o = (i * 2 + hp) * 8
                        P.mm(gb[:, o:o + 8], q32[r, hp, i * 128:(i + 1) * 128], ksum[r, hp, :])
                P.tt("dve", gm[:, :, :].r("p (i hp hh) n -> p i hp hh n", hp=2, hh=2)[:, :, :, hh, :],
                     gb[:, 0:256].r("p (i hp n) -> p i hp n", hp=2, n=8),
                     self.c("pastbias").r("p (i hp hh n) -> p i hp hh n", hp=2, hh=2, n=8)[:, :, :, hh, :], ALU.add)
            for a in range(64):
                P.max8(thr[:, a, :], gm[:, a, :])
            P.tt("dve", sel[:, :].r("p (a n) -> p a n", n=8), gm[:, :, :], thr[:, :, 2:3].bc([128, 64, 8]), ALU.is_ge)
            P.tt("dve", sel[:, :], sel[:, :], self.c("pastm"), ALU.mult)
            P.tt("dve", sel[:, :], sel[:, :], self.c("ownm"), ALU.add)
            P.ts("dve", sel[:, :], sel[:, :], -1.0, ALU.add, 1000.0, ALU.mult)
            if "moba_stop2" in self.dbg:
                return
            for g4 in range(4):
                tb_ = pb[g4 % 2]
                for j in range(4):
                    i = g4 * 4 + j
                    P.tr(tb_[0:32, j * 128:(j + 1) * 128], sel[:, i * 32:(i + 1) * 32], ident)
                P.copy("act", aug[0:32, g4 * 512:(g4 + 1) * 512], tb_[0:32, :])
            alibi = self.c("alibi")

            def stream(h, qb, sl):
                hp, r = h // 2, slice((h % 2) * 64, (h % 2) * 64 + 64)
                ro = slice(64 - (h % 2) * 64, 128 - (h % 2) * 64)
                q0 = qb * 512
                sb_, accb = pb[sl], pb[4 + sl]
                Ps = Pt[sl]
                nk = 4 * qb + 4
                for kt in range(nk):
                    c0 = max(0, kt - 4 * qb) * 128
                    n = kt // 2
                    P.mm(sb_[:, c0:512], kT[r, hp, kt * 128:(kt + 1) * 128], qT[r, hp, q0 + c0:q0 + 512], start=True, stop=False)
                    P.mm(sb_[:, c0:512], self.sel_bf[0:34, h * 8 + n, :], aug[0:34, q0 + c0:q0 + 512], start=False, stop=True)
                    yield
                    dl = kt - 4 * qb + 12
                    P.act(Ps[:, c0:512], sb_[:, c0:512], AF.Exp, bias=alibi[:, h * 16 + dl:h * 16 + dl + 1])
                    yield
                    if kt >= 4 * qb:
                        P.tt("pool", Ps[:, c0:c0 + 128], Ps[:, c0:c0 + 128], self.c("m_incl"), ALU.mult)
                        if c0 > 0:
                            P.memset("pool", Ps[:, 0:c0], 0.0)
                        yield
                    P.mm(accb[:, :], vv[:, h, kt, :], Ps[:, :], start=(kt == 0), stop=(kt == nk - 1))
                    yield
                P.recip(rden[sl][r, :], accb[ro, :])
                yield
                P.tt("dve", self.yT[1][r, hp, q0:q0 + 512], accb[r, :], rden[sl][r, :], ALU.mult)
                yield

            todo = sorted([(h, qb) for h in range(0 if "moba_noattn" not in self.dbg else 4, 4) for qb in range(4)], key=lambda t: -t[1])
            active = {}
            while todo or active:
                for sl in range(NS):
                    if sl not in active and todo:
                        h, qb = todo.pop(0)
                        active[sl] = stream(h, qb, sl)
                for sl in list(active):
                    try:
                        next(active[sl])
                    except StopIteration:
                        del active[sl]

    def _conv_chunk(self, wt, col0, m, c, pre, acc, cw, out_view, bias, eng):
        P = self.P
        cb_ = getattr(self, "_cbanks", (0, 1, 2, 3))
        bank = self.pb[cb_[self._pj % len(cb_)]]
        self._pj += 1
        for k in range(8):
            P.mm(bank[:, :], wt[:, k, col0:col0 + 128], self.hT[:, k, c * 512:(c + 1) * 512], start=(k == 0), stop=(k == 7))
        if c == 0:
            P.memset(eng, pre[:, m, 0:3], 0.0)
        else:
            P.copy(eng, pre[:, m, 0:3], pre[:, m, 512:515])
        P.copy("act", pre[:, m, 3:515], bank[:, :])
        P.ts(eng, acc[:, :], pre[:, m, 3:515], cw[:, m * 4 + 3:m * 4 + 4], ALU.mult)
        for j in range(1, 4):
            P.stt("dve", acc[:, :], pre[:, m, 3 - j:515 - j], cw[:, m * 4 + 3 - j:m * 4 + 4 - j], acc[:, :], ALU.mult, ALU.add)
        if bias is None:
            P.act(out_view, acc[:, :], AF.Silu)
        else:
            P.act(out_view, acc[:, :], AF.Silu, bias=bias)

    def u_ssd(self, l, s):
        P = self.P
        pb = self.pb
        self._pj = 0
        ident = self.c("ident")
        ones = self.c("ones")
        with P.scope():
            wt = P.sb("dw", [128, 8, 1028], BF16)
            pre = P.sb("dpre", [128, 6, 515], F32)
            accs = [P.sb("dacc%d" % i, [128, 512], F32) for i in range(2)]
            xbc = [P.sb("dxbc%d" % i, [128, 6, 512], F32) for i in range(2)]
            zs = P.sb("dzs", [128, 4, 256], F32)
            dt = P.sb("ddt", [128, 16, 4], F32)
            gg = P.sb("dgg", [128, 16, 4], F32)
            aneg = P.sb("daneg", [128, 4], F32)
            tok = [P.sb("dtok%d" % i, [128, 512], F32) for i in range(2)]
            gtri = P.sb("dgtri", [128, 4, 128], F32)
            Dm = P.sb("dD", [128, 4, 128], F32)
            lm = P.sb("dlm", [128, 4, 128], F32)
            Mms = [P.sb("dM%d" % i, [128, 4, 128], F32) for i in range(2)]
            sms = [P.sb("dsm%d" % i, [128, 20], F32) for i in range(2)]
            smb = P.sb("dsmb", [128, 4], F32)
            xws = [P.sb("dxw%d" % i, [128, 256], F32) for i in range(2)]
            dx = P.sb("ddx", [128, 256], F32)
            hst = P.sb("dhst", [128, 256], F32)
            yb = [P.sb("dy%d" % i, [128, 256], F32) for i in range(2)]
            junk = P.sb("djunk", [128, 256], F32)
            zsb = [P.sb("dzs%d" % i, [128, 4, 256], F32) for i in range(2)]
            P.dma("sp", wt[:, :, :], self.wb_in[l, :, OFF_SSD:OFF_SSD + 1028].rearrange("(k p) c -> p k c", p=128))
            cw, cb = self.lpv("ssd_conv"), self.lpv("ssd_cb")
            dtb = pb[7]
            for i in range(16):
                for k in range(8):
                    P.mm(dtb[:, i * 4:(i + 1) * 4], self.hT[:, k, i * 128:(i + 1) * 128], wt[:, k, 1024:1028], start=(k == 0), stop=(k == 7))
            P.tt("dve", dt[:, :, :], dtb[:, 0:64].r("p (i h) -> p i h", h=4), self.lpv("ssd_dtb").un(1).bc([128, 16, 4]), ALU.add)
            P.act(dt[:, :, :], dt[:, :, :], AF.Exp)
            P.act(dt[:, :, :], dt[:, :, :], AF.Ln, bias=1.0)
            P.act(aneg[:, :], self.lpv("ssd_alog"), AF.Exp)
            P.stt("dve", gg[:, :, :], dt[:, :, :], -1.0, aneg[:, :].un(1).bc([128, 16, 4]), ALU.mult, ALU.mult)
            P.memset("pool", hst[:, :], 0.0)

            def chunk_front(c):
                xb = xbc[c % 2]
                self._cbanks = (0, 2, 3)
                for m in range(6):
                    self._conv_chunk(wt, 256 + m * 128, m, c, pre, accs[m % 2], cw, xb[:, m, :], cb[:, m:m + 1],
                                     "dve" if m % 2 == 0 else "pool")
                self._cbanks = (0, 1, 2, 3)
                for j in range(4):
                    i = c * 4 + j
                    bank = pb[4 + j % 2]
                    self._proj_tm(wt, 0, 256, i, bank)
                    P.act(zsb[c % 2][:, j, :], bank[:, 0:256], AF.Silu)

            def front(i):
                c, j = i // 4, i % 4
                xb = xbc[c % 2]
                cs = slice(j * 128, (j + 1) * 128)
                b = i % 2
                tk, sm, Mm, xw = tok[b], sms[b], Mms[b], xws[b]
                tb_ = pb[4]
                for m in range(4):
                    P.tr(tb_[:, m * 128:(m + 1) * 128], xb[:, m, cs], ident)
                sb_ = pb[5]
                P.mm(sb_[:, 0:4], self.c("tri128"), gg[:, i, :])
                P.tt("pool", gtri[:, :, :], self.c("tri128").un(1).bc([128, 4, 128]), gg[:, i, :].un(2).bc([128, 4, 128]), ALU.mult)
                yield
                P.copy("act", tk[:, :], tb_[:, :])
                P.copy("dve", sm[:, 0:4], sb_[:, 0:4])
                ab = pb[6]
                P.mm(ab[:, :], ones, gtri[:, :, :].r("p h l -> p (h l)"))
                yield
                P.copy("dve", sm[:, 4:8], ab[:, :].r("p (h l) -> p h l", h=4)[:, :, 127])
                P.tt("dve", Dm[:, :, :], ab[:, :].r("p (h l) -> p h l", h=4), sm[:, 0:4].un(2).bc([128, 4, 128]), ALU.subtract)
                P.act(sm[:, 8:12], sm[:, 0:4], AF.Exp)
                yield
                P.tt("pool", Dm[:, :, :], Dm[:, :, :], self.c("neg_ssd").un(1).bc([128, 4, 128]), ALU.add)
                P.act(sm[:, 12:16], sm[:, 4:8], AF.Exp)
                P.tt("dve", sm[:, 16:20], sm[:, 4:8], sm[:, 0:4], ALU.subtract)
                scb = pb[0]
                for g in range(2):
                    P.mm(scb[:, g * 128:(g + 1) * 128], xb[:, 2 + g, cs], xb[:, 4 + g, cs])
                yield
                P.act(lm[:, :, :], Dm[:, :, :], AF.Exp)
                P.act(sm[:, 16:20], sm[:, 16:20], AF.Exp)
                yield
                P.tt("dve", sm[:, 16:20], sm[:, 16:20], dt[:, i, :], ALU.mult)
                for h in range(4):
                    P.stt("dve", Mm[:, h, :], lm[:, h, :], dt[:, i, h:h + 1], scb[:, (h // 2) * 128:(h // 2 + 1) * 128], ALU.mult, ALU.mult)
                yield
                P.tt("pool", xw[:, :].r("p (h e) -> p h e", h=4), tk[:, 0:256].r("p (h e) -> p h e", h=4),
                     sm[:, 16:20].un(2).bc([128, 4, 64]), ALU.mult)
                yb_ = pb[1] if b == 0 else pb[7]
                for h in range(4):
                    P.mm(yb_[:, h * 64:(h + 1) * 64], Mm[:, h, :], tk[:, h * 64:(h + 1) * 64])
                yield

            def back(i):
                c, j = i // 4, i % 4
                xb = xbc[c % 2]
                cs = slice(j * 128, (j + 1) * 128)
                b = i % 2
                tk, sm, xw = tok[b], sms[b], xws[b]
                yb_ = pb[1] if b == 0 else pb[7]
                for h in range(4):
                    P.mm(yb_[:, 256 + h * 64:256 + (h + 1) * 64], xb[:, 4 + h // 2, cs], hst[:, h * 64:(h + 1) * 64])
                hb = pb[2]
                for g in range(2):
                    P.mm(hb[:, g * 128:(g + 1) * 128], tk[:, 256 + g * 128:256 + (g + 1) * 128], xw[:, g * 128:(g + 1) * 128])
                P.tt("pool", dx[:, :].r("p (h e) -> p h e", h=4), tk[:, 0:256].r("p (h e) -> p h e", h=4),
                     self.lpv("ssd_d").un(2).bc([128, 4, 64]), ALU.mult)
                yield
                y = yb[b]
                P.tt("dve", y[:, :].r("p (h e) -> p h e", h=4), yb_[:, 256:512].r("p (h e) -> p h e", h=4),
                     sm[:, 8:12].un(2).bc([128, 4, 64]), ALU.mult)
                P.tt("pool", hst[:, :].r("p (h e) -> p h e", h=4), hst[:, :].r("p (h e) -> p h e", h=4),
                     sm[:, 12:16].un(2).bc([128, 4, 64]), ALU.mult)
                yield
                P.tt("dve", y[:, :], y[:, :], yb_[:, 0:256], ALU.add)
                P.tt("dve", hst[:, :], hst[:, :], hb[:, 0:256], ALU.add)
                yield
                P.tt("pool", y[:, :], y[:, :], dx[:, :], ALU.add)
                yield
                P.tt("pool", y[:, :], y[:, :], zsb[c % 2][:, j, :], ALU.mult)
                yield
                P.act(junk[:, :], y[:, :], AF.Square, accum=smb[:, 0:1])
                yield
                P.act(smb[:, 1:2], smb[:, 0:1], AF.Ln, bias=EPS, scale=1.0 / 256)
                P.act(smb[:, 1:2], smb[:, 1:2], AF.Exp, scale=-0.5)
                yield
                P.stt("dve", y[:, :], y[:, :], smb[:, 1:2], self.lpv("ssd_norm"), ALU.mult, ALU.mult)
                yield
                ob = pb[3]
                for k in range(2):
                    P.tr(ob[:, k * 128:(k + 1) * 128], y[:, k * 128:(k + 1) * 128], ident)
                yield
                P.copy("act", self.yT[3][:, :, i * 128:(i + 1) * 128], ob[:, 0:256].r("p (k t) -> p k t", k=2))
                yield

            def run_rr(gens):
                gens = list(gens)
                while gens:
                    for g in list(gens):
                        try:
                            next(g)
                        except StopIteration:
                            gens.remove(g)

            chunk_front(0)
            run_rr([front(0)])
            for i in range(16):
                if i % 4 == 0 and i + 4 < 16:
                    chunk_front(i // 4 + 1)
                gens = [back(i)]
                if i + 1 < 16:
                    gens.append(front(i + 1))
                run_rr(gens)

    def u_gdn(self, l, s):
        P = self.P
        pb = self.pb
        self._pj = 0
        ident = self.c("ident")
        ones = self.c("ones")
        with P.scope():
            wt = P.sb("gw", [128, 8, 1032], BF16)
            pre = P.sb("gpre", [128, 6, 515], F32)
            accs = [P.sb("gacc%d" % i, [128, 512], F32) for i in range(2)]
            qkv = [P.sb("gqkv%d" % i, [128, 6, 512], F32) for i in range(2)]
            sq = P.sb("gsq", [128, 512], F32)
            rsn = P.sb("grsn", [128, 512], F32)
            bg = P.sb("gbg", [128, 16, 8], F32)
            beta = P.sb("gbeta", [128, 16, 4], F32)
            lnb = P.sb("glnb", [128, 16, 4], F32)
            gg = P.sb("ggg", [128, 16, 4], F32)
            aneg = P.sb("ganeg", [128, 4], F32)
            kv = [P.sb("gkv%d" % i, [128, 512], F32) for i in range(2)]
            r1 = P.sb("gr1", [128, 4, 128], F32)
            r2 = P.sb("gr2", [128, 4, 128], F32)
            X2 = P.sb("gX2", [128, 4, 128], F32)
            X3 = P.sb("gX3", [128, 4, 128], F32)
            egB = P.sb("gegB", [128, 4, 128], F32)
            smps = [P.sb("gsmp%d" % i, [128, 20], F32) for i in range(2)]
            sms = P.sb("gsms", [128, 8], F32)
            zsb = [P.sb("gzs%d" % i, [128, 4, 256], F32) for i in range(2)]
            qd = [P.sb("gqd%d" % i, [128, 2, 128], F32) for i in range(2)]
            kdec = [P.sb("gkd%d" % i, [128, 256], F32) for i in range(2)]
            kbg = P.sb("gkbg", [128, 256], F32)
            vb = P.sb("gvb", [128, 256], F32)
            NT = [P.sb("gNT%d" % h, [128, 128], F32) for h in range(4)]
            Nn = [P.sb("gN%d" % h, [128, 128], F32) for h in range(4)]
            Ma = [P.sb("gMa%d" % h, [128, 128], F32) for h in range(4)]
            MTa = [P.sb("gMTa%d" % h, [128, 128], F32) for h in range(4)]
            PT = [P.sb("gPT%d" % h, [128, 128], F32) for h in range(4)]
            Aq = [[P.sb("gAq%d_%d" % (b, h), [128, 128], F32) for h in range(4)] for b in range(2)]
            us = [P.sb("gu%d" % i, [128, 4, 64], F32) for i in range(2)]
            wTs = [P.sb("gwT%d" % i, [128, 2, 128], F32) for i in range(2)]
            gts = [P.sb("ggt%d" % i, [128, 4, 2], F32) for i in range(2)]
            S = P.sb("gS", [128, 2, 64], F32)
            vnew = P.sb("gvn", [128, 4, 64], F32)
            ot = P.sb("got", [128, 4, 64], F32)
            on = P.sb("gon", [128, 256], F32)
            P.dma("sp", wt[:, :, :], self.wb_in[l, :, OFF_GDN:OFF_GDN + 1032].rearrange("(k p) c -> p k c", p=128))
            cw = self.lpv("gdn_conv")
            bgb = pb[7]
            for i in range(16):
                for k in range(8):
                    P.mm(bgb[:, i * 8:(i + 1) * 8], self.hT[:, k, i * 128:(i + 1) * 128], wt[:, k, 1024:1032], start=(k == 0), stop=(k == 7))
            P.copy("dve", bg[:, :, :], bgb[:, 0:128].r("p (i c) -> p i c", c=8))
            P.act(lnb[:, :, :], bg[:, :, 0:4], AF.Exp, scale=-1.0)
            P.act(lnb[:, :, :], lnb[:, :, :], AF.Ln, bias=1.0)
            P.ts("dve", lnb[:, :, :], lnb[:, :, :], -1.0, ALU.mult)
            P.act(beta[:, :, :], lnb[:, :, :], AF.Exp)
            P.tt("dve", gg[:, :, :], bg[:, :, 4:8], self.lpv("gdn_dtb").un(1).bc([128, 16, 4]), ALU.add)
            P.act(gg[:, :, :], gg[:, :, :], AF.Exp)
            P.act(gg[:, :, :], gg[:, :, :], AF.Ln, bias=1.0)
            P.act(aneg[:, :], self.lpv("gdn_alog"), AF.Exp)
            P.stt("dve", gg[:, :, :], gg[:, :, :], -1.0, aneg[:, :].un(1).bc([128, 16, 4]), ALU.mult, ALU.mult)
            P.memset("pool", S[:, :, :], 0.0)

            def prep_common(i, qk):
                j = i % 4
                cs = slice(j * 128, (j + 1) * 128)
                b = i % 2
                kvt = kv[b]
                smp = smps[b]
                tb_ = pb[4]
                for m in range(4):
                    P.tr(tb_[:, m * 128:(m + 1) * 128], qk[:, 2 + m, cs], ident)
                P.copy("act", kvt[:, :], tb_[:, :])
                sb_ = pb[3]
                P.mm(sb_[:, 0:4], self.c("tri_bd"), gg[:, i, :])
                P.copy("dve", smp[:, 0:4], sb_[:, 0:4])
                P.tt("pool", r1[:, :, :], self.c("tri_bd").un(1).bc([128, 4, 128]), gg[:, i, :].un(2).bc([128, 4, 128]), ALU.mult)
                P.tt("pool", r2[:, :, :], self.c("ident").un(1).bc([128, 4, 128]), lnb[:, i, :].un(2).bc([128, 4, 128]), ALU.mult)
                P.tt("pool", r2[:, :, :], r2[:, :, :], r1[:, :, :], ALU.add)
                gcB, gcbB = pb[0], pb[1]
                P.mm(gcB[:, :], ones, r1[:, :, :].r("p h c -> p (h c)"))
                P.mm(gcbB[:, :], ones, r2[:, :, :].r("p h c -> p (h c)"))
                gc_b = smp[:, 0:4].un(2).bc([128, 4, 128])
                P.tt("dve", X2[:, :, :], gcB[:, :].r("p (h c) -> p h c", h=4), gc_b, ALU.subtract)
                P.tt("pool", X2[:, :, :], X2[:, :, :], self.c("neg_incl_bd").un(1).bc([128, 4, 128]), ALU.add)
                P.act(X2[:, :, :], X2[:, :, :], AF.Exp)
                P.tt("dve", X3[:, :, :], gcbB[:, :].r("p (h c) -> p h c", h=4), gc_b, ALU.subtract)
                P.tt("pool", X3[:, :, :], X3[:, :, :], self.c("neg_strict_bd").un(1).bc([128, 4, 128]), ALU.add)
                P.act(X3[:, :, :], X3[:, :, :], AF.Exp)
                P.act(egB[:, :, :], gcB[:, :].r("p (h c) -> p h c", h=4), AF.Exp)
                gt = gts[b]
                P.act(gt[:, :, :], gcB[:, :].r("p (h c) -> p h c", h=4)[:, :, 63:128:64], AF.Exp)
                P.copy("dve", smp[0:64, 4:8], gcB[0:64, :].r("p (h c) -> p h c", h=4)[:, :, 63])
                P.copy("dve", smp[64:128, 4:8], gcB[64:128, :].r("p (h c) -> p h c", h=4)[:, :, 127])
                P.act(smp[:, 8:12], smp[:, 0:4], AF.Exp)
                P.tt("dve", smp[:, 12:16], smp[:, 4:8], smp[:, 0:4], ALU.subtract)
                P.act(smp[:, 12:16], smp[:, 12:16], AF.Exp)
                P.tt("dve", smp[:, 16:20], smp[:, 8:12], beta[:, i, :], ALU.mult)
                kd = kdec[b]
                k4 = kvt[:, 0:256].r("p (h d) -> p h d", h=4)
                P.tt("pool", kd[:, :].r("p (h d) -> p h d", h=4), k4, smp[:, 12:16].un(2).bc([128, 4, 64]), ALU.mult)
                P.tt("pool", kbg[:, :].r("p (h d) -> p h d", h=4), k4, smp[:, 16:20].un(2).bc([128, 4, 64]), ALU.mult)
                P.tt("pool", vb[:, :].r("p (h d) -> p h d", h=4), kvt[:, 256:512].r("p (h d) -> p h d", h=4),
                     beta[:, i, :].un(2).bc([128, 4, 64]), ALU.mult)
                qdt = qd[b]
                for hp in range(2):
                    for hh in range(2):
                        r = slice(hh * 64, hh * 64 + 64)
                        P.tt("pool", qdt[r, hp, :], qk[r, hp, cs], egB[r, hp * 2 + hh, :], ALU.mult)

            def head_chain(i, qk, h):
                j = i % 4
                cs = slice(j * 128, (j + 1) * 128)
                b = i % 2
                hp, r = h // 2, slice((h % 2) * 64, (h % 2) * 64 + 64)
                hb = pb[h]
                P.mm(hb[:, 0:128], qk[r, 2 + hp, cs], qk[r, 2 + hp, cs])
                P.mm(hb[:, 128:256], qk[r, 2 + hp, cs], qk[r, hp, cs])
                yield
                P.stt("dve", NT[h][:, :], hb[:, 0:128], -1.0, X3[:, h, :], ALU.mult, ALU.mult)
                P.tt("dve", Aq[b][h][:, :], hb[:, 128:256], X2[:, h, :], ALU.mult)
                yield
                P.tr(hb[:, 256:384], NT[h][:, :], ident)
                P.tt("pool", PT[h][:, :], NT[h][:, :], ident, ALU.add)
                yield
                P.copy("act", Nn[h][:, :], hb[:, 256:384])
                yield
                M, MT = Nn[h], NT[h]
                for lvl in range(5):
                    P.mm(hb[:, 0:128], MT[:, :], M[:, :])
                    if lvl < 4:
                        P.mm(hb[:, 128:256], M[:, :], MT[:, :])
                    yield
                    M2 = Ma[h] if lvl % 2 == 0 else Nn[h]
                    MT2 = MTa[h] if lvl % 2 == 0 else NT[h]
                    P.copy("act", M2[:, :], hb[:, 0:128])
                    if lvl < 4:
                        P.copy("dve", MT2[:, :], hb[:, 128:256])
                    yield
                    P.mm(hb[:, 256:384], M2[:, :], PT[h][:, :])
                    yield
                    P.tt("dve", PT[h][:, :], PT[h][:, :], hb[:, 256:384], ALU.add)
                    yield
                    M, MT = M2, MT2
                P.mm(hb[:, 0:64], PT[h][:, :], vb[:, h * 64:(h + 1) * 64])
                P.mm(hb[:, 128:256], kbg[:, hp * 128:(hp + 1) * 128], PT[h][:, :])
                yield
                P.copy("act", us[b][:, h, :], hb[:, 0:64])
                P.copy("dve", wTs[b][r, hp, :], hb[r, 128:256])
                yield

            def scan(i):
                b = i % 2
                j4 = i % 4
                for jc in range(2):
                    rj = slice(jc * 64, jc * 64 + 64)
                    wsb, ob, snb = pb[5], pb[6], pb[7]
                    for hh in range(2):
                        for h in (hh, hh + 2):
                            hp, r = h // 2, slice((h % 2) * 64, (h % 2) * 64 + 64)
                            P.mm(wsb[:, h * 64:(h + 1) * 64], wTs[b][r, hp, :], S[r, hp, :])
                    yield
                    P.tt("dve", vnew[rj, :, :], us[b][rj, :, :], wsb[rj, 0:256].r("p (h e) -> p h e", h=4), ALU.subtract)
                    yield
                    for h in range(4):
                        hp, r = h // 2, slice((h % 2) * 64, (h % 2) * 64 + 64)
                        P.mm(ob[:, h * 64:(h + 1) * 64], qd[b][r, hp, :], S[r, hp, :], start=True, stop=False)
                        P.mm(ob[:, h * 64:(h + 1) * 64], Aq[b][h][rj, :], vnew[rj, h, :], start=False, stop=True)
                    for h in range(4):
                        hp = h // 2
                        P.mm(snb[:, h * 64:(h + 1) * 64], kdec[b][rj, hp * 128:(hp + 1) * 128], vnew[rj, h, :])
                    yield
                    for h in range(4):
                        hp, r = h // 2, slice((h % 2) * 64, (h % 2) * 64 + 64)
                        P.stt("dve", S[r, hp, :], S[r, hp, :], gts[b][r, h, jc:jc + 1], snb[r, h * 64:(h + 1) * 64], ALU.mult, ALU.add)
                    P.copy("act", ot[rj, :, :], ob[rj, 0:256].r("p (h e) -> p h e", h=4))
                    yield
                P.tt("pool", on[:, :], ot[:, :, :].r("p h e -> p (h e)"), ot[:, :, :].r("p h e -> p (h e)"), ALU.mult)
                P.reduce("dve", sms[:, 0:4], on[:, :].r("p (h e) -> p h e", h=4))
                yield
                P.act(sms[:, 4:8], sms[:, 0:4], AF.Ln, bias=EPS, scale=1.0 / 64)
                P.act(sms[:, 4:8], sms[:, 4:8], AF.Exp, scale=-0.5)
                yield
                P.tt("pool", on[:, :].r("p (h e) -> p h e", h=4), ot[:, :, :], sms[:, 4:8].un(2).bc([128, 4, 64]), ALU.mult)
                P.tt("pool", on[:, :].r("p (h e) -> p h e", h=4), on[:, :].r("p (h e) -> p h e", h=4),
                     self.lpv("gdn_norm").un(1).bc([128, 4, 64]), ALU.mult)
                P.tt("pool", on[:, :], on[:, :], zsb[(i // 4) % 2][:, j4, :], ALU.mult)
                yield
                tb2 = pb[4]
                for k in range(2):
                    P.tr(tb2[:, 256 + k * 128:256 + (k + 1) * 128], on[:, k * 128:(k + 1) * 128], ident)
                P.copy("act", self.yT[0][:, :, i * 128:(i + 1) * 128], tb2[:, 256:512].r("p (k t) -> p k t", k=2))
                yield

            def chunk_front(c):
                qk = qkv[c % 2]
                for m in range(6):
                    self._conv_chunk(wt, m * 128, m, c, pre, accs[m % 2], cw, qk[:, m, :], None, "dve" if m % 2 == 0 else "pool")
                for m in range(4):
                    P.act(sq[:, :], qk[:, m, :], AF.Square)
                    nb = pb[m % 2]
                    P.mm(nb[:, :], self.c("blk64"), sq[:, :])
                    P.act(rsn[:, :], nb[:, :], AF.Ln, bias=EPS)
                    P.act(rsn[:, :], rsn[:, :], AF.Exp, scale=-0.5)
                    if m < 2:
                        P.stt("dve", qk[:, m, :], qk[:, m, :], 0.125, rsn[:, :], ALU.mult, ALU.mult)
                    else:
                        P.tt("pool", qk[:, m, :], qk[:, m, :], rsn[:, :], ALU.mult)
                for j in range(4):
                    i = c * 4 + j
                    bank = pb[4 + j % 2]
                    self._proj_tm(wt, 768, 256, i, bank)
                    P.act(zsb[c % 2][:, j, :], bank[:, 0:256], AF.Silu)

            def run_rr(gens):
                gens = list(gens)
                while gens:
                    for g in list(gens):
                        try:
                            next(g)
                        except StopIteration:
                            gens.remove(g)

            prev = None
            for c in range(4):
                chunk_front(c)
                for j in range(4):
                    i = c * 4 + j
                    prep_common(i, qkv[c % 2])
                    gens = [head_chain(i, qkv[c % 2], h) for h in range(4)]
                    if prev is not None:
                        gens.append(scan(prev))
                    run_rr(gens)
                    prev = i
            run_rr([scan(prev)])


def _prep_inputs(inputs, n_layers=2):
    f = lambda k: np.ascontiguousarray(np.asarray(inputs[k], dtype=np.float32)[:n_layers])
    shared = {"w_in": f("w_in"), "w_gate": f("w_gate"), "w_branch": f("w_branch"), "w_out": f("w_out"),
              "w_up": f("w_up"), "w_down": f("w_down"), "consts": CONST_ARR,
              "lp": np.stack([_layer_params(inputs, l) for l in range(n_layers)])}
    return shared


def kernel(**inputs):
    x = np.ascontiguousarray(np.asarray(inputs["x"], dtype=np.float32))
    n_cores = 8
    per = x.shape[0] // n_cores
    shared = _prep_inputs(inputs)
    nc = K(n_seq=per).build()
    in_maps = []
    for c in range(n_cores):
        m = dict(shared)
        m["x"] = x[c * per:(c + 1) * per].reshape(per * T, D)
        in_maps.append(m)
    res = run_bass_kernel_spmd(nc, in_maps, core_ids=list(range(n_cores)))
    out = np.stack([np.asarray(r["out"]).reshape(per, T, D) for r in res.results], axis=0)
    return out.reshape(x.shape).astype(np.float32)
```

```python
import numpy as np
from contextlib import ExitStack, contextmanager
import concourse.bass as bass
import concourse.mybir as mybir
from concourse.bass_utils import run_bass_kernel_spmd

F32 = mybir.dt.float32
BF16 = mybir.dt.bfloat16
AF = mybir.ActivationFunctionType
ALU = mybir.AluOpType
AX = mybir.AxisListType

ENGINES = ("pe", "act", "dve", "pool", "sp")


class Res:
    __slots__ = ("name", "last_w", "readers", "slot", "t", "excl", "pe_last")

    def __init__(self, name, t=None, excl=False):
        self.name = name
        self.excl = excl
        self.pe_last = None
        self.last_w = None
        self.readers = []
        self.slot = None
        self.t = t

    def __getitem__(self, idx):
        return V(self, self.t[idx])


class V:
    __slots__ = ("res", "ap")

    def __init__(self, res, ap):
        self.res = res
        self.ap = ap

    def __getitem__(self, idx):
        return V(self.res, self.ap[idx])

    def r(self, pat, **kw):
        return V(self.res, self.ap.rearrange(pat, **kw))

    def un(self, axis):
        return V(self.res, self.ap.unsqueeze(axis))

    def bc(self, shape):
        return V(self.res, self.ap.broadcast_to(list(shape)))


def _ap(x):
    return x.ap if isinstance(x, V) else x


def _rs(*xs):
    return [x.res for x in xs if isinstance(x, V)]


class Op:
    __slots__ = ("eng", "fn", "seq", "waits", "signal", "is_dma", "slot", "cnt",
                 "clock", "semval", "kind")

    def __init__(self, eng, fn, is_dma=False, slot=None):
        self.eng = eng
        self.fn = fn
        self.is_dma = is_dma
        self.slot = slot
        self.waits = []
        self.signal = False
        self.clock = None
        self.semval = None
        self.cnt = 0


class Prog:
    def __init__(self, nc):
        self.nc = nc
        self.ops = {e: [] for e in ENGINES}
        self.clock = {e: {} for e in ENGINES}
        self.nslots = 0
        self.slot_last = {}
        self.slot_cnt = {}
        self.all_res = []
        self.stack = None
        self.n_wait = 0
        self.free_slots = []

    def sb(self, name, shape, dtype):
        self.uid = getattr(self, "uid", 0) + 1
        name = "%s_%d" % (name, self.uid)
        t = self.stack.enter_context(self.nc.sbuf_tensor(name, list(shape), dtype))
        r = Res(name, t)
        self.all_res.append(r)
        return r

    def ps(self, name, shape, dtype=F32):
        t = self.stack.enter_context(self.nc.psum_tensor(name, list(shape), dtype))
        r = Res(name, t, excl=True)
        self.all_res.append(r)
        return r

    def res(self, name):
        r = Res(name, None)
        self.all_res.append(r)
        return r

    def _key(self, op):
        return ("s", op.slot) if op.is_dma else op.eng

    def _val(self, op):
        return op.cnt if op.is_dma else op.seq

    def _add_dep(self, op, dep, raw, force=False):
        if dep is None or dep is op:
            return
        if not force and not dep.is_dma and not op.is_dma and dep.eng == op.eng:
            if op.eng == "pe":
                return
        k = self._key(dep)
        v = self._val(dep)
        ck = self.clock[op.eng]
        if ck.get(k, -1) >= v:
            return
        op.waits.append(dep)
        dep.signal = True
        for kk, vv in dep.clock.items():
            if ck.get(kk, -1) < vv:
                ck[kk] = vv
        ck[k] = v

    def add(self, eng, fn, reads=(), writes=(), is_dma=False, force_dep=None):
        op = Op(eng, fn, is_dma=is_dma)
        xr = [r for r in reads if r.excl]
        if xr:
            reads = [r for r in reads if not r.excl]
            writes = list(writes) + [r for r in xr if r not in writes]
        lst = self.ops[eng]
        op.seq = len(lst)
        if is_dma:
            tile = None
            for r in list(writes) + list(reads):
                if r.t is not None:
                    tile = r
                    break
            assert tile is not None, "dma needs an sbuf tile resource"
            if tile.slot is None:
                if self.free_slots:
                    tile.slot = self.free_slots.pop()
                else:
                    tile.slot = self.nslots
                    self.nslots += 1
            op.slot = tile.slot
            op.cnt = self.slot_cnt.get(op.slot, 0) + 1
            self.slot_cnt[op.slot] = op.cnt
        cand = []
        for r in reads:
            if r.last_w is not None:
                cand.append((r.last_w, True))
        for w in writes:
            if w.last_w is not None:
                cand.append((w.last_w, True))
            for rd in w.readers:
                cand.append((rd, False))
        cand.sort(key=lambda t: -self._val(t[0]))
        for d, raw in cand:
            self._add_dep(op, d, raw)
        if force_dep is not None:
            self._add_dep(op, force_dep, True, force=True)
        for r in reads:
            r.readers.append(op)
        for w in writes:
            w.last_w = op
            w.readers = []
        ck = dict(self.clock[eng])
        if not is_dma:
            ck[eng] = op.seq
        else:
            ck[("s", op.slot)] = op.cnt
        op.clock = ck
        lst.append(op)
        return op

    def dma(self, q, out, in_, reads=(), writes=()):
        o, i = _ap(out), _ap(in_)
        return self.add(q, lambda e: e.dma_start(out=o, in_=i), list(reads) + _rs(in_), list(writes) + _rs(out), is_dma=True)

    def _pe_rowgroup_dep(self, out, stat):
        bp = stat.ap.base_partition()
        bp = bp() if callable(bp) else bp
        k = stat.ap.shape[0]
        groups = set(range(bp // 32, (bp + k + 31) // 32))
        prev = out.res.pe_last
        force = prev[0] if (prev is not None and prev[1].isdisjoint(groups)) else None
        return groups, force

    def mm(self, out, lhsT, rhs, start=True, stop=True):
        o, l, r = out.ap, lhsT.ap, rhs.ap
        groups, force = self._pe_rowgroup_dep(out, lhsT)
        op = self.add("pe", lambda e: e.matmul(o, lhsT=l, rhs=r, start=start, stop=stop), _rs(lhsT, rhs), _rs(out), force_dep=force)
        out.res.pe_last = (op, groups)
        return op

    def tr(self, out, in_, ident):
        o, i, d = out.ap, in_.ap, ident.ap
        groups, force = self._pe_rowgroup_dep(out, in_)
        op = self.add("pe", lambda e: e.transpose(out=o, in_=i, identity=d), _rs(in_, ident), _rs(out), force_dep=force)
        out.res.pe_last = (op, groups)
        return op

    def act(self, out, in_, func, bias=0.0, scale=1.0, accum=None):
        o, i, b, sc = out.ap, in_.ap, _ap(bias), _ap(scale)
        if accum is None:
            return self.add("act", lambda e: e.activation(out=o, in_=i, func=func, bias=b, scale=sc), _rs(in_, bias, scale), _rs(out))
        a = accum.ap
        return self.add("act", lambda e: e.activation(out=o, in_=i, func=func, bias=b, scale=sc, accum_out=a), _rs(in_, bias, scale), _rs(out, accum))

    def tt(self, eng, out, in0, in1, op):
        o, a, b = out.ap, in0.ap, in1.ap
        return self.add(eng, lambda e: e.tensor_tensor(out=o, in0=a, in1=b, op=op), _rs(in0, in1), _rs(out))

    def ts(self, eng, out, in0, s1, op0, s2=None, op1=None):
        o, a, x1, x2 = out.ap, in0.ap, _ap(s1), _ap(s2)
        if op1 is None:
            return self.add(eng, lambda e: e.tensor_scalar(out=o, in0=a, scalar1=x1, scalar2=None, op0=op0), _rs(in0, s1), _rs(out))
        return self.add(eng, lambda e: e.tensor_scalar(out=o, in0=a, scalar1=x1, scalar2=x2, op0=op0, op1=op1), _rs(in0, s1, s2), _rs(out))

    def stt(self, eng, out, in0, scalar, in1, op0, op1):
        o, a, sc, b = out.ap, in0.ap, _ap(scalar), in1.ap
        return self.add(eng, lambda e: e.scalar_tensor_tensor(out=o, in0=a, scalar=sc, in1=b, op0=op0, op1=op1), _rs(in0, scalar, in1), _rs(out))

    def copy(self, eng, out, in_):
        o, i = out.ap, in_.ap
        if eng == "act":
            return self.add("act", lambda e: e.copy(out=o, in_=i), _rs(in_), _rs(out))
        return self.add(eng, lambda e: e.tensor_copy(out=o, in_=i), _rs(in_), _rs(out))

    def memset(self, eng, out, val):
        o = out.ap
        return self.add(eng, lambda e: e.memset(o, val), [], _rs(out))

    def reduce(self, eng, out, in_, op=None):
        o, i = out.ap, in_.ap
        op = op or ALU.add
        return self.add(eng, lambda e: e.tensor_reduce(out=o, in_=i, axis=AX.X, op=op), _rs(in_), _rs(out))

    def max8(self, out, in_):
        o, i = out.ap, in_.ap
        return self.add("dve", lambda e: e.max(out=o, in_=i), _rs(in_), _rs(out))

    def recip(self, out, in_):
        o, i = out.ap, in_.ap
        return self.add("dve", lambda e: e.reciprocal(out=o, in_=i), _rs(in_), _rs(out))

    def barrier(self):
        lasts = []
        for e in ENGINES:
            for op in reversed(self.ops[e]):
                if not op.is_dma and op.fn is not None:
                    lasts.append(op)
                    break
        dmas = [op for e in ENGINES for op in self.ops[e] if op.is_dma and self.slot_cnt[op.slot] == op.cnt]
        for e in ENGINES:
            op = Op(e, None)
            op.seq = len(self.ops[e])
            for d in lasts:
                if d.eng != e:
                    self._add_dep(op, d, True)
            for d in dmas:
                self._add_dep(op, d, True)
            ck = dict(self.clock[e])
            op.clock = ck
            op.seq = -1
            self.ops[e].append(op)

    @contextmanager
    def scope(self):
        old = self.stack
        mark = len(self.all_res)
        with ExitStack() as st:
            self.stack = st
            yield
            self.barrier()
            for r in self.all_res[mark:]:
                if r.slot is not None:
                    self.free_slots.append(r.slot)
                    r.slot = None
            del self.all_res[mark:]
        self.stack = old

    def emit(self, final_wait_ops=()):
        nc = self.nc
        from contextlib import ExitStack
        with ExitStack() as st:
            esem = {e: st.enter_context(nc.semaphore("sem_" + e)) for e in ENGINES}
            ssem = {s: st.enter_context(nc.semaphore("dsem%d" % s)) for s in range(self.nslots)}
            for e in ENGINES:
                v = 0
                for op in self.ops[e]:
                    if op.fn is None or op.is_dma:
                        continue
                    if op.signal:
                        v += 1
                    op.semval = v
            block = st.enter_context(nc.Block())

            def run(e, eng):
                for op in self.ops[e]:
                    for d in op.waits:
                        if d.is_dma:
                            eng.wait_ge(ssem[d.slot], 16 * d.cnt)
                        else:
                            eng.wait_ge(esem[d.eng], d.semval)
                        self.n_wait += 1
                    if op.fn is None:
                        continue
                    ins = op.fn(eng)
                    if op.is_dma:
                        ins.then_inc(ssem[op.slot], 16)
                    elif op.signal:
                        ins.then_inc(esem[e], 1)

            @block.tensor
            def _(eng):
                run("pe", eng)

            @block.scalar
            def _(eng):
                run("act", eng)

            @block.vector
            def _(eng):
                run("dve", eng)

            @block.gpsimd
            def _(eng):
                run("pool", eng)

            @block.sync
            def _(eng):
                run("sp", eng)

D = 1024
T = 2048
NH = 4
HD = 64
BW = 256
DFF = 4096
OFF_GDN, OFF_MOBA, OFF_SB, OFF_SSD, INW = 0, 1032, 1800, 2568, 3596
EPS = 1e-6
NEG = -30000.0


def _const_table():
    p = np.arange(128)[:, None]
    f = np.arange(128)[None, :]
    c = {}
    c["ident"] = (p == f)
    c["ones"] = np.ones((128, 128))
    c["blk64"] = (p // 64 == f // 64)
    c["tri128"] = (p <= f)
    c["neg_ssd"] = np.where(f >= p, 0.0, NEG)
    same = (p // 64 == f // 64)
    c["tri_bd"] = same & (p <= f)
    c["neg_incl_bd"] = np.where(same & (f >= p), 0.0, NEG)
    c["neg_strict_bd"] = np.where(same & (f > p), 0.0, NEG)
    c["m_strict"] = (f > p)
    c["m_incl"] = (f >= p)
    c["u_incl"] = (p >= f)
    i = np.arange(16)[:, None, None]
    n = np.arange(8)[None, None, :]
    h = np.arange(4)[None, :, None]
    past = np.broadcast_to(n < i // 2, (16, 4, 8))
    own = np.broadcast_to(n == i // 2, (16, 4, 8))
    slopes = np.array([2.0 ** (-8.0 * (k + 1) / 4) for k in range(4)])
    dl = np.arange(16)[None, None, :] - 12
    c["alibi"] = (slopes[None, :, None] * (dl * 128 + np.arange(128)[:, None, None])).reshape(128, 64)
    v = np.zeros((128, 32))
    for r in range(32):
        v[r, r] = 1.0
    for hh in range(4):
        v[32, hh * 8:(hh + 1) * 8] = slopes[hh]
        v[33, hh * 8:(hh + 1) * 8] = slopes[hh]
    c["selv"] = v
    t = np.arange(512)
    a = np.zeros((128, 512))
    a[32] = -128.0 * (t // 128)
    a[33] = -(t % 128).astype(np.float64)
    c["augrows"] = a
    c["pastbias"] = np.broadcast_to(np.where(past, 0.0, -1e30).reshape(1, 512), (128, 512))
    c["pastm"] = np.broadcast_to(past.reshape(1, 512).astype(np.float64), (128, 512))
    c["ownm"] = np.broadcast_to(own.reshape(1, 512).astype(np.float64), (128, 512))
    offs = {}
    cols = []
    o = 0
    for k, val in c.items():
        val = np.asarray(val, dtype=np.float32)
        offs[k] = (o, val.shape[1])
        cols.append(val)
        o += val.shape[1]
    return np.ascontiguousarray(np.concatenate(cols, axis=1)), offs


CONST_ARR, CONST_OFF = _const_table()
NCONST = CONST_ARR.shape[1]
NCONST_RES = CONST_OFF["pastbias"][0]

LP_OFF = {}
_o = 0
for _k, _w in (("g_mix_post", 1024), ("g_ffn_post", 1024), ("gdn_conv", 24), ("ssd_conv", 24), ("ssd_cb", 6),
               ("gdn_alog", 4), ("gdn_dtb", 4), ("ssd_alog", 4), ("ssd_dtb", 4), ("ssd_d", 4),
               ("gdn_norm", 64), ("ssd_norm", 256), ("rowg", 16)):
    LP_OFF[_k] = (_o, _w)
    _o += _w
NLP = _o


def _layer_params(inp, l):
    lp = np.zeros((128, NLP), np.float32)

    def put(k, arr):
        o, w = LP_OFF[k]
        lp[:, o:o + w] = arr
    put("g_mix_post", np.broadcast_to(inp["norm_mix_post"][l][None, :], (128, 1024)))
    put("g_ffn_post", np.broadcast_to(inp["norm_ffn_post"][l][None, :], (128, 1024)))
    put("gdn_conv", inp["gdn_conv"][l].reshape(4, 6, 128).transpose(2, 1, 0).reshape(128, 24))
    put("ssd_conv", inp["ssd_conv"][l].reshape(4, 6, 128).transpose(2, 1, 0).reshape(128, 24))
    put("ssd_cb", inp["ssd_conv_bias"][l].reshape(6, 128).T)
    for k, src in (("gdn_alog", "gdn_a_log"), ("gdn_dtb", "gdn_dt_bias"), ("ssd_alog", "ssd_a_log"),
                   ("ssd_dtb", "ssd_dt_bias"), ("ssd_d", "ssd_d")):
        put(k, np.broadcast_to(inp[src][l][None, :], (128, 4)))
    put("gdn_norm", np.broadcast_to(inp["gdn_norm"][l][None, :], (128, 64)))
    put("ssd_norm", np.broadcast_to(inp["ssd_norm"][l][None, :], (128, 256)))
    rg = np.concatenate([inp["norm_mix_pre"][l].reshape(8, 128).T, inp["norm_ffn_pre"][l].reshape(8, 128).T], axis=1)
    put("rowg", rg)
    return lp


class K:
    def __init__(self, n_seq=4, n_layers=2, units=("gdn", "moba", "sb", "ssd", "merge"), dbg=None):
        self.n_seq, self.n_layers, self.units, self.dbg = n_seq, n_layers, units, (dbg or {})
        nc = bass.Bass("TRN2", target_bir_lowering=False)
        self.nc = nc
        self.P = Prog(nc)
        NL = n_layers
        di = lambda name, shape, dt=F32: nc.dram_tensor(name, list(shape), dt, kind="ExternalInput").ap()
        dn = lambda name, shape, dt=BF16: nc.dram_tensor(name, list(shape), dt).ap()
        self.x = di("x", [n_seq * T, D])
        self.w_in = di("w_in", [NL, D, INW])
        self.w_gate = di("w_gate", [NL, 4, D, D])
        self.w_branch = di("w_branch", [NL, 4, BW, D])
        self.w_out = di("w_out", [NL, D, D])
        self.w_up = di("w_up", [NL, D, DFF])
        self.w_down = di("w_down", [NL, DFF, D])
        self.consts = di("consts", [128, NCONST])
        self.lpd = di("lp", [NL, 128, NLP])
        self.out = nc.dram_tensor("out", [n_seq * T, D], F32, kind="ExternalOutput").ap()
        self.xmid = dn("xmid", [n_seq * T, D], F32)
        self.wb_in = dn("wb_in", [NL, D, INW])
        self.wb_gate = dn("wb_gate", [NL, 8, 128, 4, 8, 128])
        self.wb_br = dn("wb_br", [NL, 8, 128, 4, 2, 128])
        self.wb_out = dn("wb_out", [NL, D, D])
        self.wb_up = dn("wb_up", [NL, 32, 128, 8, 128])
        self.wb_down = dn("wb_down", [NL, DFF, D])
        if "inject_yT" in self.dbg:
            self.dbg_yT = di("dbg_yT", [4, BW, T])
        if "dump_yT" in self.dbg:
            self.dbg_yT_out = nc.dram_tensor("dbg_yT_out", [4, BW, T], F32, kind="ExternalOutput").ap()
        if "dump_hT" in self.dbg:
            self.dbg_hT_out = nc.dram_tensor("dbg_hT_out", [D, T], F32, kind="ExternalOutput").ap()
        self._q = 0

    def q(self):
        return "sp"

    def c(self, name):
        o, w = CONST_OFF[name]
        if o >= NCONST_RES:
            return self.mcst[:, o - NCONST_RES:o - NCONST_RES + w]
        return self.cst[:, o:o + w]

    def lpv(self, name):
        o, w = LP_OFF[name]
        return self.lp[:, o:o + w]

    def build(self):
        P = self.P
        with ExitStack() as st:
            P.stack = st
            self.pb = [P.ps("pb%d" % i, [128, 512], F32) for i in range(8)]
            self.cst = P.sb("cst", [128, NCONST_RES], F32)
            self.lp = P.sb("lp_sb", [128, NLP], F32)
            self.ones_bf = P.sb("ones_bf", [128, 128], BF16)
            self.uincl_bf = P.sb("uincl_bf", [128, 128], BF16)
            self.sel_bf = P.sb("sel_bf", [128, 32, 128], BF16)
            self.hT = P.sb("hT", [128, 8, T], BF16)
            self.yT = [P.sb("yT%d" % g, [128, 2, T], BF16) for g in range(4)]
            P.dma("sp", self.cst[:, :], self.consts[:, 0:NCONST_RES])
            P.copy("dve", self.ones_bf[:, :], self.c("ones"))
            P.copy("dve", self.uincl_bf[:, :], self.c("u_incl"))
            P.copy("pool", self.sel_bf[0:34, :, :], self.c("selv")[0:34, :].un(2).bc([34, 32, 128]))
            if "dump_yT" in self.dbg:
                for g in range(4):
                    P.memset("pool", self.yT[g][:, :, :], 0.0)
            for l in range(self.n_layers):
                P.dma("sp", self.lp[:, :], self.lpd[l, :, :])
                if "noconvert" not in self.dbg:
                    self.convert_weights(l)
                xsrc = self.x if l == 0 else self.xmid
                xdst = self.out if l == self.n_layers - 1 else self.xmid
                for s in range(self.n_seq):
                    self.u_norm(l, s, xsrc)
                    if "dump_hT" in self.dbg:
                        self.dump_hT()
                    if "inject_yT" in self.dbg:
                        self.inject_yT()
                    if "sb" in self.units:
                        self.u_sb(l, s)
                    if "moba" in self.units:
                        self.u_moba(l, s)
                    if "ssd" in self.units:
                        self.u_ssd(l, s)
                    if "gdn" in self.units:
                        self.u_gdn(l, s)
                    if "dump_yT" in self.dbg:
                        self.dump_yT()
                    if "merge" in self.units:
                        self.u_merge(l, s, xsrc, xdst)
            P.barrier()
            P.emit()
        return self.nc

    def dump_hT(self):
        P = self.P
        with P.scope():
            t = P.sb("dh", [128, 8, T], F32)
            P.copy("dve", t[:, :, :], self.hT[:, :, :])
            P.dma("sp", self.dbg_hT_out.rearrange("(k p) t -> p k t", p=128), t[:, :, :])

    def dump_yT(self):
        P = self.P
        with P.scope():
            for g in range(4):
                t = P.sb("dy%d" % g, [128, 2, T], F32)
                P.copy("dve", t[:, :, :], self.yT[g][:, :, :])
                P.dma("sp", self.dbg_yT_out[g].rearrange("(k p) t -> p k t", p=128), t[:, :, :])

    def inject_yT(self):
        P = self.P
        with P.scope():
            for g in range(4):
                t = P.sb("iy%d" % g, [128, 2, T], F32)
                P.dma("sp", t[:, :, :], self.dbg_yT[g].rearrange("(k p) t -> p k t", p=128))
                P.copy("dve", self.yT[g][:, :, :], t[:, :, :])

    def convert_weights(self, l):
        P = self.P
        with P.scope():
            st32 = [P.sb("cv32_%d" % i, [128, 4096], F32) for i in range(2)]
            st16 = [P.sb("cv16_%d" % i, [128, 4096], BF16) for i in range(2)]
            cnt = [0]
            rowg = self.lpv("rowg")

            def cv(src, w, scales, stores):
                b = cnt[0] % 2
                cnt[0] += 1
                s32, s16 = st32[b], st16[b]
                P.dma(self.q(), s32[:, 0:w] if len(src.shape) == 2 else s32[:, 0:w].r("p (k c) -> p k c", k=src.shape[1]), src)
                eng = "dve"
                if scales is None:
                    P.copy(("act", "act", "dve")[cnt[0] % 3], s16[:, 0:w], s32[:, 0:w])
                else:
                    for (a, bnd, col) in scales:
                        P.ts(eng, s16[:, a:bnd], s32[:, a:bnd], rowg[:, col:col + 1], ALU.mult)
                for (dst, a, bnd, pat, kw) in stores:
                    v = s16[:, a:bnd]
                    if pat:
                        v = v.r(pat, **kw)
                    P.dma(self.q(), dst, v)

            for kt in range(8):
                cv(self.w_in[l, kt * 128:(kt + 1) * 128, :], INW, [(0, INW, kt)],
                   [(self.wb_in[l, kt * 128:(kt + 1) * 128, :], 0, INW, None, None)])
            for g in range(4):
                for half in range(2):
                    src = self.w_gate[l, g, half * 512:(half + 1) * 512, :].rearrange("(k p) c -> p k c", p=128)
                    cv(src, 4096, [(k * 1024, (k + 1) * 1024, half * 4 + k) for k in range(4)],
                       [(self.wb_gate[l, :, :, g, half * 4 + k, :].rearrange("c p j -> p c j"), k * 1024, (k + 1) * 1024,
                         "p (c j) -> p c j", dict(j=128)) for k in range(4)])
            for g in range(4):
                src = self.w_branch[l, g].rearrange("(k p) c -> p k c", p=128)
                cv(src, 2048, None,
                   [(self.wb_br[l, :, :, g, k, :].rearrange("c p j -> p c j"), k * 1024, (k + 1) * 1024,
                     "p (c j) -> p c j", dict(j=128)) for k in range(2)])
            for half in range(2):
                src = self.w_out[l, half * 512:(half + 1) * 512, :].rearrange("(k p) c -> p k c", p=128)
                cv(src, 4096, None,
                   [(self.wb_out[l, half * 512:(half + 1) * 512, :].rearrange("(k p) c -> p k c", p=128), 0, 4096,
                     "p (k c) -> p k c", dict(k=4))])
            for kt in range(8):
                cv(self.w_up[l, kt * 128:(kt + 1) * 128, :], 4096, [(0, 4096, 8 + kt)],
                   [(self.wb_up[l, f0:f0 + 8, :, kt, :].rearrange("f p j -> p f j"), f0 * 128, (f0 + 8) * 128,
                     "p (f j) -> p f j", dict(j=128)) for f0 in range(0, 32, 8)])
            for qd in range(8):
                src = self.w_down[l, qd * 512:(qd + 1) * 512, :].rearrange("(k p) c -> p k c", p=128)
                cv(src, 4096, None,
                   [(self.wb_down[l, qd * 512:(qd + 1) * 512, :].rearrange("(k p) c -> p k c", p=128), 0, 4096,
                     "p (k c) -> p k c", dict(k=4))])

    def u_norm(self, l, s, xsrc):
        P = self.P
        ident = self.c("ident")
        with P.scope():
            NB = 4
            xt = [P.sb("nx%d" % i, [128, D], F32) for i in range(NB)]
            hh = [P.sb("nh%d" % i, [128, D], F32) for i in range(NB)]
            junk = P.sb("njunk", [128, D], BF16)
            ss = [P.sb("nss%d" % i, [128, 1], F32) for i in range(NB)]
            rs = [P.sb("nrs%d" % i, [128, 1], F32) for i in range(NB)]
            for i in range(16):
                b = i % NB
                r0 = s * T + i * 128
                P.dma(self.q(), xt[b][:, :], xsrc[r0:r0 + 128, :])
                P.act(junk[:, :], xt[b][:, :], AF.Square, accum=ss[b][:, :])
                P.act(rs[b][:, :], ss[b][:, :], AF.Ln, bias=EPS, scale=1.0 / D)
                P.act(rs[b][:, :], rs[b][:, :], AF.Exp, scale=-0.5)
                P.ts("dve", hh[b][:, :], xt[b][:, :], rs[b][:, 0:1], ALU.mult)
                for g in range(2):
                    bank = self.pb[(2 * i + g) % 8]
                    for k in range(4):
                        kk = g * 4 + k
                        P.tr(bank[:, k * 128:(k + 1) * 128], hh[b][:, kk * 128:(kk + 1) * 128], ident)
                    P.copy("act" if g == 0 else "dve", self.hT[:, g * 4:(g + 1) * 4, i * 128:(i + 1) * 128],
                           bank[:, :].r("p (k n) -> p k n", k=4))

    def u_merge(self, l, s, xsrc, xdst):
        P = self.P
        ident = self.c("ident")
        g_post, g_fpost = self.lpv("g_mix_post"), self.lpv("g_ffn_post")
        pb = self.pb
        with P.scope():
            ws = [P.sb("mw%d" % i, [128, 4096], BF16) for i in range(2)]
            wgs = [P.sb("mwg%d" % i, [128, 8, 128], BF16) for i in range(3)]
            wbr = [P.sb("mwb%d" % i, [128, 4, 2, 128], BF16) for i in range(2)]
            xs = P.sb("mx", [128, 4, D], F32)
            acc = P.sb("macc", [128, 512], F32)
            sg = [P.sb("msg%d" % i, [128, 512], F32) for i in range(2)]
            mTs = [P.sb("mmT%d" % i, [128, 8, 512], BF16) for i in range(2)]
            junk = P.sb("mjunk", [128, D], BF16)
            tmp = P.sb("mtmp", [128, D], F32)
            h2T = P.sb("mh2T", [128, 8, 512], BF16)
            aT = P.sb("maT", [128, 32, 512], BF16)
            rl = [P.sb("mrl%d" % i, [128, 512], F32) for i in range(2)]
            ss = P.sb("mss", [128, 8], F32)
            rs = P.sb("mrs", [128, 4], F32)
            wi = [0]

            def wload(src, pat=None, **kw):
                t = ws[wi[0] % 2]
                wi[0] += 1
                P.dma("sp", t[:, :].r(pat, **kw) if pat else t[:, :], src)
                return t

            def gates(tb):
                t0 = tb * 512
                mT = mTs[tb % 2]
                gi = [0]

                def gload(cc, g):
                    t = wgs[gi[0] % 3]
                    gi[0] += 1
                    P.dma("sp", t[:, :, :], self.wb_gate[l, cc, :, g])
                    return t
                nxt = gload(0, 0)
                for cc in range(8):
                    wb_ = wbr[cc % 2]
                    P.dma("sp", wb_[:, :, :, :], self.wb_br[l, cc])
                    for g in range(4):
                        wg = nxt
                        if not (cc == 7 and g == 3):
                            nxt = gload(cc + (g + 1) // 4, (g + 1) % 4)
                        gp, bp = pb[(2 * g) % 4], pb[(2 * g + 1) % 4]
                        for k in range(8):
                            P.mm(gp[:, :], wg[:, k, :], self.hT[:, k, t0:t0 + 512],
                                 start=(k == 0), stop=(k == 7))
                        for k in range(2):
                            P.mm(bp[:, :], wb_[:, g, k, :], self.yT[g][:, k, t0:t0 + 512], start=(k == 0), stop=(k == 1))
                        sgt = sg[g % 2]
                        P.act(sgt[:, :], gp[:, :], AF.Sigmoid)
                        if g == 0:
                            P.tt("dve", acc[:, :], sgt[:, :], bp[:, :], ALU.mult)
                        else:
                            P.tt("dve", sgt[:, :], sgt[:, :], bp[:, :], ALU.mult)
                            if g < 3:
                                P.tt("pool", acc[:, :], acc[:, :], sgt[:, :], ALU.add)
                            else:
                                P.tt("pool", mT[:, cc, :], acc[:, :], sgt[:, :], ALU.add)
                        yield

            def rest(tb):
                t0 = tb * 512
                r0 = s * T + t0
                mT = mTs[tb % 2]
                P.dma("sp", xs[:, :, :], xsrc[r0:r0 + 512, :].rearrange("(j p) c -> p j c", p=128))
                for jp in range(2):
                    for half in range(2):
                        wo = wload(self.wb_out[l, :, half * 512:(half + 1) * 512].rearrange("(k p) c -> p k c", p=128),
                                   "p (k c) -> p k c", k=8)
                        for j in (2 * jp, 2 * jp + 1):
                            bank = pb[4 + half * 2 + j % 2]
                            for k in range(8):
                                P.mm(bank[:, :], mT[:, k, j * 128:(j + 1) * 128], wo[:, k * 512:(k + 1) * 512],
                                     start=(k == 0), stop=(k == 7))
                    yield
                    for j in (2 * jp, 2 * jp + 1):
                        bk = [pb[4 + half * 2 + j % 2] for half in range(2)]
                        for half in range(2):
                            P.act(junk[:, half * 512:(half + 1) * 512], bk[half][:, :], AF.Square, accum=ss[:, half:half + 1])
                        P.tt("dve", ss[:, 2:3], ss[:, 0:1], ss[:, 1:2], ALU.add)
                        yield
                        P.act(rs[:, 0:1], ss[:, 2:3], AF.Ln, bias=EPS, scale=1.0 / D)
                        P.act(rs[:, 0:1], rs[:, 0:1], AF.Exp, scale=-0.5)
                        yield
                        for half in range(2):
                            cs = slice(half * 512, (half + 1) * 512)
                            P.stt("dve", tmp[:, cs], bk[half][:, :], rs[:, 0:1], g_post[:, cs], ALU.mult, ALU.mult)
                        yield
                        P.tt("pool", xs[:, j, :], xs[:, j, :], tmp[:, :], ALU.add)
                        P.act(junk[:, :], xs[:, j, :], AF.Square, accum=ss[:, 3:4])
                        yield
                        P.act(rs[:, 1:2], ss[:, 3:4], AF.Ln, bias=EPS, scale=1.0 / D)
                        P.act(rs[:, 1:2], rs[:, 1:2], AF.Exp, scale=-0.5)
                        yield
                        P.ts("dve", tmp[:, :], xs[:, j, :], rs[:, 1:2], ALU.mult)
                        yield
                        for g in range(2):
                            bank = bk[g]
                            for k in range(4):
                                kk = g * 4 + k
                                P.tr(bank[:, k * 128:(k + 1) * 128], tmp[:, kk * 128:(kk + 1) * 128], ident)
                            P.copy("act" if g == 0 else "dve", h2T[:, g * 4:(g + 1) * 4, j * 128:(j + 1) * 128],
                                   bank[:, :].r("p (k n) -> p k n", k=4))
                        yield
                for f0 in range(0, 32, 4):
                    wu = wload(self.wb_up[l, f0:f0 + 4].rearrange("f p k j -> p f (k j)"), "p (f c) -> p f c", f=4)
                    for fi in range(4):
                        f = f0 + fi
                        bank = pb[4 + f % 4]
                        for k in range(8):
                            P.mm(bank[:, :], wu[:, (fi * 8 + k) * 128:(fi * 8 + k + 1) * 128], h2T[:, k, :],
                                 start=(k == 0), stop=(k == 7))
                        r = rl[f % 2]
                        P.act(r[:, :], bank[:, :], AF.Relu)
                        P.tt("pool", aT[:, f, :], r[:, :], r[:, :], ALU.mult)
                    yield

            def down(tb):
                for k0 in range(0, 32, 4):
                    wd = wload(self.wb_down[l, k0 * 128:(k0 + 4) * 128, :].rearrange("(k p) c -> p k c", p=128), "p (k c) -> p k c", k=4)
                    for ki in range(4):
                        k = k0 + ki
                        for j in range(4):
                            for half in range(2):
                                P.mm(pb[2 * j + half][:, :], aT[:, k, j * 128:(j + 1) * 128],
                                     wd[:, ki * 1024 + half * 512:ki * 1024 + (half + 1) * 512],
                                     start=(k == 0), stop=(k == 31))

            def fnorm(tb):
                r0 = s * T + tb * 512
                for j in range(4):
                    for half in range(2):
                        P.act(junk[:, half * 512:(half + 1) * 512], pb[2 * j + half][:, :], AF.Square,
                              accum=ss[:, 4 + half:5 + half])
                    P.tt("dve", ss[:, 6:7], ss[:, 4:5], ss[:, 5:6], ALU.add)
                    yield
                    P.act(rs[:, 2:3], ss[:, 6:7], AF.Ln, bias=EPS, scale=1.0 / D)
                    P.act(rs[:, 2:3], rs[:, 2:3], AF.Exp, scale=-0.5)
                    yield
                    for half in range(2):
                        cs = slice(half * 512, (half + 1) * 512)
                        P.stt("dve", tmp[:, cs], pb[2 * j + half][:, :], rs[:, 2:3], g_fpost[:, cs], ALU.mult, ALU.mult)
                    yield
                    P.tt("pool", xs[:, j, :], xs[:, j, :], tmp[:, :], ALU.add)
                    yield
                P.dma("sp", xdst[r0:r0 + 512, :].rearrange("(j p) c -> p j c", p=128), xs[:, :, :])

            def step(g):
                try:
                    next(g)
                    return True
                except StopIteration:
                    return False

            def run_rr(gens):
                gens = list(gens)
                while gens:
                    for g in list(gens):
                        if not step(g):
                            gens.remove(g)

            run_rr([gates(0)])
            pend = gates(1)
            for tb in range(4):
                run_rr([rest(tb)] + ([pend] if pend is not None else []))
                down(tb)
                fn = fnorm(tb)
                pend = gates(tb + 2) if tb + 2 < 4 else None
                alive, nfn = True, 0
                while alive:
                    alive = step(fn)
                    nfn += 1
                    if nfn >= 7 and pend is not None and not step(pend):
                        pend = None

    def _proj_fm(self, wt, col0, out_writer, nchunks=4):
        P = self.P
        for c in range(nchunks):
            bank = self.pb[self._pj % 4]
            self._pj += 1
            for k in range(8):
                P.mm(bank[:, :], wt[:, k, col0:col0 + 128], self.hT[:, k, c * 512:(c + 1) * 512], start=(k == 0), stop=(k == 7))
            out_writer(c, bank[:, :])

    def _proj_tm(self, wt, col0, ncol, i, bank):
        P = self.P
        for k in range(8):
            P.mm(bank[:, 0:ncol], self.hT[:, k, i * 128:(i + 1) * 128], wt[:, k, col0:col0 + ncol], start=(k == 0), stop=(k == 7))

    def u_sb(self, l, s):
        P = self.P
        pb = self.pb
        self._pj = 0
        with P.scope():
            wt = P.sb("sw", [128, 8, 768], BF16)
            qT = P.sb("sq", [128, 2, T], BF16)
            kT = P.sb("sk", [128, 2, T], BF16)
            vv = P.sb("sv", [128, 16, 256], BF16)
            NS = 4
            E = [P.sb("sE%d" % i, [128, 512], F32) for i in range(NS)]
            Lp = [P.sb("sL%d" % i, [128, 512], BF16) for i in range(NS)]
            G = [P.sb("sG%d" % i, [128, 512], F32) for i in range(NS)]
            Wt = [P.sb("sW%d" % i, [128, 512], BF16) for i in range(NS)]
            Pacc = [P.sb("sPacc%d" % i, [128, 512], BF16) for i in range(NS)]
            P.dma("sp", wt[:, :, :], self.wb_in[l, :, OFF_SB:OFF_SB + 768].rearrange("(k p) c -> p k c", p=128))
            for hp in range(2):
                self._proj_fm(wt, hp * 128, lambda c, ps, hp=hp: P.ts("dve", qT[:, hp, c * 512:(c + 1) * 512], ps, 0.125, ALU.mult))
                self._proj_fm(wt, 256 + hp * 128, lambda c, ps, hp=hp: P.copy("act", kT[:, hp, c * 512:(c + 1) * 512], ps))
            for i in range(16):
                bank = pb[4 + i % 2]
                self._proj_tm(wt, 512, 256, i, bank)
                P.copy("act" if i % 2 else "dve", vv[:, i, :], bank[:, 0:256])

            def stream(h, qb, sl):
                hp, r = h // 2, slice((h % 2) * 64, (h % 2) * 64 + 64)
                q0 = qb * 512
                wb_, accb = pb[sl], pb[4 + sl]
                Es, Ls, Gs, Ws, Pa = E[sl], Lp[sl], G[sl], Wt[sl], Pacc[sl]
                P.memset("pool", Pa[:, :], 0.0)
                nk = 4 * qb + 4
                for kt in range(nk - 1, -1, -1):
                    c0 = max(0, kt - 4 * qb) * 128
                    first = (kt == nk - 1)
                    P.mm(wb_[:, c0:512], kT[r, hp, kt * 128:(kt + 1) * 128], qT[r, hp, q0 + c0:q0 + 512])
                    yield
                    P.act(Es[:, c0:512], wb_[:, c0:512], AF.Exp)
                    yield
                    if kt >= 4 * qb:
                        P.tt("pool", Es[:, c0:c0 + 128], Es[:, c0:c0 + 128], self.c("m_strict"), ALU.mult)
                        yield
                    P.act(Ls[:, c0:512], Es[:, c0:512], AF.Ln, bias=1.0)
                    yield
                    P.mm(wb_[:, c0:512], self.uincl_bf[:, :], Ls[:, c0:512], start=True, stop=first)
                    if not first:
                        P.mm(wb_[:, c0:512], self.ones_bf[:, :], Pa[:, c0:512], start=False, stop=True)
                    yield
                    P.act(Gs[:, c0:512], wb_[:, c0:512], AF.Exp, scale=-1.0)
                    if c0 > 0:
                        P.memset("pool", Ws[:, 0:c0], 0.0)
                    yield
                    P.tt("dve", Ws[:, c0:512], Es[:, c0:512], Gs[:, c0:512], ALU.mult)
                    if kt > 0:
                        P.tt("pool", Pa[:, c0:512], Pa[:, c0:512], Ls[:, c0:512], ALU.add)
                    yield
                    P.mm(accb[:, :], vv[:, kt, hp * 128:(hp + 1) * 128], Ws[:, :], start=first, stop=(kt == 0))
                    yield
                P.copy("act", self.yT[2][r, hp, q0:q0 + 512], accb[r, :])
                yield

            todo = sorted([(h, qb) for h in range(4) for qb in range(4)], key=lambda t: -t[1])
            active = {}
            while todo or active:
                for sl in range(NS):
                    if sl not in active and todo:
                        h, qb = todo.pop(0)
                        active[sl] = stream(h, qb, sl)
                for sl in list(active):
                    try:
                        next(active[sl])
                    except StopIteration:
                        del active[sl]

    def u_moba(self, l, s):
        P = self.P
        pb = self.pb
        self._pj = 0
        ident = self.c("ident")
        with P.scope():
            wt = P.sb("bw", [128, 8, 768], BF16)
            q32 = P.sb("bq32", [128, 2, T], F32)
            qT = P.sb("bq", [128, 2, T], BF16)
            kT = P.sb("bk", [128, 2, T], BF16)
            vv = P.sb("bv", [128, 4, 16, 128], BF16)
            ksum = P.sb("bks", [128, 2, 8], F32)
            gm = P.sb("bgm", [128, 64, 8], F32)
            thr = P.sb("bthr", [128, 64, 8], F32)
            sel = P.sb("bsel", [128, 512], F32)
            aug = P.sb("baug", [128, T], BF16)
            NS = 4
            Pt = [P.sb("bP%d" % i, [128, 512], BF16) for i in range(NS)]
            rden = [P.sb("brd%d" % i, [128, 512], F32) for i in range(NS)]
            P.memset("pool", vv[:, :, :, :], 1.0)
            self.mcst = P.sb("bmcst", [128, NCONST - NCONST_RES], F32)
            P.dma("sp", self.mcst[:, :], self.consts[:, NCONST_RES:NCONST])
            P.dma("sp", wt[:, :, :], self.wb_in[l, :, OFF_MOBA:OFF_MOBA + 768].rearrange("(k p) c -> p k c", p=128))
            P.copy("pool", aug[32:34, :].r("p (a t) -> p a t", a=4), self.c("augrows")[32:34, :].un(1).bc([2, 4, 512]))
            for hp in range(2):
                def wq(c, ps, hp=hp):
                    P.ts("dve", q32[:, hp, c * 512:(c + 1) * 512], ps, 0.125, ALU.mult)
                    P.copy("pool", qT[:, hp, c * 512:(c + 1) * 512], q32[:, hp, c * 512:(c + 1) * 512])
                self._proj_fm(wt, hp * 128, wq)

                def wk(c, ps, hp=hp):
                    P.copy("act", kT[:, hp, c * 512:(c + 1) * 512], ps)
                    P.reduce("dve", ksum[:, hp, 2 * c:2 * c + 2], ps.r("p (n t) -> p n t", n=2))
                self._proj_fm(wt, 256 + hp * 128, wk)
            for i in range(16):
                bank = pb[4 + i % 2]
                self._proj_tm(wt, 512, 256, i, bank)
                v4 = bank[:, 0:256].r("p (h d) -> p h d", h=4)
                P.copy("act", vv[:, 0:4:2, i, 0:64], v4[:, 0:4:2, :])
                P.copy("dve", vv[:, 1:4:2, i, 64:128], v4[:, 1:4:2, :])
            if "moba_stop1" in self.dbg:
                return
            for hh in range(2):
                gb = pb[6 + hh]
                r = slice(hh * 64, hh * 64 + 64)
                for i in range(16):
                    for hp in range(2):
                        o = (i * 2 + hp) * 8
                        P.mm(gb[:, o:o + 8], q32[r, hp, i * 128:(i + 1) * 128], ksum[r, hp, :])
                P.tt("dve", gm[:, :, :].r("p (i hp hh) n -> p i hp hh n", hp=2, hh=2)[:, :, :, hh, :],
                     gb[:, 0:256].r("p (i hp n) -> p i hp n", hp=2, n=8),
                     self.c("pastbias").r("p (i hp hh n) -> p i hp hh n", hp=2, hh=2, n=8)[:, :, :, hh, :], ALU.add)
            for a in range(64):
                P.max8(thr[:, a, :], gm[:, a, :])
            P.tt("dve", sel[:, :].r("p (a n) -> p a n", n=8), gm[:, :, :], thr[:, :, 2:3].bc([128, 64, 8]), ALU.is_ge)
            P.tt("dve", sel[:, :], sel[:, :], self.c("pastm"), ALU.mult)
            P.tt("dve", sel[:, :], sel[:, :], self.c("ownm"), ALU.add)
            P.ts("dve", sel[:, :], sel[:, :], -1.0, ALU.add, 1000.0, ALU.mult)
            if "moba_stop2" in self.dbg:
                return
            for g4 in range(4):
                tb_ = pb[g4 % 2]
                for j in range(4):
                    i = g4 * 4 + j
                    P.tr(tb_[0:32, j * 128:(j + 1) * 128], sel[:, i * 32:(i + 1) * 32], ident)
                P.copy("act", aug[0:32, g4 * 512:(g4 + 1) * 512], tb_[0:32, :])
            alibi = self.c("alibi")

            def stream(h, qb, sl):
                hp, r = h // 2, slice((h % 2) * 64, (h % 2) * 64 + 64)
                ro = slice(64 - (h % 2) * 64, 128 - (h % 2) * 64)
                q0 = qb * 512
                sb_, accb = pb[sl], pb[4 + sl]
                Ps = Pt[sl]
                nk = 4 * qb + 4
                for kt in range(nk):
                    c0 = max(0, kt - 4 * qb) * 128
                    n = kt // 2
                    P.mm(sb_[:, c0:512], kT[r, hp, kt * 128:(kt + 1) * 128], qT[r, hp, q0 + c0:q0 + 512], start=True, stop=False)
                    P.mm(sb_[:, c0:512], self.sel_bf[0:34, h * 8 + n, :], aug[0:34, q0 + c0:q0 + 512], start=False, stop=True)
                    yield
                    dl = kt - 4 * qb + 12
                    P.act(Ps[:, c0:512], sb_[:, c0:512], AF.Exp, bias=alibi[:, h * 16 + dl:h * 16 + dl + 1])
                    yield
                    if kt >= 4 * qb:
                        P.tt("pool", Ps[:, c0:c0 + 128], Ps[:, c0:c0 + 128], self.c("m_incl"), ALU.mult)
                        if c0 > 0:
                            P.memset("pool", Ps[:, 0:c0], 0.0)
                        yield
                    P.mm(accb[:, :], vv[:, h, kt, :], Ps[:, :], start=(kt == 0), stop=(kt == nk - 1))
                    yield
                P.recip(rden[sl][r, :], accb[ro, :])
                yield
                P.tt("dve", self.yT[1][r, hp, q0:q0 + 512], accb[r, :], rden[sl][r, :], ALU.mult)
                yield

            todo = sorted([(h, qb) for h in range(0 if "moba_noattn" not in self.dbg else 4, 4) for qb in range(4)], key=lambda t: -t[1])
            active = {}
            while todo or active:
                for sl in range(NS):
                    if sl not in active and todo:
                        h, qb = todo.pop(0)
                        active[sl] = stream(h, qb, sl)
                for sl in list(active):
                    try:
                        next(active[sl])
                    except StopIteration:
                        del active[sl]

    def _conv_chunk(self, wt, col0, m, c, pre, acc, cw, out_view, bias, eng):
        P = self.P
        cb_ = getattr(self, "_cbanks", (0, 1, 2, 3))
        bank = self.pb[cb_[self._pj % len(cb_)]]
        self._pj += 1
        for k in range(8):
            P.mm(bank[:, :], wt[:, k, col0:col0 + 128], self.hT[:, k, c * 512:(c + 1) * 512], start=(k == 0), stop=(k == 7))
        if c == 0:
            P.memset(eng, pre[:, m, 0:3], 0.0)
        else:
            P.copy(eng, pre[:, m, 0:3], pre[:, m, 512:515])
        P.copy("act", pre[:, m, 3:515], bank[:, :])
        P.ts(eng, acc[:, :], pre[:, m, 3:515], cw[:, m * 4 + 3:m * 4 + 4], ALU.mult)
        for j in range(1, 4):
            P.stt("dve", acc[:, :], pre[:, m, 3 - j:515 - j], cw[:, m * 4 + 3 - j:m * 4 + 4 - j], acc[:, :], ALU.mult, ALU.add)
        if bias is None:
            P.act(out_view, acc[:, :], AF.Silu)
        else:
            P.act(out_view, acc[:, :], AF.Silu, bias=bias)

    def u_ssd(self, l, s):
        P = self.P
        pb = self.pb
        self._pj = 0
        ident = self.c("ident")
        ones = self.c("ones")
        with P.scope():
            wt = P.sb("dw", [128, 8, 1028], BF16)
            pre = P.sb("dpre", [128, 6, 515], F32)
            accs = [P.sb("dacc%d" % i, [128, 512], F32) for i in range(2)]
            xbc = [P.sb("dxbc%d" % i, [128, 6, 512], F32) for i in range(2)]
            zs = P.sb("dzs", [128, 4, 256], F32)
            dt = P.sb("ddt", [128, 16, 4], F32)
            gg = P.sb("dgg", [128, 16, 4], F32)
            aneg = P.sb("daneg", [128, 4], F32)
            tok = [P.sb("dtok%d" % i, [128, 512], F32) for i in range(2)]
            gtri = P.sb("dgtri", [128, 4, 128], F32)
            Dm = P.sb("dD", [128, 4, 128], F32)
            lm = P.sb("dlm", [128, 4, 128], F32)
            Mms = [P.sb("dM%d" % i, [128, 4, 128], F32) for i in range(2)]
            sms = [P.sb("dsm%d" % i, [128, 20], F32) for i in range(2)]
            smb = P.sb("dsmb", [128, 4], F32)
            xws = [P.sb("dxw%d" % i, [128, 256], F32) for i in range(2)]
            dx = P.sb("ddx", [128, 256], F32)
            hst = P.sb("dhst", [128, 256], F32)
            yb = [P.sb("dy%d" % i, [128, 256], F32) for i in range(2)]
            junk = P.sb("djunk", [128, 256], F32)
            zsb = [P.sb("dzs%d" % i, [128, 4, 256], F32) for i in range(2)]
            P.dma("sp", wt[:, :, :], self.wb_in[l, :, OFF_SSD:OFF_SSD + 1028].rearrange("(k p) c -> p k c", p=128))
            cw, cb = self.lpv("ssd_conv"), self.lpv("ssd_cb")
            dtb = pb[7]
            for i in range(16):
                for k in range(8):
                    P.mm(dtb[:, i * 4:(i + 1) * 4], self.hT[:, k, i * 128:(i + 1) * 128], wt[:, k, 1024:1028], start=(k == 0), stop=(k == 7))
            P.tt("dve", dt[:, :, :], dtb[:, 0:64].r("p (i h) -> p i h", h=4), self.lpv("ssd_dtb").un(1).bc([128, 16, 4]), ALU.add)
            P.act(dt[:, :, :], dt[:, :, :], AF.Exp)
            P.act(dt[:, :, :], dt[:, :, :], AF.Ln, bias=1.0)
            P.act(aneg[:, :], self.lpv("ssd_alog"), AF.Exp)
            P.stt("dve", gg[:, :, :], dt[:, :, :], -1.0, aneg[:, :].un(1).bc([128, 16, 4]), ALU.mult, ALU.mult)
            P.memset("pool", hst[:, :], 0.0)

            def chunk_front(c):
                xb = xbc[c % 2]
                self._cbanks = (0, 2, 3)
                for m in range(6):
                    self._conv_chunk(wt, 256 + m * 128, m, c, pre, accs[m % 2], cw, xb[:, m, :], cb[:, m:m + 1],
                                     "dve" if m % 2 == 0 else "pool")
                self._cbanks = (0, 1, 2, 3)
                for j in range(4):
                    i = c * 4 + j
                    bank = pb[4 + j % 2]
                    self._proj_tm(wt, 0, 256, i, bank)
                    P.act(zsb[c % 2][:, j, :], bank[:, 0:256], AF.Silu)

            def front(i):
                c, j = i // 4, i % 4
                xb = xbc[c % 2]
                cs = slice(j * 128, (j + 1) * 128)
                b = i % 2
                tk, sm, Mm, xw = tok[b], sms[b], Mms[b], xws[b]
                tb_ = pb[4]
                for m in range(4):
                    P.tr(tb_[:, m * 128:(m + 1) * 128], xb[:, m, cs], ident)
                sb_ = pb[5]
                P.mm(sb_[:, 0:4], self.c("tri128"), gg[:, i, :])
                P.tt("pool", gtri[:, :, :], self.c("tri128").un(1).bc([128, 4, 128]), gg[:, i, :].un(2).bc([128, 4, 128]), ALU.mult)
                yield
                P.copy("act", tk[:, :], tb_[:, :])
                P.copy("dve", sm[:, 0:4], sb_[:, 0:4])
                ab = pb[6]
                P.mm(ab[:, :], ones, gtri[:, :, :].r("p h l -> p (h l)"))
                yield
                P.copy("dve", sm[:, 4:8], ab[:, :].r("p (h l) -> p h l", h=4)[:, :, 127])
                P.tt("dve", Dm[:, :, :], ab[:, :].r("p (h l) -> p h l", h=4), sm[:, 0:4].un(2).bc([128, 4, 128]), ALU.subtract)
                P.act(sm[:, 8:12], sm[:, 0:4], AF.Exp)
                yield
                P.tt("pool", Dm[:, :, :], Dm[:, :, :], self.c("neg_ssd").un(1).bc([128, 4, 128]), ALU.add)
                P.act(sm[:, 12:16], sm[:, 4:8], AF.Exp)
                P.tt("dve", sm[:, 16:20], sm[:, 4:8], sm[:, 0:4], ALU.subtract)
                scb = pb[0]
                for g in range(2):
                    P.mm(scb[:, g * 128:(g + 1) * 128], xb[:, 2 + g, cs], xb[:, 4 + g, cs])
                yield
                P.act(lm[:, :, :], Dm[:, :, :], AF.Exp)
                P.act(sm[:, 16:20], sm[:, 16:20], AF.Exp)
                yield
                P.tt("dve", sm[:, 16:20], sm[:, 16:20], dt[:, i, :], ALU.mult)
                for h in range(4):
                    P.stt("dve", Mm[:, h, :], lm[:, h, :], dt[:, i, h:h + 1], scb[:, (h // 2) * 128:(h // 2 + 1) * 128], ALU.mult, ALU.mult)
                yield
                P.tt("pool", xw[:, :].r("p (h e) -> p h e", h=4), tk[:, 0:256].r("p (h e) -> p h e", h=4),
                     sm[:, 16:20].un(2).bc([128, 4, 64]), ALU.mult)
                yb_ = pb[1] if b == 0 else pb[7]
                for h in range(4):
                    P.mm(yb_[:, h * 64:(h + 1) * 64], Mm[:, h, :], tk[:, h * 64:(h + 1) * 64])
                yield

            def back(i):
                c, j = i // 4, i % 4
                xb = xbc[c % 2]
                cs = slice(j * 128, (j + 1) * 128)
                b = i % 2
                tk, sm, xw = tok[b], sms[b], xws[b]
                yb_ = pb[1] if b == 0 else pb[7]
                for h in range(4):
                    P.mm(yb_[:, 256 + h * 64:256 + (h + 1) * 64], xb[:, 4 + h // 2, cs], hst[:, h * 64:(h + 1) * 64])
                hb = pb[2]
                for g in range(2):
                    P.mm(hb[:, g * 128:(g + 1) * 128], tk[:, 256 + g * 128:256 + (g + 1) * 128], xw[:, g * 128:(g + 1) * 128])
                P.tt("pool", dx[:, :].r("p (h e) -> p h e", h=4), tk[:, 0:256].r("p (h e) -> p h e", h=4),
                     self.lpv("ssd_d").un(2).bc([128, 4, 64]), ALU.mult)
                yield
                y = yb[b]
                P.tt("dve", y[:, :].r("p (h e) -> p h e", h=4), yb_[:, 256:512].r("p (h e) -> p h e", h=4),
                     sm[:, 8:12].un(2).bc([128, 4, 64]), ALU.mult)
                P.tt("pool", hst[:, :].r("p (h e) -> p h e", h=4), hst[:, :].r("p (h e) -> p h e", h=4),
                     sm[:, 12:16].un(2).bc([128, 4, 64]), ALU.mult)
                yield
                P.tt("dve", y[:, :], y[:, :], yb_[:, 0:256], ALU.add)
                P.tt("dve", hst[:, :], hst[:, :], hb[:, 0:256], ALU.add)
                yield
                P.tt("pool", y[:, :], y[:, :], dx[:, :], ALU.add)
                yield
                P.tt("pool", y[:, :], y[:, :], zsb[c % 2][:, j, :], ALU.mult)
                yield
                P.act(junk[:, :], y[:, :], AF.Square, accum=smb[:, 0:1])
                yield
                P.act(smb[:, 1:2], smb[:, 0:1], AF.Ln, bias=EPS, scale=1.0 / 256)
                P.act(smb[:, 1:2], smb[:, 1:2], AF.Exp, scale=-0.5)
                yield
                P.stt("dve", y[:, :], y[:, :], smb[:, 1:2], self.lpv("ssd_norm"), ALU.mult, ALU.mult)
                yield
                ob = pb[3]
                for k in range(2):
                    P.tr(ob[:, k * 128:(k + 1) * 128], y[:, k * 128:(k + 1) * 128], ident)
                yield
                P.copy("act", self.yT[3][:, :, i * 128:(i + 1) * 128], ob[:, 0:256].r("p (k t) -> p k t", k=2))
                yield

            def run_rr(gens):
                gens = list(gens)
                while gens:
                    for g in list(gens):
                        try:
                            next(g)
                        except StopIteration:
                            gens.remove(g)

            chunk_front(0)
            run_rr([front(0)])
            for i in range(16):
                if i % 4 == 0 and i + 4 < 16:
                    chunk_front(i // 4 + 1)
                gens = [back(i)]
                if i + 1 < 16:
                    gens.append(front(i + 1))
                run_rr(gens)

    def u_gdn(self, l, s):
        P = self.P
        pb = self.pb
        self._pj = 0
        ident = self.c("ident")
        ones = self.c("ones")
        with P.scope():
            wt = P.sb("gw", [128, 8, 1032], BF16)
            pre = P.sb("gpre", [128, 6, 515], F32)
            accs = [P.sb("gacc%d" % i, [128, 512], F32) for i in range(2)]
            qkv = [P.sb("gqkv%d" % i, [128, 6, 512], F32) for i in range(2)]
            sq = P.sb("gsq", [128, 512], F32)
            rsn = P.sb("grsn", [128, 512], F32)
            bg = P.sb("gbg", [128, 16, 8], F32)
            beta = P.sb("gbeta", [128, 16, 4], F32)
            lnb = P.sb("glnb", [128, 16, 4], F32)
            gg = P.sb("ggg", [128, 16, 4], F32)
            aneg = P.sb("ganeg", [128, 4], F32)
            kv = [P.sb("gkv%d" % i, [128, 512], F32) for i in range(2)]
            r1 = P.sb("gr1", [128, 4, 128], F32)
            r2 = P.sb("gr2", [128, 4, 128], F32)
            X2 = P.sb("gX2", [128, 4, 128], F32)
            X3 = P.sb("gX3", [128, 4, 128], F32)
            egB = P.sb("gegB", [128, 4, 128], F32)
            smps = [P.sb("gsmp%d" % i, [128, 20], F32) for i in range(2)]
            sms = P.sb("gsms", [128, 8], F32)
            zsb = [P.sb("gzs%d" % i, [128, 4, 256], F32) for i in range(2)]
            qd = [P.sb("gqd%d" % i, [128, 2, 128], F32) for i in range(2)]
            kdec = [P.sb("gkd%d" % i, [128, 256], F32) for i in range(2)]
            kbg = P.sb("gkbg", [128, 256], F32)
            vb = P.sb("gvb", [128, 256], F32)
            NT = [P.sb("gNT%d" % h, [128, 128], F32) for h in range(4)]
            Nn = [P.sb("gN%d" % h, [128, 128], F32) for h in range(4)]
            Ma = [P.sb("gMa%d" % h, [128, 128], F32) for h in range(4)]
            MTa = [P.sb("gMTa%d" % h, [128, 128], F32) for h in range(4)]
            PT = [P.sb("gPT%d" % h, [128, 128], F32) for h in range(4)]
            Aq = [[P.sb("gAq%d_%d" % (b, h), [128, 128], F32) for h in range(4)] for b in range(2)]
            us = [P.sb("gu%d" % i, [128, 4, 64], F32) for i in range(2)]
            wTs = [P.sb("gwT%d" % i, [128, 2, 128], F32) for i in range(2)]
            gts = [P.sb("ggt%d" % i, [128, 4, 2], F32) for i in range(2)]
            S = P.sb("gS", [128, 2, 64], F32)
            vnew = P.sb("gvn", [128, 4, 64], F32)
            ot = P.sb("got", [128, 4, 64], F32)
            on = P.sb("gon", [128, 256], F32)
            P.dma("sp", wt[:, :, :], self.wb_in[l, :, OFF_GDN:OFF_GDN + 1032].rearrange("(k p) c -> p k c", p=128))
            cw = self.lpv("gdn_conv")
            bgb = pb[7]
            for i in range(16):
                for k in range(8):
                    P.mm(bgb[:, i * 8:(i + 1) * 8], self.hT[:, k, i * 128:(i + 1) * 128], wt[:, k, 1024:1032], start=(k == 0), stop=(k == 7))
            P.copy("dve", bg[:, :, :], bgb[:, 0:128].r("p (i c) -> p i c", c=8))
            P.act(lnb[:, :, :], bg[:, :, 0:4], AF.Exp, scale=-1.0)
            P.act(lnb[:, :, :], lnb[:, :, :], AF.Ln, bias=1.0)
            P.ts("dve", lnb[:, :, :], lnb[:, :, :], -1.0, ALU.mult)
            P.act(beta[:, :, :], lnb[:, :, :], AF.Exp)
            P.tt("dve", gg[:, :, :], bg[:, :, 4:8], self.lpv("gdn_dtb").un(1).bc([128, 16, 4]), ALU.add)
            P.act(gg[:, :, :], gg[:, :, :], AF.Exp)
            P.act(gg[:, :, :], gg[:, :, :], AF.Ln, bias=1.0)
            P.act(aneg[:, :], self.lpv("gdn_alog"), AF.Exp)
            P.stt("dve", gg[:, :, :], gg[:, :, :], -1.0, aneg[:, :].un(1).bc([128, 16, 4]), ALU.mult, ALU.mult)
            P.memset("pool", S[:, :, :], 0.0)

            def prep_common(i, qk):
                j = i % 4
                cs = slice(j * 128, (j + 1) * 128)
                b = i % 2
                kvt = kv[b]
                smp = smps[b]
                tb_ = pb[4]
                for m in range(4):
                    P.tr(tb_[:, m * 128:(m + 1) * 128], qk[:, 2 + m, cs], ident)
                P.copy("act", kvt[:, :], tb_[:, :])
                sb_ = pb[3]
                P.mm(sb_[:, 0:4], self.c("tri_bd"), gg[:, i, :])
                P.copy("dve", smp[:, 0:4], sb_[:, 0:4])
                P.tt("pool", r1[:, :, :], self.c("tri_bd").un(1).bc([128, 4, 128]), gg[:, i, :].un(2).bc([128, 4, 128]), ALU.mult)
                P.tt("pool", r2[:, :, :], self.c("ident").un(1).bc([128, 4, 128]), lnb[:, i, :].un(2).bc([128, 4, 128]), ALU.mult)
                P.tt("pool", r2[:, :, :], r2[:, :, :], r1[:, :, :], ALU.add)
                gcB, gcbB = pb[0], pb[1]
                P.mm(gcB[:, :], ones, r1[:, :, :].r("p h c -> p (h c)"))
                P.mm(gcbB[:, :], ones, r2[:, :, :].r("p h c -> p (h c)"))
                gc_b = smp[:, 0:4].un(2).bc([128, 4, 128])
                P.tt("dve", X2[:, :, :], gcB[:, :].r("p (h c) -> p h c", h=4), gc_b, ALU.subtract)
                P.tt("pool", X2[:, :, :], X2[:, :, :], self.c("neg_incl_bd").un(1).bc([128, 4, 128]), ALU.add)
                P.act(X2[:, :, :], X2[:, :, :], AF.Exp)
                P.tt("dve", X3[:, :, :], gcbB[:, :].r("p (h c) -> p h c", h=4), gc_b, ALU.subtract)
                P.tt("pool", X3[:, :, :], X3[:, :, :], self.c("neg_strict_bd").un(1).bc([128, 4, 128]), ALU.add)
                P.act(X3[:, :, :], X3[:, :, :], AF.Exp)
                P.act(egB[:, :, :], gcB[:, :].r("p (h c) -> p h c", h=4), AF.Exp)
                gt = gts[b]
                P.act(gt[:, :, :], gcB[:, :].r("p (h c) -> p h c", h=4)[:, :, 63:128:64], AF.Exp)
                P.copy("dve", smp[0:64, 4:8], gcB[0:64, :].r("p (h c) -> p h c", h=4)[:, :, 63])
                P.copy("dve", smp[64:128, 4:8], gcB[64:128, :].r("p (h c) -> p h c", h=4)[:, :, 127])
                P.act(smp[:, 8:12], smp[:, 0:4], AF.Exp)
                P.tt("dve", smp[:, 12:16], smp[:, 4:8], smp[:, 0:4], ALU.subtract)
                P.act(smp[:, 12:16], smp[:, 12:16], AF.Exp)
                P.tt("dve", smp[:, 16:20], smp[:, 8:12], beta[:, i, :], ALU.mult)
                kd = kdec[b]
                k4 = kvt[:, 0:256].r("p (h d) -> p h d", h=4)
                P.tt("pool", kd[:, :].r("p (h d) -> p h d", h=4), k4, smp[:, 12:16].un(2).bc([128, 4, 64]), ALU.mult)
                P.tt("pool", kbg[:, :].r("p (h d) -> p h d", h=4), k4, smp[:, 16:20].un(2).bc([128, 4, 64]), ALU.mult)
                P.tt("pool", vb[:, :].r("p (h d) -> p h d", h=4), kvt[:, 256:512].r("p (h d) -> p h d", h=4),
                     beta[:, i, :].un(2).bc([128, 4, 64]), ALU.mult)
                qdt = qd[b]
                for hp in range(2):
                    for hh in range(2):
                        r = slice(hh * 64, hh * 64 + 64)
                        P.tt("pool", qdt[r, hp, :], qk[r, hp, cs], egB[r, hp * 2 + hh, :], ALU.mult)

            def head_chain(i, qk, h):
                j = i % 4
                cs = slice(j * 128, (j + 1) * 128)
                b = i % 2
                hp, r = h // 2, slice((h % 2) * 64, (h % 2) * 64 + 64)
                hb = pb[h]
                P.mm(hb[:, 0:128], qk[r, 2 + hp, cs], qk[r, 2 + hp, cs])
                P.mm(hb[:, 128:256], qk[r, 2 + hp, cs], qk[r, hp, cs])
                yield
                P.stt("dve", NT[h][:, :], hb[:, 0:128], -1.0, X3[:, h, :], ALU.mult, ALU.mult)
                P.tt("dve", Aq[b][h][:, :], hb[:, 128:256], X2[:, h, :], ALU.mult)
                yield
                P.tr(hb[:, 256:384], NT[h][:, :], ident)
                P.tt("pool", PT[h][:, :], NT[h][:, :], ident, ALU.add)
                yield
                P.copy("act", Nn[h][:, :], hb[:, 256:384])
                yield
                M, MT = Nn[h], NT[h]
                for lvl in range(5):
                    P.mm(hb[:, 0:128], MT[:, :], M[:, :])
                    if lvl < 4:
                        P.mm(hb[:, 128:256], M[:, :], MT[:, :])
                    yield
                    M2 = Ma[h] if lvl % 2 == 0 else Nn[h]
                    MT2 = MTa[h] if lvl % 2 == 0 else NT[h]
                    P.copy("act", M2[:, :], hb[:, 0:128])
                    if lvl < 4:
                        P.copy("dve", MT2[:, :], hb[:, 128:256])
                    yield
                    P.mm(hb[:, 256:384], M2[:, :], PT[h][:, :])
                    yield
                    P.tt("dve", PT[h][:, :], PT[h][:, :], hb[:, 256:384], ALU.add)
                    yield
                    M, MT = M2, MT2
                P.mm(hb[:, 0:64], PT[h][:, :], vb[:, h * 64:(h + 1) * 64])
                P.mm(hb[:, 128:256], kbg[:, hp * 128:(hp + 1) * 128], PT[h][:, :])
                yield
                P.copy("act", us[b][:, h, :], hb[:, 0:64])
                P.copy("dve", wTs[b][r, hp, :], hb[r, 128:256])
                yield

            def scan(i):
                b = i % 2
                j4 = i % 4
                for jc in range(2):
                    rj = slice(jc * 64, jc * 64 + 64)
                    wsb, ob, snb = pb[5], pb[6], pb[7]
                    for hh in range(2):
                        for h in (hh, hh + 2):
                            hp, r = h // 2, slice((h % 2) * 64, (h % 2) * 64 + 64)
                            P.mm(wsb[:, h * 64:(h + 1) * 64], wTs[b][r, hp, :], S[r, hp, :])
                    yield
                    P.tt("dve", vnew[rj, :, :], us[b][rj, :, :], wsb[rj, 0:256].r("p (h e) -> p h e", h=4), ALU.subtract)
                    yield
                    for h in range(4):
                        hp, r = h // 2, slice((h % 2) * 64, (h % 2) * 64 + 64)
                        P.mm(ob[:, h * 64:(h + 1) * 64], qd[b][r, hp, :], S[r, hp, :], start=True, stop=False)
                        P.mm(ob[:, h * 64:(h + 1) * 64], Aq[b][h][rj, :], vnew[rj, h, :], start=False, stop=True)
                    for h in range(4):
                        hp = h // 2
                        P.mm(snb[:, h * 64:(h + 1) * 64], kdec[b][rj, hp * 128:(hp + 1) * 128], vnew[rj, h, :])
                    yield
                    for h in range(4):
                        hp, r = h // 2, slice((h % 2) * 64, (h % 2) * 64 + 64)
                        P.stt("dve", S[r, hp, :], S[r, hp, :], gts[b][r, h, jc:jc + 1], snb[r, h * 64:(h + 1) * 64], ALU.mult, ALU.add)
                    P.copy("act", ot[rj, :, :], ob[rj, 0:256].r("p (h e) -> p h e", h=4))
                    yield
                P.tt("pool", on[:, :], ot[:, :, :].r("p h e -> p (h e)"), ot[:, :, :].r("p h e -> p (h e)"), ALU.mult)
                P.reduce("dve", sms[:, 0:4], on[:, :].r("p (h e) -> p h e", h=4))
                yield
                P.act(sms[:, 4:8], sms[:, 0:4], AF.Ln, bias=EPS, scale=1.0 / 64)
                P.act(sms[:, 4:8], sms[:, 4:8], AF.Exp, scale=-0.5)
                yield
                P.tt("pool", on[:, :].r("p (h e) -> p h e", h=4), ot[:, :, :], sms[:, 4:8].un(2).bc([128, 4, 64]), ALU.mult)
                P.tt("pool", on[:, :].r("p (h e) -> p h e", h=4), on[:, :].r("p (h e) -> p h e", h=4),
                     self.lpv("gdn_norm").un(1).bc([128, 4, 64]), ALU.mult)
                P.tt("pool", on[:, :], on[:, :], zsb[(i // 4) % 2][:, j4, :], ALU.mult)
                yield
                tb2 = pb[4]
                for k in range(2):
                    P.tr(tb2[:, 256 + k * 128:256 + (k + 1) * 128], on[:, k * 128:(k + 1) * 128], ident)
                P.copy("act", self.yT[0][:, :, i * 128:(i + 1) * 128], tb2[:, 256:512].r("p (k t) -> p k t", k=2))
                yield

            def chunk_front(c):
                qk = qkv[c % 2]
                self._cbanks = (4,)
                for m in range(6):
                    self._conv_chunk(wt, m * 128, m, c, pre, accs[m % 2], cw, qk[:, m, :], None, "dve" if m % 2 == 0 else "pool")
                    yield
                self._cbanks = (0, 1, 2, 3)
                for m in range(4):
                    P.act(sq[:, :], qk[:, m, :], AF.Square)
                    nb = pb[4]
                    P.mm(nb[:, :], self.c("blk64"), sq[:, :])
                    P.act(rsn[:, :], nb[:, :], AF.Ln, bias=EPS)
                    P.act(rsn[:, :], rsn[:, :], AF.Exp, scale=-0.5)
                    if m < 2:
                        P.stt("dve", qk[:, m, :], qk[:, m, :], 0.125, rsn[:, :], ALU.mult, ALU.mult)
                    else:
                        P.tt("pool", qk[:, m, :], qk[:, m, :], rsn[:, :], ALU.mult)
                    yield
                for j in range(4):
                    i = c * 4 + j
                    bank = pb[4]
                    self._proj_tm(wt, 768, 256, i, bank)
                    P.act(zsb[c % 2][:, j, :], bank[:, 0:256], AF.Silu)
                    yield

            def run_rr(gens):
                gens = list(gens)
                while gens:
                    for g in list(gens):
                        try:
                            next(g)
                        except StopIteration:
                            gens.remove(g)

            prev = None
            run_rr([chunk_front(0)])
            for c in range(4):
                for j in range(4):
                    i = c * 4 + j
                    prep_common(i, qkv[c % 2])
                    gens = [head_chain(i, qkv[c % 2], h) for h in range(4)]
                    if prev is not None:
                        gens.append(scan(prev))
                    if j == 1 and c + 1 < 4:
                        gens.append(chunk_front(c + 1))
                    run_rr(gens)
                    prev = i
            run_rr([scan(prev)])


def _prep_inputs(inputs, n_layers=2):
    f = lambda k: np.ascontiguousarray(np.asarray(inputs[k], dtype=np.float32)[:n_layers])
    shared = {"w_in": f("w_in"), "w_gate": f("w_gate"), "w_branch": f("w_branch"), "w_out": f("w_out"),
              "w_up": f("w_up"), "w_down": f("w_down"), "consts": CONST_ARR,
              "lp": np.stack([_layer_params(inputs, l) for l in range(n_layers)])}
    return shared


def kernel(**inputs):
    x = np.ascontiguousarray(np.asarray(inputs["x"], dtype=np.float32))
    n_cores = 8
    per = x.shape[0] // n_cores
    shared = _prep_inputs(inputs)
    nc = K(n_seq=per).build()
    in_maps = []
    for c in range(n_cores):
        m = dict(shared)
        m["x"] = x[c * per:(c + 1) * per].reshape(per * T, D)
        in_maps.append(m)
    res = run_bass_kernel_spmd(nc, in_maps, core_ids=list(range(n_cores)))
    out = np.stack([np.asarray(r["out"]).reshape(per, T, D) for r in res.results], axis=0)
    return out.reshape(x.shape).astype(np.float32)
```

```python
import numpy as np
from contextlib import ExitStack, contextmanager
import concourse.bass as bass
import concourse.mybir as mybir
from concourse.bass_utils import run_bass_kernel_spmd

F32 = mybir.dt.float32
BF16 = mybir.dt.bfloat16
AF = mybir.ActivationFunctionType
ALU = mybir.AluOpType
AX = mybir.AxisListType

ENGINES = ("pe", "act", "dve", "pool", "sp")


class Res:
    __slots__ = ("name", "last_w", "readers", "slot", "t", "excl", "pe_last")

    def __init__(self, name, t=None, excl=False):
        self.name = name
        self.excl = excl
        self.pe_last = None
        self.last_w = None
        self.readers = []
        self.slot = None
        self.t = t

    def __getitem__(self, idx):
        return V(self, self.t[idx])


class V:
    __slots__ = ("res", "ap")

    def __init__(self, res, ap):
        self.res = res
        self.ap = ap

    def __getitem__(self, idx):
        return V(self.res, self.ap[idx])

    def r(self, pat, **kw):
        return V(self.res, self.ap.rearrange(pat, **kw))

    def un(self, axis):
        return V(self.res, self.ap.unsqueeze(axis))

    def bc(self, shape):
        return V(self.res, self.ap.broadcast_to(list(shape)))


def _ap(x):
    return x.ap if isinstance(x, V) else x


def _rs(*xs):
    return [x.res for x in xs if isinstance(x, V)]


class Op:
    __slots__ = ("eng", "fn", "seq", "waits", "signal", "is_dma", "slot", "cnt",
                 "clock", "semval", "kind")

    def __init__(self, eng, fn, is_dma=False, slot=None):
        self.eng = eng
        self.fn = fn
        self.is_dma = is_dma
        self.slot = slot
        self.waits = []
        self.signal = False
        self.clock = None
        self.semval = None
        self.cnt = 0


class Prog:
    def __init__(self, nc):
        self.nc = nc
        self.ops = {e: [] for e in ENGINES}
        self.clock = {e: {} for e in ENGINES}
        self.nslots = 0
        self.slot_last = {}
        self.slot_cnt = {}
        self.all_res = []
        self.stack = None
        self.n_wait = 0
        self.free_slots = []

    def sb(self, name, shape, dtype):
        self.uid = getattr(self, "uid", 0) + 1
        name = "%s_%d" % (name, self.uid)
        t = self.stack.enter_context(self.nc.sbuf_tensor(name, list(shape), dtype))
        r = Res(name, t)
        self.all_res.append(r)
        return r

    def ps(self, name, shape, dtype=F32):
        t = self.stack.enter_context(self.nc.psum_tensor(name, list(shape), dtype))
        r = Res(name, t, excl=True)
        self.all_res.append(r)
        return r

    def res(self, name):
        r = Res(name, None)
        self.all_res.append(r)
        return r

    def _key(self, op):
        return ("s", op.slot) if op.is_dma else op.eng

    def _val(self, op):
        return op.cnt if op.is_dma else op.seq

    def _add_dep(self, op, dep, raw, force=False):
        if dep is None or dep is op:
            return
        if not force and not dep.is_dma and not op.is_dma and dep.eng == op.eng:
            if op.eng == "pe":
                return
        k = self._key(dep)
        v = self._val(dep)
        ck = self.clock[op.eng]
        if ck.get(k, -1) >= v:
            return
        op.waits.append(dep)
        dep.signal = True
        for kk, vv in dep.clock.items():
            if ck.get(kk, -1) < vv:
                ck[kk] = vv
        ck[k] = v

    def add(self, eng, fn, reads=(), writes=(), is_dma=False, force_dep=None):
        op = Op(eng, fn, is_dma=is_dma)
        xr = [r for r in reads if r.excl]
        if xr:
            reads = [r for r in reads if not r.excl]
            writes = list(writes) + [r for r in xr if r not in writes]
        lst = self.ops[eng]
        op.seq = len(lst)
        if is_dma:
            tile = None
            for r in list(writes) + list(reads):
                if r.t is not None:
                    tile = r
                    break
            assert tile is not None, "dma needs an sbuf tile resource"
            if tile.slot is None:
                if self.free_slots:
                    tile.slot = self.free_slots.pop()
                else:
                    tile.slot = self.nslots
                    self.nslots += 1
            op.slot = tile.slot
            op.cnt = self.slot_cnt.get(op.slot, 0) + 1
            self.slot_cnt[op.slot] = op.cnt
        cand = []
        for r in reads:
            if r.last_w is not None:
                cand.append((r.last_w, True))
        for w in writes:
            if w.last_w is not None:
                cand.append((w.last_w, True))
            for rd in w.readers:
                cand.append((rd, False))
        cand.sort(key=lambda t: -self._val(t[0]))
        for d, raw in cand:
            self._add_dep(op, d, raw)
        if force_dep is not None:
            self._add_dep(op, force_dep, True, force=True)
        for r in reads:
            r.readers.append(op)
        for w in writes:
            w.last_w = op
            w.readers = []
        ck = dict(self.clock[eng])
        if not is_dma:
            ck[eng] = op.seq
        else:
            ck[("s", op.slot)] = op.cnt
        op.clock = ck
        lst.append(op)
        return op

    def dma(self, q, out, in_, reads=(), writes=()):
        o, i = _ap(out), _ap(in_)
        return self.add(q, lambda e: e.dma_start(out=o, in_=i), list(reads) + _rs(in_), list(writes) + _rs(out), is_dma=True)

    def _pe_rowgroup_dep(self, out, stat):
        bp = stat.ap.base_partition()
        bp = bp() if callable(bp) else bp
        k = stat.ap.shape[0]
        groups = set(range(bp // 32, (bp + k + 31) // 32))
        prev = out.res.pe_last
        force = prev[0] if (prev is not None and prev[1].isdisjoint(groups)) else None
        return groups, force

    def mm(self, out, lhsT, rhs, start=True, stop=True):
        o, l, r = out.ap, lhsT.ap, rhs.ap
        groups, force = self._pe_rowgroup_dep(out, lhsT)
        op = self.add("pe", lambda e: e.matmul(o, lhsT=l, rhs=r, start=start, stop=stop), _rs(lhsT, rhs), _rs(out), force_dep=force)
        out.res.pe_last = (op, groups)
        return op

    def tr(self, out, in_, ident):
        o, i, d = out.ap, in_.ap, ident.ap
        groups, force = self._pe_rowgroup_dep(out, in_)
        op = self.add("pe", lambda e: e.transpose(out=o, in_=i, identity=d), _rs(in_, ident), _rs(out), force_dep=force)
        out.res.pe_last = (op, groups)
        return op

    def act(self, out, in_, func, bias=0.0, scale=1.0, accum=None):
        o, i, b, sc = out.ap, in_.ap, _ap(bias), _ap(scale)
        if accum is None:
            return self.add("act", lambda e: e.activation(out=o, in_=i, func=func, bias=b, scale=sc), _rs(in_, bias, scale), _rs(out))
        a = accum.ap
        return self.add("act", lambda e: e.activation(out=o, in_=i, func=func, bias=b, scale=sc, accum_out=a), _rs(in_, bias, scale), _rs(out, accum))

    def tt(self, eng, out, in0, in1, op):
        o, a, b = out.ap, in0.ap, in1.ap
        return self.add(eng, lambda e: e.tensor_tensor(out=o, in0=a, in1=b, op=op), _rs(in0, in1), _rs(out))

    def ts(self, eng, out, in0, s1, op0, s2=None, op1=None):
        o, a, x1, x2 = out.ap, in0.ap, _ap(s1), _ap(s2)
        if op1 is None:
            return self.add(eng, lambda e: e.tensor_scalar(out=o, in0=a, scalar1=x1, scalar2=None, op0=op0), _rs(in0, s1), _rs(out))
        return self.add(eng, lambda e: e.tensor_scalar(out=o, in0=a, scalar1=x1, scalar2=x2, op0=op0, op1=op1), _rs(in0, s1, s2), _rs(out))

    def stt(self, eng, out, in0, scalar, in1, op0, op1):
        o, a, sc, b = out.ap, in0.ap, _ap(scalar), in1.ap
        return self.add(eng, lambda e: e.scalar_tensor_tensor(out=o, in0=a, scalar=sc, in1=b, op0=op0, op1=op1), _rs(in0, scalar, in1), _rs(out))

    def copy(self, eng, out, in_):
        o, i = out.ap, in_.ap
        if eng == "act":
            return self.add("act", lambda e: e.copy(out=o, in_=i), _rs(in_), _rs(out))
        return self.add(eng, lambda e: e.tensor_copy(out=o, in_=i), _rs(in_), _rs(out))

    def memset(self, eng, out, val):
        o = out.ap
        return self.add(eng, lambda e: e.memset(o, val), [], _rs(out))

    def reduce(self, eng, out, in_, op=None):
        o, i = out.ap, in_.ap
        op = op or ALU.add
        return self.add(eng, lambda e: e.tensor_reduce(out=o, in_=i, axis=AX.X, op=op), _rs(in_), _rs(out))

    def max8(self, out, in_):
        o, i = out.ap, in_.ap
        return self.add("dve", lambda e: e.max(out=o, in_=i), _rs(in_), _rs(out))

    def recip(self, out, in_):
        o, i = out.ap, in_.ap
        return self.add("dve", lambda e: e.reciprocal(out=o, in_=i), _rs(in_), _rs(out))

    def barrier(self):
        lasts = []
        for e in ENGINES:
            for op in reversed(self.ops[e]):
                if not op.is_dma and op.fn is not None:
                    lasts.append(op)
                    break
        dmas = [op for e in ENGINES for op in self.ops[e] if op.is_dma and self.slot_cnt[op.slot] == op.cnt]
        for e in ENGINES:
            op = Op(e, None)
            op.seq = len(self.ops[e])
            for d in lasts:
                if d.eng != e:
                    self._add_dep(op, d, True)
            for d in dmas:
                self._add_dep(op, d, True)
            ck = dict(self.clock[e])
            op.clock = ck
            op.seq = -1
            self.ops[e].append(op)

    @contextmanager
    def scope(self):
        old = self.stack
        mark = len(self.all_res)
        with ExitStack() as st:
            self.stack = st
            yield
            self.barrier()
            for r in self.all_res[mark:]:
                if r.slot is not None:
                    self.free_slots.append(r.slot)
                    r.slot = None
            del self.all_res[mark:]
        self.stack = old

    def emit(self, final_wait_ops=()):
        nc = self.nc
        from contextlib import ExitStack
        with ExitStack() as st:
            esem = {e: st.enter_context(nc.semaphore("sem_" + e)) for e in ENGINES}
            ssem = {s: st.enter_context(nc.semaphore("dsem%d" % s)) for s in range(self.nslots)}
            for e in ENGINES:
                v = 0
                for op in self.ops[e]:
                    if op.fn is None or op.is_dma:
                        continue
                    if op.signal:
                        v += 1
                    op.semval = v
            block = st.enter_context(nc.Block())

            def run(e, eng):
                for op in self.ops[e]:
                    for d in op.waits:
                        if d.is_dma:
                            eng.wait_ge(ssem[d.slot], 16 * d.cnt)
                        else:
                            eng.wait_ge(esem[d.eng], d.semval)
                        self.n_wait += 1
                    if op.fn is None:
                        continue
                    ins = op.fn(eng)
                    if op.is_dma:
                        ins.then_inc(ssem[op.slot], 16)
                    elif op.signal:
                        ins.then_inc(esem[e], 1)

            @block.tensor
            def _(eng):
                run("pe", eng)

            @block.scalar
            def _(eng):
                run("act", eng)

            @block.vector
            def _(eng):
                run("dve", eng)

            @block.gpsimd
            def _(eng):
                run("pool", eng)

            @block.sync
            def _(eng):
                run("sp", eng)

D = 1024
T = 2048
NH = 4
HD = 64
BW = 256
DFF = 4096
OFF_GDN, OFF_MOBA, OFF_SB, OFF_SSD, INW = 0, 1032, 1800, 2568, 3596
EPS = 1e-6
NEG = -30000.0


def _const_table():
    p = np.arange(128)[:, None]
    f = np.arange(128)[None, :]
    c = {}
    c["ident"] = (p == f)
    c["ones"] = np.ones((128, 128))
    c["blk64"] = (p // 64 == f // 64)
    c["tri128"] = (p <= f)
    c["neg_ssd"] = np.where(f >= p, 0.0, NEG)
    same = (p // 64 == f // 64)
    c["tri_bd"] = same & (p <= f)
    c["neg_incl_bd"] = np.where(same & (f >= p), 0.0, NEG)
    c["neg_strict_bd"] = np.where(same & (f > p), 0.0, NEG)
    c["m_strict"] = (f > p)
    c["m_incl"] = (f >= p)
    c["u_incl"] = (p >= f)
    i = np.arange(16)[:, None, None]
    n = np.arange(8)[None, None, :]
    h = np.arange(4)[None, :, None]
    past = np.broadcast_to(n < i // 2, (16, 4, 8))
    own = np.broadcast_to(n == i // 2, (16, 4, 8))
    slopes = np.array([2.0 ** (-8.0 * (k + 1) / 4) for k in range(4)])
    dl = np.arange(16)[None, None, :] - 12
    c["alibi"] = (slopes[None, :, None] * (dl * 128 + np.arange(128)[:, None, None])).reshape(128, 64)
    v = np.zeros((128, 32))
    for r in range(32):
        v[r, r] = 1.0
    for hh in range(4):
        v[32, hh * 8:(hh + 1) * 8] = slopes[hh]
        v[33, hh * 8:(hh + 1) * 8] = slopes[hh]
    c["selv"] = v
    t = np.arange(512)
    a = np.zeros((128, 512))
    a[32] = -128.0 * (t // 128)
    a[33] = -(t % 128).astype(np.float64)
    c["augrows"] = a
    c["pastbias"] = np.broadcast_to(np.where(past, 0.0, -1e30).reshape(1, 512), (128, 512))
    c["pastm"] = np.broadcast_to(past.reshape(1, 512).astype(np.float64), (128, 512))
    c["ownm"] = np.broadcast_to(own.reshape(1, 512).astype(np.float64), (128, 512))
    offs = {}
    cols = []
    o = 0
    for k, val in c.items():
        val = np.asarray(val, dtype=np.float32)
        offs[k] = (o, val.shape[1])
        cols.append(val)
        o += val.shape[1]
    return np.ascontiguousarray(np.concatenate(cols, axis=1)), offs


CONST_ARR, CONST_OFF = _const_table()
NCONST = CONST_ARR.shape[1]
NCONST_RES = CONST_OFF["pastbias"][0]

LP_OFF = {}
_o = 0
for _k, _w in (("g_mix_post", 1024), ("g_ffn_post", 1024), ("gdn_conv", 24), ("ssd_conv", 24), ("ssd_cb", 6),
               ("gdn_alog", 4), ("gdn_dtb", 4), ("ssd_alog", 4), ("ssd_dtb", 4), ("ssd_d", 4),
               ("gdn_norm", 64), ("ssd_norm", 256), ("rowg", 16)):
    LP_OFF[_k] = (_o, _w)
    _o += _w
NLP = _o


def _layer_params(inp, l):
    lp = np.zeros((128, NLP), np.float32)

    def put(k, arr):
        o, w = LP_OFF[k]
        lp[:, o:o + w] = arr
    put("g_mix_post", np.broadcast_to(inp["norm_mix_post"][l][None, :], (128, 1024)))
    put("g_ffn_post", np.broadcast_to(inp["norm_ffn_post"][l][None, :], (128, 1024)))
    put("gdn_conv", inp["gdn_conv"][l].reshape(4, 6, 128).transpose(2, 1, 0).reshape(128, 24))
    put("ssd_conv", inp["ssd_conv"][l].reshape(4, 6, 128).transpose(2, 1, 0).reshape(128, 24))
    put("ssd_cb", inp["ssd_conv_bias"][l].reshape(6, 128).T)
    for k, src in (("gdn_alog", "gdn_a_log"), ("gdn_dtb", "gdn_dt_bias"), ("ssd_alog", "ssd_a_log"),
                   ("ssd_dtb", "ssd_dt_bias"), ("ssd_d", "ssd_d")):
        put(k, np.broadcast_to(inp[src][l][None, :], (128, 4)))
    put("gdn_norm", np.broadcast_to(inp["gdn_norm"][l][None, :], (128, 64)))
    put("ssd_norm", np.broadcast_to(inp["ssd_norm"][l][None, :], (128, 256)))
    rg = np.concatenate([inp["norm_mix_pre"][l].reshape(8, 128).T, inp["norm_ffn_pre"][l].reshape(8, 128).T], axis=1)
    put("rowg", rg)
    return lp


class K:
    def __init__(self, n_seq=4, n_layers=2, units=("gdn", "moba", "sb", "ssd", "merge"), dbg=None):
        self.n_seq, self.n_layers, self.units, self.dbg = n_seq, n_layers, units, (dbg or {})
        nc = bass.Bass("TRN2", target_bir_lowering=False)
        self.nc = nc
        self.P = Prog(nc)
        NL = n_layers
        di = lambda name, shape, dt=F32: nc.dram_tensor(name, list(shape), dt, kind="ExternalInput").ap()
        dn = lambda name, shape, dt=BF16: nc.dram_tensor(name, list(shape), dt).ap()
        self.x = di("x", [n_seq * T, D])
        self.w_in = di("w_in", [NL, D, INW])
        self.w_gate = di("w_gate", [NL, 4, D, D])
        self.w_branch = di("w_branch", [NL, 4, BW, D])
        self.w_out = di("w_out", [NL, D, D])
        self.w_up = di("w_up", [NL, D, DFF])
        self.w_down = di("w_down", [NL, DFF, D])
        self.consts = di("consts", [128, NCONST])
        self.lpd = di("lp", [NL, 128, NLP])
        self.out = nc.dram_tensor("out", [n_seq * T, D], F32, kind="ExternalOutput").ap()
        self.xmid = dn("xmid", [n_seq * T, D], F32)
        self.wb_in = dn("wb_in", [NL, D, INW])
        self.wb_gate = dn("wb_gate", [NL, 8, 128, 4, 8, 128])
        self.wb_br = dn("wb_br", [NL, 8, 128, 4, 2, 128])
        self.wb_out = dn("wb_out", [NL, D, D])
        self.wb_up = dn("wb_up", [NL, 32, 128, 8, 128])
        self.wb_down = dn("wb_down", [NL, DFF, D])
        if "inject_yT" in self.dbg:
            self.dbg_yT = di("dbg_yT", [4, BW, T])
        if "dump_yT" in self.dbg:
            self.dbg_yT_out = nc.dram_tensor("dbg_yT_out", [4, BW, T], F32, kind="ExternalOutput").ap()
        if "dump_hT" in self.dbg:
            self.dbg_hT_out = nc.dram_tensor("dbg_hT_out", [D, T], F32, kind="ExternalOutput").ap()
        self._q = 0

    def q(self):
        return "sp"

    def c(self, name):
        o, w = CONST_OFF[name]
        if o >= NCONST_RES:
            return self.mcst[:, o - NCONST_RES:o - NCONST_RES + w]
        return self.cst[:, o:o + w]

    def lpv(self, name):
        o, w = LP_OFF[name]
        return self.lp[:, o:o + w]

    def build(self):
        P = self.P
        with ExitStack() as st:
            P.stack = st
            self.pb = [P.ps("pb%d" % i, [128, 512], F32) for i in range(8)]
            self.cst = P.sb("cst", [128, NCONST_RES], F32)
            self.lp = P.sb("lp_sb", [128, NLP], F32)
            self.ones_bf = P.sb("ones_bf", [128, 128], BF16)
            self.uincl_bf = P.sb("uincl_bf", [128, 128], BF16)
            self.sel_bf = P.sb("sel_bf", [128, 32, 128], BF16)
            self.hT = P.sb("hT", [128, 8, T], BF16)
            self.yT = [P.sb("yT%d" % g, [128, 2, T], BF16) for g in range(4)]
            P.dma("sp", self.cst[:, :], self.consts[:, 0:NCONST_RES])
            P.copy("dve", self.ones_bf[:, :], self.c("ones"))
            P.copy("dve", self.uincl_bf[:, :], self.c("u_incl"))
            P.copy("pool", self.sel_bf[0:34, :, :], self.c("selv")[0:34, :].un(2).bc([34, 32, 128]))
            if "dump_yT" in self.dbg:
                for g in range(4):
                    P.memset("pool", self.yT[g][:, :, :], 0.0)
            for l in range(self.n_layers):
                P.dma("sp", self.lp[:, :], self.lpd[l, :, :])
                if "noconvert" not in self.dbg:
                    self.convert_weights(l)
                xsrc = self.x if l == 0 else self.xmid
                xdst = self.out if l == self.n_layers - 1 else self.xmid
                for s in range(self.n_seq):
                    self.u_norm(l, s, xsrc)
                    if "dump_hT" in self.dbg:
                        self.dump_hT()
                    if "inject_yT" in self.dbg:
                        self.inject_yT()
                    if "sb" in self.units:
                        self.u_sb(l, s)
                    if "moba" in self.units:
                        self.u_moba(l, s)
                    if "ssd" in self.units:
                        self.u_ssd(l, s)
                    if "gdn" in self.units:
                        self.u_gdn(l, s)
                    if "dump_yT" in self.dbg:
                        self.dump_yT()
                    if "merge" in self.units:
                        self.u_merge(l, s, xsrc, xdst)
            P.barrier()
            P.emit()
        return self.nc

    def dump_hT(self):
        P = self.P
        with P.scope():
            t = P.sb("dh", [128, 8, T], F32)
            P.copy("dve", t[:, :, :], self.hT[:, :, :])
            P.dma("sp", self.dbg_hT_out.rearrange("(k p) t -> p k t", p=128), t[:, :, :])

    def dump_yT(self):
        P = self.P
        with P.scope():
            for g in range(4):
                t = P.sb("dy%d" % g, [128, 2, T], F32)
                P.copy("dve", t[:, :, :], self.yT[g][:, :, :])
                P.dma("sp", self.dbg_yT_out[g].rearrange("(k p) t -> p k t", p=128), t[:, :, :])

    def inject_yT(self):
        P = self.P
        with P.scope():
            for g in range(4):
                t = P.sb("iy%d" % g, [128, 2, T], F32)
                P.dma("sp", t[:, :, :], self.dbg_yT[g].rearrange("(k p) t -> p k t", p=128))
                P.copy("dve", self.yT[g][:, :, :], t[:, :, :])

    def convert_weights(self, l):
        P = self.P
        with P.scope():
            st32 = [P.sb("cv32_%d" % i, [128, 4096], F32) for i in range(2)]
            st16 = [P.sb("cv16_%d" % i, [128, 4096], BF16) for i in range(2)]
            cnt = [0]
            rowg = self.lpv("rowg")

            def cv(src, w, scales, stores):
                b = cnt[0] % 2
                cnt[0] += 1
                s32, s16 = st32[b], st16[b]
                P.dma(self.q(), s32[:, 0:w] if len(src.shape) == 2 else s32[:, 0:w].r("p (k c) -> p k c", k=src.shape[1]), src)
                eng = "dve"
                if scales is None:
                    P.copy(("act", "act", "dve")[cnt[0] % 3], s16[:, 0:w], s32[:, 0:w])
                else:
                    for (a, bnd, col) in scales:
                        P.ts(eng, s16[:, a:bnd], s32[:, a:bnd], rowg[:, col:col + 1], ALU.mult)
                for (dst, a, bnd, pat, kw) in stores:
                    v = s16[:, a:bnd]
                    if pat:
                        v = v.r(pat, **kw)
                    P.dma("act", dst, v)

            for kt in range(8):
                cv(self.w_in[l, kt * 128:(kt + 1) * 128, :], INW, [(0, INW, kt)],
                   [(self.wb_in[l, kt * 128:(kt + 1) * 128, :], 0, INW, None, None)])
            for g in range(4):
                for half in range(2):
                    src = self.w_gate[l, g, half * 512:(half + 1) * 512, :].rearrange("(k p) c -> p k c", p=128)
                    cv(src, 4096, [(k * 1024, (k + 1) * 1024, half * 4 + k) for k in range(4)],
                       [(self.wb_gate[l, :, :, g, half * 4 + k, :].rearrange("c p j -> p c j"), k * 1024, (k + 1) * 1024,
                         "p (c j) -> p c j", dict(j=128)) for k in range(4)])
            for g in range(4):
                src = self.w_branch[l, g].rearrange("(k p) c -> p k c", p=128)
                cv(src, 2048, None,
                   [(self.wb_br[l, :, :, g, k, :].rearrange("c p j -> p c j"), k * 1024, (k + 1) * 1024,
                     "p (c j) -> p c j", dict(j=128)) for k in range(2)])
            for half in range(2):
                src = self.w_out[l, half * 512:(half + 1) * 512, :].rearrange("(k p) c -> p k c", p=128)
                cv(src, 4096, None,
                   [(self.wb_out[l, half * 512:(half + 1) * 512, :].rearrange("(k p) c -> p k c", p=128), 0, 4096,
                     "p (k c) -> p k c", dict(k=4))])
            for kt in range(8):
                cv(self.w_up[l, kt * 128:(kt + 1) * 128, :], 4096, [(0, 4096, 8 + kt)],
                   [(self.wb_up[l, f0:f0 + 8, :, kt, :].rearrange("f p j -> p f j"), f0 * 128, (f0 + 8) * 128,
                     "p (f j) -> p f j", dict(j=128)) for f0 in range(0, 32, 8)])
            for qd in range(8):
                src = self.w_down[l, qd * 512:(qd + 1) * 512, :].rearrange("(k p) c -> p k c", p=128)
                cv(src, 4096, None,
                   [(self.wb_down[l, qd * 512:(qd + 1) * 512, :].rearrange("(k p) c -> p k c", p=128), 0, 4096,
                     "p (k c) -> p k c", dict(k=4))])

    def u_norm(self, l, s, xsrc):
        P = self.P
        ident = self.c("ident")
        with P.scope():
            NB = 4
            xt = [P.sb("nx%d" % i, [128, D], F32) for i in range(NB)]
            hh = [P.sb("nh%d" % i, [128, D], F32) for i in range(NB)]
            junk = P.sb("njunk", [128, D], BF16)
            ss = [P.sb("nss%d" % i, [128, 1], F32) for i in range(NB)]
            rs = [P.sb("nrs%d" % i, [128, 1], F32) for i in range(NB)]
            for i in range(16):
                b = i % NB
                r0 = s * T + i * 128
                P.dma(self.q(), xt[b][:, :], xsrc[r0:r0 + 128, :])
                P.act(junk[:, :], xt[b][:, :], AF.Square, accum=ss[b][:, :])
                P.act(rs[b][:, :], ss[b][:, :], AF.Ln, bias=EPS, scale=1.0 / D)
                P.act(rs[b][:, :], rs[b][:, :], AF.Exp, scale=-0.5)
                P.ts("dve", hh[b][:, :], xt[b][:, :], rs[b][:, 0:1], ALU.mult)
                for g in range(2):
                    bank = self.pb[(2 * i + g) % 8]
                    for k in range(4):
                        kk = g * 4 + k
                        P.tr(bank[:, k * 128:(k + 1) * 128], hh[b][:, kk * 128:(kk + 1) * 128], ident)
                    P.copy("act" if g == 0 else "dve", self.hT[:, g * 4:(g + 1) * 4, i * 128:(i + 1) * 128],
                           bank[:, :].r("p (k n) -> p k n", k=4))

    def u_merge(self, l, s, xsrc, xdst):
        P = self.P
        ident = self.c("ident")
        g_post, g_fpost = self.lpv("g_mix_post"), self.lpv("g_ffn_post")
        pb = self.pb
        with P.scope():
            ws = [P.sb("mw%d" % i, [128, 4096], BF16) for i in range(2)]
            wgs = [P.sb("mwg%d" % i, [128, 8, 128], BF16) for i in range(3)]
            wbr = [P.sb("mwb%d" % i, [128, 4, 2, 128], BF16) for i in range(2)]
            xs = P.sb("mx", [128, 4, D], F32)
            acc = P.sb("macc", [128, 512], F32)
            sg = [P.sb("msg%d" % i, [128, 512], F32) for i in range(2)]
            mTs = [P.sb("mmT%d" % i, [128, 8, 512], BF16) for i in range(2)]
            junk = P.sb("mjunk", [128, D], BF16)
            tmp = P.sb("mtmp", [128, D], F32)
            h2T = P.sb("mh2T", [128, 8, 512], BF16)
            aT = P.sb("maT", [128, 32, 512], BF16)
            rl = [P.sb("mrl%d" % i, [128, 512], F32) for i in range(2)]
            ss = P.sb("mss", [128, 8], F32)
            rs = P.sb("mrs", [128, 4], F32)
            wi = [0]

            def wload(src, pat=None, **kw):
                t = ws[wi[0] % 2]
                wi[0] += 1
                P.dma("sp", t[:, :].r(pat, **kw) if pat else t[:, :], src)
                return t

            def gates(tb):
                t0 = tb * 512
                mT = mTs[tb % 2]
                gi = [0]

                def gload(cc, g):
                    t = wgs[gi[0] % 3]
                    gi[0] += 1
                    P.dma("sp", t[:, :, :], self.wb_gate[l, cc, :, g])
                    return t
                nxt = gload(0, 0)
                for cc in range(8):
                    wb_ = wbr[cc % 2]
                    P.dma("sp", wb_[:, :, :, :], self.wb_br[l, cc])
                    for g in range(4):
                        wg = nxt
                        if not (cc == 7 and g == 3):
                            nxt = gload(cc + (g + 1) // 4, (g + 1) % 4)
                        gp, bp = pb[(2 * g) % 4], pb[(2 * g + 1) % 4]
                        for k in range(8):
                            P.mm(gp[:, :], wg[:, k, :], self.hT[:, k, t0:t0 + 512],
                                 start=(k == 0), stop=(k == 7))
                        for k in range(2):
                            P.mm(bp[:, :], wb_[:, g, k, :], self.yT[g][:, k, t0:t0 + 512], start=(k == 0), stop=(k == 1))
                        sgt = sg[g % 2]
                        P.act(sgt[:, :], gp[:, :], AF.Sigmoid)
                        if g == 0:
                            P.tt("dve", acc[:, :], sgt[:, :], bp[:, :], ALU.mult)
                        else:
                            P.tt("dve", sgt[:, :], sgt[:, :], bp[:, :], ALU.mult)
                            if g < 3:
                                P.tt("pool", acc[:, :], acc[:, :], sgt[:, :], ALU.add)
                            else:
                                P.tt("pool", mT[:, cc, :], acc[:, :], sgt[:, :], ALU.add)
                        yield

            def rest(tb):
                t0 = tb * 512
                r0 = s * T + t0
                mT = mTs[tb % 2]
                P.dma("sp", xs[:, :, :], xsrc[r0:r0 + 512, :].rearrange("(j p) c -> p j c", p=128))
                for jp in range(2):
                    for half in range(2):
                        wo = wload(self.wb_out[l, :, half * 512:(half + 1) * 512].rearrange("(k p) c -> p k c", p=128),
                                   "p (k c) -> p k c", k=8)
                        for j in (2 * jp, 2 * jp + 1):
                            bank = pb[4 + half * 2 + j % 2]
                            for k in range(8):
                                P.mm(bank[:, :], mT[:, k, j * 128:(j + 1) * 128], wo[:, k * 512:(k + 1) * 512],
                                     start=(k == 0), stop=(k == 7))
                    yield
                    for j in (2 * jp, 2 * jp + 1):
                        bk = [pb[4 + half * 2 + j % 2] for half in range(2)]
                        for half in range(2):
                            P.act(junk[:, half * 512:(half + 1) * 512], bk[half][:, :], AF.Square, accum=ss[:, half:half + 1])
                        P.tt("dve", ss[:, 2:3], ss[:, 0:1], ss[:, 1:2], ALU.add)
                        yield
                        P.act(rs[:, 0:1], ss[:, 2:3], AF.Ln, bias=EPS, scale=1.0 / D)
                        P.act(rs[:, 0:1], rs[:, 0:1], AF.Exp, scale=-0.5)
                        yield
                        for half in range(2):
                            cs = slice(half * 512, (half + 1) * 512)
                            P.stt("dve", tmp[:, cs], bk[half][:, :], rs[:, 0:1], g_post[:, cs], ALU.mult, ALU.mult)
                        yield
                        P.tt("pool", xs[:, j, :], xs[:, j, :], tmp[:, :], ALU.add)
                        P.act(junk[:, :], xs[:, j, :], AF.Square, accum=ss[:, 3:4])
                        yield
                        P.act(rs[:, 1:2], ss[:, 3:4], AF.Ln, bias=EPS, scale=1.0 / D)
                        P.act(rs[:, 1:2], rs[:, 1:2], AF.Exp, scale=-0.5)
                        yield
                        P.ts("dve", tmp[:, :], xs[:, j, :], rs[:, 1:2], ALU.mult)
                        yield
                        for g in range(2):
                            bank = bk[g]
                            for k in range(4):
                                kk = g * 4 + k
                                P.tr(bank[:, k * 128:(k + 1) * 128], tmp[:, kk * 128:(kk + 1) * 128], ident)
                            P.copy("act" if g == 0 else "dve", h2T[:, g * 4:(g + 1) * 4, j * 128:(j + 1) * 128],
                                   bank[:, :].r("p (k n) -> p k n", k=4))
                        yield
                for f0 in range(0, 32, 4):
                    wu = wload(self.wb_up[l, f0:f0 + 4].rearrange("f p k j -> p f (k j)"), "p (f c) -> p f c", f=4)
                    for fi in range(4):
                        f = f0 + fi
                        bank = pb[4 + f % 4]
                        for k in range(8):
                            P.mm(bank[:, :], wu[:, (fi * 8 + k) * 128:(fi * 8 + k + 1) * 128], h2T[:, k, :],
                                 start=(k == 0), stop=(k == 7))
                        r = rl[f % 2]
                        P.act(r[:, :], bank[:, :], AF.Relu)
                        P.tt("pool", aT[:, f, :], r[:, :], r[:, :], ALU.mult)
                    yield

            def down(tb):
                for k0 in range(0, 32, 4):
                    wd = wload(self.wb_down[l, k0 * 128:(k0 + 4) * 128, :].rearrange("(k p) c -> p k c", p=128), "p (k c) -> p k c", k=4)
                    for ki in range(4):
                        k = k0 + ki
                        for j in range(4):
                            for half in range(2):
                                P.mm(pb[2 * j + half][:, :], aT[:, k, j * 128:(j + 1) * 128],
                                     wd[:, ki * 1024 + half * 512:ki * 1024 + (half + 1) * 512],
                                     start=(k == 0), stop=(k == 31))

            def fnorm(tb):
                r0 = s * T + tb * 512
                for j in range(4):
                    for half in range(2):
                        P.act(junk[:, half * 512:(half + 1) * 512], pb[2 * j + half][:, :], AF.Square,
                              accum=ss[:, 4 + half:5 + half])
                    P.tt("dve", ss[:, 6:7], ss[:, 4:5], ss[:, 5:6], ALU.add)
                    yield
                    P.act(rs[:, 2:3], ss[:, 6:7], AF.Ln, bias=EPS, scale=1.0 / D)
                    P.act(rs[:, 2:3], rs[:, 2:3], AF.Exp, scale=-0.5)
                    yield
                    for half in range(2):
                        cs = slice(half * 512, (half + 1) * 512)
                        P.stt("dve", tmp[:, cs], pb[2 * j + half][:, :], rs[:, 2:3], g_fpost[:, cs], ALU.mult, ALU.mult)
                    yield
                    P.tt("pool", xs[:, j, :], xs[:, j, :], tmp[:, :], ALU.add)
                    yield
                P.dma("sp", xdst[r0:r0 + 512, :].rearrange("(j p) c -> p j c", p=128), xs[:, :, :])

            def step(g):
                try:
                    next(g)
                    return True
                except StopIteration:
                    return False

            def run_rr(gens):
                gens = list(gens)
                while gens:
                    for g in list(gens):
                        if not step(g):
                            gens.remove(g)

            run_rr([gates(0)])
            pend = gates(1)
            for tb in range(4):
                run_rr([rest(tb)] + ([pend] if pend is not None else []))
                down(tb)
                fn = fnorm(tb)
                pend = gates(tb + 2) if tb + 2 < 4 else None
                alive, nfn = True, 0
                while alive:
                    alive = step(fn)
                    nfn += 1
                    if nfn >= 7 and pend is not None and not step(pend):
                        pend = None

    def _proj_fm(self, wt, col0, out_writer, nchunks=4):
        P = self.P
        for c in range(nchunks):
            bank = self.pb[self._pj % 4]
            self._pj += 1
            for k in range(8):
                P.mm(bank[:, :], wt[:, k, col0:col0 + 128], self.hT[:, k, c * 512:(c + 1) * 512], start=(k == 0), stop=(k == 7))
            out_writer(c, bank[:, :])

    def _proj_tm(self, wt, col0, ncol, i, bank):
        P = self.P
        for k in range(8):
            P.mm(bank[:, 0:ncol], self.hT[:, k, i * 128:(i + 1) * 128], wt[:, k, col0:col0 + ncol], start=(k == 0), stop=(k == 7))

    def u_sb(self, l, s):
        P = self.P
        pb = self.pb
        self._pj = 0
        with P.scope():
            wt = P.sb("sw", [128, 8, 768], BF16)
            qT = P.sb("sq", [128, 2, T], BF16)
            kT = P.sb("sk", [128, 2, T], BF16)
            vv = P.sb("sv", [128, 16, 256], BF16)
            NS = 4
            E = [P.sb("sE%d" % i, [128, 512], F32) for i in range(NS)]
            Lp = [P.sb("sL%d" % i, [128, 512], BF16) for i in range(NS)]
            G = [P.sb("sG%d" % i, [128, 512], F32) for i in range(NS)]
            Wt = [P.sb("sW%d" % i, [128, 512], BF16) for i in range(NS)]
            Pacc = [P.sb("sPacc%d" % i, [128, 512], BF16) for i in range(NS)]
            P.dma("sp", wt[:, :, :], self.wb_in[l, :, OFF_SB:OFF_SB + 768].rearrange("(k p) c -> p k c", p=128))
            for hp in range(2):
                self._proj_fm(wt, hp * 128, lambda c, ps, hp=hp: P.ts("dve", qT[:, hp, c * 512:(c + 1) * 512], ps, 0.125, ALU.mult))
                self._proj_fm(wt, 256 + hp * 128, lambda c, ps, hp=hp: P.copy("act", kT[:, hp, c * 512:(c + 1) * 512], ps))
            for i in range(16):
                bank = pb[4 + i % 2]
                self._proj_tm(wt, 512, 256, i, bank)
                P.copy("act" if i % 2 else "dve", vv[:, i, :], bank[:, 0:256])

            def stream(h, qb, sl):
                hp, r = h // 2, slice((h % 2) * 64, (h % 2) * 64 + 64)
                q0 = qb * 512
                wb_, accb = pb[sl], pb[4 + sl]
                Es, Ls, Gs, Ws, Pa = E[sl], Lp[sl], G[sl], Wt[sl], Pacc[sl]
                P.memset("pool", Pa[:, :], 0.0)
                nk = 4 * qb + 4
                for kt in range(nk - 1, -1, -1):
                    c0 = max(0, kt - 4 * qb) * 128
                    first = (kt == nk - 1)
                    P.mm(wb_[:, c0:512], kT[r, hp, kt * 128:(kt + 1) * 128], qT[r, hp, q0 + c0:q0 + 512])
                    yield
                    P.act(Es[:, c0:512], wb_[:, c0:512], AF.Exp)
                    yield
                    if kt >= 4 * qb:
                        P.tt("pool", Es[:, c0:c0 + 128], Es[:, c0:c0 + 128], self.c("m_strict"), ALU.mult)
                        yield
                    P.act(Ls[:, c0:512], Es[:, c0:512], AF.Ln, bias=1.0)
                    yield
                    P.mm(wb_[:, c0:512], self.uincl_bf[:, :], Ls[:, c0:512], start=True, stop=first)
                    if not first:
                        P.mm(wb_[:, c0:512], self.ones_bf[:, :], Pa[:, c0:512], start=False, stop=True)
                    yield
                    P.act(Gs[:, c0:512], wb_[:, c0:512], AF.Exp, scale=-1.0)
                    if c0 > 0:
                        P.memset("pool", Ws[:, 0:c0], 0.0)
                    yield
                    P.tt("dve", Ws[:, c0:512], Es[:, c0:512], Gs[:, c0:512], ALU.mult)
                    if kt > 0:
                        P.tt("pool", Pa[:, c0:512], Pa[:, c0:512], Ls[:, c0:512], ALU.add)
                    yield
                    P.mm(accb[:, :], vv[:, kt, hp * 128:(hp + 1) * 128], Ws[:, :], start=first, stop=(kt == 0))
                    yield
                P.copy("act", self.yT[2][r, hp, q0:q0 + 512], accb[r, :])
                yield

            todo = sorted([(h, qb) for h in range(4) for qb in range(4)], key=lambda t: -t[1])
            active = {}
            while todo or active:
                for sl in range(NS):
                    if sl not in active and todo:
                        h, qb = todo.pop(0)
                        active[sl] = stream(h, qb, sl)
                for sl in list(active):
                    try:
                        next(active[sl])
                    except StopIteration:
                        del active[sl]

    def u_moba(self, l, s):
        P = self.P
        pb = self.pb
        self._pj = 0
        ident = self.c("ident")
        with P.scope():
            wt = P.sb("bw", [128, 8, 768], BF16)
            q32 = P.sb("bq32", [128, 2, T], F32)
            qT = P.sb("bq", [128, 2, T], BF16)
            kT = P.sb("bk", [128, 2, T], BF16)
            vv = P.sb("bv", [128, 4, 16, 128], BF16)
            ksum = P.sb("bks", [128, 2, 8], F32)
            gm = P.sb("bgm", [128, 64, 8], F32)
            thr = P.sb("bthr", [128, 64, 8], F32)
            sel = P.sb("bsel", [128, 512], F32)
            aug = P.sb("baug", [128, T], BF16)
            NS = 4
            Pt = [P.sb("bP%d" % i, [128, 512], BF16) for i in range(NS)]
            rden = [P.sb("brd%d" % i, [128, 512], F32) for i in range(NS)]
            P.memset("pool", vv[:, :, :, :], 1.0)
            self.mcst = P.sb("bmcst", [128, NCONST - NCONST_RES], F32)
            P.dma("sp", self.mcst[:, :], self.consts[:, NCONST_RES:NCONST])
            P.dma("sp", wt[:, :, :], self.wb_in[l, :, OFF_MOBA:OFF_MOBA + 768].rearrange("(k p) c -> p k c", p=128))
            P.copy("pool", aug[32:34, :].r("p (a t) -> p a t", a=4), self.c("augrows")[32:34, :].un(1).bc([2, 4, 512]))
            for hp in range(2):
                def wq(c, ps, hp=hp):
                    P.ts("dve", q32[:, hp, c * 512:(c + 1) * 512], ps, 0.125, ALU.mult)
                    P.copy("pool", qT[:, hp, c * 512:(c + 1) * 512], q32[:, hp, c * 512:(c + 1) * 512])
                self._proj_fm(wt, hp * 128, wq)

                def wk(c, ps, hp=hp):
                    P.copy("act", kT[:, hp, c * 512:(c + 1) * 512], ps)
                    P.reduce("dve", ksum[:, hp, 2 * c:2 * c + 2], ps.r("p (n t) -> p n t", n=2))
                self._proj_fm(wt, 256 + hp * 128, wk)
            for i in range(16):
                bank = pb[4 + i % 2]
                self._proj_tm(wt, 512, 256, i, bank)
                v4 = bank[:, 0:256].r("p (h d) -> p h d", h=4)
                P.copy("act", vv[:, 0:4:2, i, 0:64], v4[:, 0:4:2, :])
                P.copy("dve", vv[:, 1:4:2, i, 64:128], v4[:, 1:4:2, :])
            if "moba_stop1" in self.dbg:
                return
            for hh in range(2):
                gb = pb[6 + hh]
                r = slice(hh * 64, hh * 64 + 64)
                for i in range(16):
                    for hp in range(2):
                        o = (i * 2 + hp) * 8
                        P.mm(gb[:, o:o + 8], q32[r, hp, i * 128:(i + 1) * 128], ksum[r, hp, :])
                P.tt("dve", gm[:, :, :].r("p (i hp hh) n -> p i hp hh n", hp=2, hh=2)[:, :, :, hh, :],
                     gb[:, 0:256].r("p (i hp n) -> p i hp n", hp=2, n=8),
                     self.c("pastbias").r("p (i hp hh n) -> p i hp hh n", hp=2, hh=2, n=8)[:, :, :, hh, :], ALU.add)
            for a in range(64):
                P.max8(thr[:, a, :], gm[:, a, :])
            P.tt("dve", sel[:, :].r("p (a n) -> p a n", n=8), gm[:, :, :], thr[:, :, 2:3].bc([128, 64, 8]), ALU.is_ge)
            P.tt("dve", sel[:, :], sel[:, :], self.c("pastm"), ALU.mult)
            P.tt("dve", sel[:, :], sel[:, :], self.c("ownm"), ALU.add)
            P.ts("dve", sel[:, :], sel[:, :], -1.0, ALU.add, 1000.0, ALU.mult)
            if "moba_stop2" in self.dbg:
                return
            for g4 in range(4):
                tb_ = pb[g4 % 2]
                for j in range(4):
                    i = g4 * 4 + j
                    P.tr(tb_[0:32, j * 128:(j + 1) * 128], sel[:, i * 32:(i + 1) * 32], ident)
                P.copy("act", aug[0:32, g4 * 512:(g4 + 1) * 512], tb_[0:32, :])
            alibi = self.c("alibi")

            def stream(h, qb, sl):
                hp, r = h // 2, slice((h % 2) * 64, (h % 2) * 64 + 64)
                ro = slice(64 - (h % 2) * 64, 128 - (h % 2) * 64)
                q0 = qb * 512
                sb_, accb = pb[sl], pb[4 + sl]
                Ps = Pt[sl]
                nk = 4 * qb + 4
                for kt in range(nk):
                    c0 = max(0, kt - 4 * qb) * 128
                    n = kt // 2
                    P.mm(sb_[:, c0:512], kT[r, hp, kt * 128:(kt + 1) * 128], qT[r, hp, q0 + c0:q0 + 512], start=True, stop=False)
                    P.mm(sb_[:, c0:512], self.sel_bf[0:34, h * 8 + n, :], aug[0:34, q0 + c0:q0 + 512], start=False, stop=True)
                    yield
                    dl = kt - 4 * qb + 12
                    P.act(Ps[:, c0:512], sb_[:, c0:512], AF.Exp, bias=alibi[:, h * 16 + dl:h * 16 + dl + 1])
                    yield
                    if kt >= 4 * qb:
                        P.tt("pool", Ps[:, c0:c0 + 128], Ps[:, c0:c0 + 128], self.c("m_incl"), ALU.mult)
                        if c0 > 0:
                            P.memset("pool", Ps[:, 0:c0], 0.0)
                        yield
                    P.mm(accb[:, :], vv[:, h, kt, :], Ps[:, :], start=(kt == 0), stop=(kt == nk - 1))
                    yield
                P.recip(rden[sl][r, :], accb[ro, :])
                yield
                P.tt("dve", self.yT[1][r, hp, q0:q0 + 512], accb[r, :], rden[sl][r, :], ALU.mult)
                yield

            todo = sorted([(h, qb) for h in range(0 if "moba_noattn" not in self.dbg else 4, 4) for qb in range(4)], key=lambda t: -t[1])
            active = {}
            while todo or active:
                for sl in range(NS):
                    if sl not in active and todo:
                        h, qb = todo.pop(0)
                        active[sl] = stream(h, qb, sl)
                for sl in list(active):
                    try:
                        next(active[sl])
                    except StopIteration:
                        del active[sl]

    def _conv_chunk(self, wt, col0, m, c, pre, acc, cw, out_view, bias, eng):
        P = self.P
        cb_ = getattr(self, "_cbanks", (0, 1, 2, 3))
        bank = self.pb[cb_[self._pj % len(cb_)]]
        self._pj += 1
        for k in range(8):
            P.mm(bank[:, :], wt[:, k, col0:col0 + 128], self.hT[:, k, c * 512:(c + 1) * 512], start=(k == 0), stop=(k == 7))
        if c == 0:
            P.memset(eng, pre[:, m, 0:3], 0.0)
        else:
            P.copy(eng, pre[:, m, 0:3], pre[:, m, 512:515])
        P.copy("act", pre[:, m, 3:515], bank[:, :])
        P.ts(eng, acc[:, :], pre[:, m, 3:515], cw[:, m * 4 + 3:m * 4 + 4], ALU.mult)
        for j in range(1, 4):
            P.stt("dve", acc[:, :], pre[:, m, 3 - j:515 - j], cw[:, m * 4 + 3 - j:m * 4 + 4 - j], acc[:, :], ALU.mult, ALU.add)
        if bias is None:
            P.act(out_view, acc[:, :], AF.Silu)
        else:
            P.act(out_view, acc[:, :], AF.Silu, bias=bias)

    def u_ssd(self, l, s):
        P = self.P
        pb = self.pb
        self._pj = 0
        ident = self.c("ident")
        ones = self.c("ones")
        with P.scope():
            wt = P.sb("dw", [128, 8, 1028], BF16)
            pre = P.sb("dpre", [128, 6, 515], F32)
            accs = [P.sb("dacc%d" % i, [128, 512], F32) for i in range(2)]
            xbc = [P.sb("dxbc%d" % i, [128, 6, 512], F32) for i in range(2)]
            zs = P.sb("dzs", [128, 4, 256], F32)
            dt = P.sb("ddt", [128, 16, 4], F32)
            gg = P.sb("dgg", [128, 16, 4], F32)
            aneg = P.sb("daneg", [128, 4], F32)
            tok = [P.sb("dtok%d" % i, [128, 512], F32) for i in range(2)]
            gtri = P.sb("dgtri", [128, 4, 128], F32)
            Dm = P.sb("dD", [128, 4, 128], F32)
            lm = P.sb("dlm", [128, 4, 128], F32)
            Mms = [P.sb("dM%d" % i, [128, 4, 128], F32) for i in range(2)]
            sms = [P.sb("dsm%d" % i, [128, 20], F32) for i in range(2)]
            smb = P.sb("dsmb", [128, 4], F32)
            xws = [P.sb("dxw%d" % i, [128, 256], F32) for i in range(2)]
            dx = P.sb("ddx", [128, 256], F32)
            hst = P.sb("dhst", [128, 256], F32)
            yb = [P.sb("dy%d" % i, [128, 256], F32) for i in range(2)]
            junk = P.sb("djunk", [128, 256], F32)
            zsb = [P.sb("dzs%d" % i, [128, 4, 256], F32) for i in range(2)]
            P.dma("sp", wt[:, :, :], self.wb_in[l, :, OFF_SSD:OFF_SSD + 1028].rearrange("(k p) c -> p k c", p=128))
            cw, cb = self.lpv("ssd_conv"), self.lpv("ssd_cb")
            dtb = pb[7]
            for i in range(16):
                for k in range(8):
                    P.mm(dtb[:, i * 4:(i + 1) * 4], self.hT[:, k, i * 128:(i + 1) * 128], wt[:, k, 1024:1028], start=(k == 0), stop=(k == 7))
            P.tt("dve", dt[:, :, :], dtb[:, 0:64].r("p (i h) -> p i h", h=4), self.lpv("ssd_dtb").un(1).bc([128, 16, 4]), ALU.add)
            P.act(dt[:, :, :], dt[:, :, :], AF.Exp)
            P.act(dt[:, :, :], dt[:, :, :], AF.Ln, bias=1.0)
            P.act(aneg[:, :], self.lpv("ssd_alog"), AF.Exp)
            P.stt("dve", gg[:, :, :], dt[:, :, :], -1.0, aneg[:, :].un(1).bc([128, 16, 4]), ALU.mult, ALU.mult)
            P.memset("pool", hst[:, :], 0.0)

            def chunk_front(c):
                xb = xbc[c % 2]
                self._cbanks = (0, 2, 3)
                for m in range(6):
                    self._conv_chunk(wt, 256 + m * 128, m, c, pre, accs[m % 2], cw, xb[:, m, :], cb[:, m:m + 1],
                                     "dve" if m % 2 == 0 else "pool")
                self._cbanks = (0, 1, 2, 3)
                for j in range(4):
                    i = c * 4 + j
                    bank = pb[4 + j % 2]
                    self._proj_tm(wt, 0, 256, i, bank)
                    P.act(zsb[c % 2][:, j, :], bank[:, 0:256], AF.Silu)

            def front(i):
                c, j = i // 4, i % 4
                xb = xbc[c % 2]
                cs = slice(j * 128, (j + 1) * 128)
                b = i % 2
                tk, sm, Mm, xw = tok[b], sms[b], Mms[b], xws[b]
                tb_ = pb[4]
                for m in range(4):
                    P.tr(tb_[:, m * 128:(m + 1) * 128], xb[:, m, cs], ident)
                sb_ = pb[5]
                P.mm(sb_[:, 0:4], self.c("tri128"), gg[:, i, :])
                P.tt("pool", gtri[:, :, :], self.c("tri128").un(1).bc([128, 4, 128]), gg[:, i, :].un(2).bc([128, 4, 128]), ALU.mult)
                yield
                P.copy("act", tk[:, :], tb_[:, :])
                P.copy("dve", sm[:, 0:4], sb_[:, 0:4])
                ab = pb[6]
                P.mm(ab[:, :], ones, gtri[:, :, :].r("p h l -> p (h l)"))
                yield
                P.copy("dve", sm[:, 4:8], ab[:, :].r("p (h l) -> p h l", h=4)[:, :, 127])
                P.tt("dve", Dm[:, :, :], ab[:, :].r("p (h l) -> p h l", h=4), sm[:, 0:4].un(2).bc([128, 4, 128]), ALU.subtract)
                P.act(sm[:, 8:12], sm[:, 0:4], AF.Exp)
                yield
                P.tt("pool", Dm[:, :, :], Dm[:, :, :], self.c("neg_ssd").un(1).bc([128, 4, 128]), ALU.add)
                P.act(sm[:, 12:16], sm[:, 4:8], AF.Exp)
                P.tt("dve", sm[:, 16:20], sm[:, 4:8], sm[:, 0:4], ALU.subtract)
                scb = pb[0]
                for g in range(2):
                    P.mm(scb[:, g * 128:(g + 1) * 128], xb[:, 2 + g, cs], xb[:, 4 + g, cs])
                yield
                P.act(lm[:, :, :], Dm[:, :, :], AF.Exp)
                P.act(sm[:, 16:20], sm[:, 16:20], AF.Exp)
                yield
                P.tt("dve", sm[:, 16:20], sm[:, 16:20], dt[:, i, :], ALU.mult)
                for h in range(4):
                    P.stt("dve", Mm[:, h, :], lm[:, h, :], dt[:, i, h:h + 1], scb[:, (h // 2) * 128:(h // 2 + 1) * 128], ALU.mult, ALU.mult)
                yield
                P.tt("pool", xw[:, :].r("p (h e) -> p h e", h=4), tk[:, 0:256].r("p (h e) -> p h e", h=4),
                     sm[:, 16:20].un(2).bc([128, 4, 64]), ALU.mult)
                yb_ = pb[1] if b == 0 else pb[7]
                for h in range(4):
                    P.mm(yb_[:, h * 64:(h + 1) * 64], Mm[:, h, :], tk[:, h * 64:(h + 1) * 64])
                yield

            def back(i):
                c, j = i // 4, i % 4
                xb = xbc[c % 2]
                cs = slice(j * 128, (j + 1) * 128)
                b = i % 2
                tk, sm, xw = tok[b], sms[b], xws[b]
                yb_ = pb[1] if b == 0 else pb[7]
                for h in range(4):
                    P.mm(yb_[:, 256 + h * 64:256 + (h + 1) * 64], xb[:, 4 + h // 2, cs], hst[:, h * 64:(h + 1) * 64])
                hb = pb[2]
                for g in range(2):
                    P.mm(hb[:, g * 128:(g + 1) * 128], tk[:, 256 + g * 128:256 + (g + 1) * 128], xw[:, g * 128:(g + 1) * 128])
                P.tt("pool", dx[:, :].r("p (h e) -> p h e", h=4), tk[:, 0:256].r("p (h e) -> p h e", h=4),
                     self.lpv("ssd_d").un(2).bc([128, 4, 64]), ALU.mult)
                yield
                y = yb[b]
                P.tt("dve", y[:, :].r("p (h e) -> p h e", h=4), yb_[:, 256:512].r("p (h e) -> p h e", h=4),
                     sm[:, 8:12].un(2).bc([128, 4, 64]), ALU.mult)
                P.tt("pool", hst[:, :].r("p (h e) -> p h e", h=4), hst[:, :].r("p (h e) -> p h e", h=4),
                     sm[:, 12:16].un(2).bc([128, 4, 64]), ALU.mult)
                yield
                P.tt("dve", y[:, :], y[:, :], yb_[:, 0:256], ALU.add)
                P.tt("dve", hst[:, :], hst[:, :], hb[:, 0:256], ALU.add)
                yield
                P.tt("pool", y[:, :], y[:, :], dx[:, :], ALU.add)
                yield
                P.tt("pool", y[:, :], y[:, :], zsb[c % 2][:, j, :], ALU.mult)
                yield
                P.act(junk[:, :], y[:, :], AF.Square, accum=smb[:, 0:1])
                yield
                P.act(smb[:, 1:2], smb[:, 0:1], AF.Ln, bias=EPS, scale=1.0 / 256)
                P.act(smb[:, 1:2], smb[:, 1:2], AF.Exp, scale=-0.5)
                yield
                P.stt("dve", y[:, :], y[:, :], smb[:, 1:2], self.lpv("ssd_norm"), ALU.mult, ALU.mult)
                yield
                ob = pb[3]
                for k in range(2):
                    P.tr(ob[:, k * 128:(k + 1) * 128], y[:, k * 128:(k + 1) * 128], ident)
                yield
                P.copy("act", self.yT[3][:, :, i * 128:(i + 1) * 128], ob[:, 0:256].r("p (k t) -> p k t", k=2))
                yield

            def run_rr(gens):
                gens = list(gens)
                while gens:
                    for g in list(gens):
                        try:
                            next(g)
                        except StopIteration:
                            gens.remove(g)

            chunk_front(0)
            run_rr([front(0)])
            for i in range(16):
                if i % 4 == 0 and i + 4 < 16:
                    chunk_front(i // 4 + 1)
                gens = [back(i)]
                if i + 1 < 16:
                    gens.append(front(i + 1))
                run_rr(gens)

    def u_gdn(self, l, s):
        P = self.P
        pb = self.pb
        self._pj = 0
        ident = self.c("ident")
        ones = self.c("ones")
        with P.scope():
            wt = P.sb("gw", [128, 8, 1032], BF16)
            pre = P.sb("gpre", [128, 6, 515], F32)
            accs = [P.sb("gacc%d" % i, [128, 512], F32) for i in range(2)]
            qkv = [P.sb("gqkv%d" % i, [128, 6, 512], F32) for i in range(2)]
            sq = P.sb("gsq", [128, 512], F32)
            rsn = P.sb("grsn", [128, 512], F32)
            bg = P.sb("gbg", [128, 16, 8], F32)
            beta = P.sb("gbeta", [128, 16, 4], F32)
            lnb = P.sb("glnb", [128, 16, 4], F32)
            gg = P.sb("ggg", [128, 16, 4], F32)
            aneg = P.sb("ganeg", [128, 4], F32)
            kv = [P.sb("gkv%d" % i, [128, 512], F32) for i in range(2)]
            r1 = P.sb("gr1", [128, 4, 128], F32)
            r2 = P.sb("gr2", [128, 4, 128], F32)
            X2 = P.sb("gX2", [128, 4, 128], F32)
            X3 = P.sb("gX3", [128, 4, 128], F32)
            egB = P.sb("gegB", [128, 4, 128], F32)
            smps = [P.sb("gsmp%d" % i, [128, 20], F32) for i in range(2)]
            sms = P.sb("gsms", [128, 8], F32)
            zsb = [P.sb("gzs%d" % i, [128, 4, 256], F32) for i in range(2)]
            qd = [P.sb("gqd%d" % i, [128, 2, 128], F32) for i in range(2)]
            kdec = [P.sb("gkd%d" % i, [128, 256], F32) for i in range(2)]
            kbg = P.sb("gkbg", [128, 256], F32)
            vb = P.sb("gvb", [128, 256], F32)
            NT = [P.sb("gNT%d" % h, [128, 128], F32) for h in range(4)]
            Nn = [P.sb("gN%d" % h, [128, 128], F32) for h in range(4)]
            Ma = [P.sb("gMa%d" % h, [128, 128], F32) for h in range(4)]
            MTa = [P.sb("gMTa%d" % h, [128, 128], F32) for h in range(4)]
            PT = [P.sb("gPT%d" % h, [128, 128], F32) for h in range(4)]
            Aq = [[P.sb("gAq%d_%d" % (b, h), [128, 128], F32) for h in range(4)] for b in range(2)]
            us = [P.sb("gu%d" % i, [128, 4, 64], F32) for i in range(2)]
            wTs = [P.sb("gwT%d" % i, [128, 2, 128], F32) for i in range(2)]
            gts = [P.sb("ggt%d" % i, [128, 4, 2], F32) for i in range(2)]
            S = P.sb("gS", [128, 2, 64], F32)
            vnew = P.sb("gvn", [128, 4, 64], F32)
            ot = P.sb("got", [128, 4, 64], F32)
            on = P.sb("gon", [128, 256], F32)
            P.dma("sp", wt[:, :, :], self.wb_in[l, :, OFF_GDN:OFF_GDN + 1032].rearrange("(k p) c -> p k c", p=128))
            cw = self.lpv("gdn_conv")
            bgb = pb[7]
            for i in range(16):
                for k in range(8):
                    P.mm(bgb[:, i * 8:(i + 1) * 8], self.hT[:, k, i * 128:(i + 1) * 128], wt[:, k, 1024:1032], start=(k == 0), stop=(k == 7))
            P.copy("dve", bg[:, :, :], bgb[:, 0:128].r("p (i c) -> p i c", c=8))
            P.act(lnb[:, :, :], bg[:, :, 0:4], AF.Exp, scale=-1.0)
            P.act(lnb[:, :, :], lnb[:, :, :], AF.Ln, bias=1.0)
            P.ts("dve", lnb[:, :, :], lnb[:, :, :], -1.0, ALU.mult)
            P.act(beta[:, :, :], lnb[:, :, :], AF.Exp)
            P.tt("dve", gg[:, :, :], bg[:, :, 4:8], self.lpv("gdn_dtb").un(1).bc([128, 16, 4]), ALU.add)
            P.act(gg[:, :, :], gg[:, :, :], AF.Exp)
            P.act(gg[:, :, :], gg[:, :, :], AF.Ln, bias=1.0)
            P.act(aneg[:, :], self.lpv("gdn_alog"), AF.Exp)
            P.stt("dve", gg[:, :, :], gg[:, :, :], -1.0, aneg[:, :].un(1).bc([128, 16, 4]), ALU.mult, ALU.mult)
            P.memset("pool", S[:, :, :], 0.0)

            def prep_common(i, qk):
                j = i % 4
                cs = slice(j * 128, (j + 1) * 128)
                b = i % 2
                kvt = kv[b]
                smp = smps[b]
                tb_ = pb[4]
                for m in range(4):
                    P.tr(tb_[:, m * 128:(m + 1) * 128], qk[:, 2 + m, cs], ident)
                P.copy("act", kvt[:, :], tb_[:, :])
                sb_ = pb[3]
                P.mm(sb_[:, 0:4], self.c("tri_bd"), gg[:, i, :])
                P.copy("dve", smp[:, 0:4], sb_[:, 0:4])
                P.tt("pool", r1[:, :, :], self.c("tri_bd").un(1).bc([128, 4, 128]), gg[:, i, :].un(2).bc([128, 4, 128]), ALU.mult)
                P.tt("pool", r2[:, :, :], self.c("ident").un(1).bc([128, 4, 128]), lnb[:, i, :].un(2).bc([128, 4, 128]), ALU.mult)
                P.tt("pool", r2[:, :, :], r2[:, :, :], r1[:, :, :], ALU.add)
                gcB, gcbB = pb[0], pb[1]
                P.mm(gcB[:, :], ones, r1[:, :, :].r("p h c -> p (h c)"))
                P.mm(gcbB[:, :], ones, r2[:, :, :].r("p h c -> p (h c)"))
                gc_b = smp[:, 0:4].un(2).bc([128, 4, 128])
                P.tt("dve", X2[:, :, :], gcB[:, :].r("p (h c) -> p h c", h=4), gc_b, ALU.subtract)
                P.tt("pool", X2[:, :, :], X2[:, :, :], self.c("neg_incl_bd").un(1).bc([128, 4, 128]), ALU.add)
                P.act(X2[:, :, :], X2[:, :, :], AF.Exp)
                P.tt("dve", X3[:, :, :], gcbB[:, :].r("p (h c) -> p h c", h=4), gc_b, ALU.subtract)
                P.tt("pool", X3[:, :, :], X3[:, :, :], self.c("neg_strict_bd").un(1).bc([128, 4, 128]), ALU.add)
                P.act(X3[:, :, :], X3[:, :, :], AF.Exp)
                P.act(egB[:, :, :], gcB[:, :].r("p (h c) -> p h c", h=4), AF.Exp)
                gt = gts[b]
                P.act(gt[:, :, :], gcB[:, :].r("p (h c) -> p h c", h=4)[:, :, 63:128:64], AF.Exp)
                P.copy("dve", smp[0:64, 4:8], gcB[0:64, :].r("p (h c) -> p h c", h=4)[:, :, 63])
                P.copy("dve", smp[64:128, 4:8], gcB[64:128, :].r("p (h c) -> p h c", h=4)[:, :, 127])
                P.act(smp[:, 8:12], smp[:, 0:4], AF.Exp)
                P.tt("dve", smp[:, 12:16], smp[:, 4:8], smp[:, 0:4], ALU.subtract)
                P.act(smp[:, 12:16], smp[:, 12:16], AF.Exp)
                P.tt("dve", smp[:, 16:20], smp[:, 8:12], beta[:, i, :], ALU.mult)
                kd = kdec[b]
                k4 = kvt[:, 0:256].r("p (h d) -> p h d", h=4)
                P.tt("pool", kd[:, :].r("p (h d) -> p h d", h=4), k4, smp[:, 12:16].un(2).bc([128, 4, 64]), ALU.mult)
                P.tt("pool", kbg[:, :].r("p (h d) -> p h d", h=4), k4, smp[:, 16:20].un(2).bc([128, 4, 64]), ALU.mult)
                P.tt("pool", vb[:, :].r("p (h d) -> p h d", h=4), kvt[:, 256:512].r("p (h d) -> p h d", h=4),
                     beta[:, i, :].un(2).bc([128, 4, 64]), ALU.mult)
                qdt = qd[b]
                for hp in range(2):
                    for hh in range(2):
                        r = slice(hh * 64, hh * 64 + 64)
                        P.tt("pool", qdt[r, hp, :], qk[r, hp, cs], egB[r, hp * 2 + hh, :], ALU.mult)

            def head_chain(i, qk, h):
                j = i % 4
                cs = slice(j * 128, (j + 1) * 128)
                b = i % 2
                hp, r = h // 2, slice((h % 2) * 64, (h % 2) * 64 + 64)
                hb = pb[h]
                P.mm(hb[:, 0:128], qk[r, 2 + hp, cs], qk[r, 2 + hp, cs])
                P.mm(hb[:, 128:256], qk[r, 2 + hp, cs], qk[r, hp, cs])
                yield
                P.stt("dve", NT[h][:, :], hb[:, 0:128], -1.0, X3[:, h, :], ALU.mult, ALU.mult)
                P.tt("dve", Aq[b][h][:, :], hb[:, 128:256], X2[:, h, :], ALU.mult)
                yield
                P.tr(hb[:, 256:384], NT[h][:, :], ident)
                P.tt("pool", PT[h][:, :], NT[h][:, :], ident, ALU.add)
                yield
                P.copy("act", Nn[h][:, :], hb[:, 256:384])
                yield
                M, MT = Nn[h], NT[h]
                for lvl in range(5):
                    P.mm(hb[:, 0:128], MT[:, :], M[:, :])
                    if lvl < 4:
                        P.mm(hb[:, 128:256], M[:, :], MT[:, :])
                    yield
                    M2 = Ma[h] if lvl % 2 == 0 else Nn[h]
                    MT2 = MTa[h] if lvl % 2 == 0 else NT[h]
                    P.copy("act", M2[:, :], hb[:, 0:128])
                    if lvl < 4:
                        P.copy("dve", MT2[:, :], hb[:, 128:256])
                    yield
                    P.mm(hb[:, 256:384], M2[:, :], PT[h][:, :])
                    yield
                    P.tt("dve", PT[h][:, :], PT[h][:, :], hb[:, 256:384], ALU.add)
                    yield
                    M, MT = M2, MT2
                P.mm(hb[:, 0:64], PT[h][:, :], vb[:, h * 64:(h + 1) * 64])
                P.mm(hb[:, 128:256], kbg[:, hp * 128:(hp + 1) * 128], PT[h][:, :])
                yield
                P.copy("act", us[b][:, h, :], hb[:, 0:64])
                P.copy("dve", wTs[b][r, hp, :], hb[r, 128:256])
                yield

            def scan(i):
                b = i % 2
                j4 = i % 4
                for jc in range(2):
                    rj = slice(jc * 64, jc * 64 + 64)
                    wsb, ob, snb = pb[5], pb[6], pb[7]
                    for hh in range(2):
                        for h in (hh, hh + 2):
                            hp, r = h // 2, slice((h % 2) * 64, (h % 2) * 64 + 64)
                            P.mm(wsb[:, h * 64:(h + 1) * 64], wTs[b][r, hp, :], S[r, hp, :])
                    yield
                    P.tt("dve", vnew[rj, :, :], us[b][rj, :, :], wsb[rj, 0:256].r("p (h e) -> p h e", h=4), ALU.subtract)
                    yield
                    for h in range(4):
                        hp, r = h // 2, slice((h % 2) * 64, (h % 2) * 64 + 64)
                        P.mm(ob[:, h * 64:(h + 1) * 64], qd[b][r, hp, :], S[r, hp, :], start=True, stop=False)
                        P.mm(ob[:, h * 64:(h + 1) * 64], Aq[b][h][rj, :], vnew[rj, h, :], start=False, stop=True)
                    for h in range(4):
                        hp = h // 2
                        P.mm(snb[:, h * 64:(h + 1) * 64], kdec[b][rj, hp * 128:(hp + 1) * 128], vnew[rj, h, :])
                    yield
                    for h in range(4):
                        hp, r = h // 2, slice((h % 2) * 64, (h % 2) * 64 + 64)
                        P.stt("dve", S[r, hp, :], S[r, hp, :], gts[b][r, h, jc:jc + 1], snb[r, h * 64:(h + 1) * 64], ALU.mult, ALU.add)
                    P.copy("act", ot[rj, :, :], ob[rj, 0:256].r("p (h e) -> p h e", h=4))
                    yield
                P.tt("pool", on[:, :], ot[:, :, :].r("p h e -> p (h e)"), ot[:, :, :].r("p h e -> p (h e)"), ALU.mult)
                P.reduce("dve", sms[:, 0:4], on[:, :].r("p (h e) -> p h e", h=4))
                yield
                P.act(sms[:, 4:8], sms[:, 0:4], AF.Ln, bias=EPS, scale=1.0 / 64)
                P.act(sms[:, 4:8], sms[:, 4:8], AF.Exp, scale=-0.5)
                yield
                P.tt("pool", on[:, :].r("p (h e) -> p h e", h=4), ot[:, :, :], sms[:, 4:8].un(2).bc([128, 4, 64]), ALU.mult)
                P.tt("pool", on[:, :].r("p (h e) -> p h e", h=4), on[:, :].r("p (h e) -> p h e", h=4),
                     self.lpv("gdn_norm").un(1).bc([128, 4, 64]), ALU.mult)
                P.tt("pool", on[:, :], on[:, :], zsb[(i // 4) % 2][:, j4, :], ALU.mult)
                yield
                tb2 = pb[4]
                for k in range(2):
                    P.tr(tb2[:, 256 + k * 128:256 + (k + 1) * 128], on[:, k * 128:(k + 1) * 128], ident)
                P.copy("act", self.yT[0][:, :, i * 128:(i + 1) * 128], tb2[:, 256:512].r("p (k t) -> p k t", k=2))
                yield

            def chunk_front(c):
                qk = qkv[c % 2]
                self._cbanks = (4,)
                for m in range(6):
                    self._conv_chunk(wt, m * 128, m, c, pre, accs[m % 2], cw, qk[:, m, :], None, "dve" if m % 2 == 0 else "pool")
                    yield
                self._cbanks = (0, 1, 2, 3)
                for m in range(4):
                    P.act(sq[:, :], qk[:, m, :], AF.Square)
                    nb = pb[4]
                    P.mm(nb[:, :], self.c("blk64"), sq[:, :])
                    P.act(rsn[:, :], nb[:, :], AF.Ln, bias=EPS)
                    P.act(rsn[:, :], rsn[:, :], AF.Exp, scale=-0.5)
                    if m < 2:
                        P.stt("dve", qk[:, m, :], qk[:, m, :], 0.125, rsn[:, :], ALU.mult, ALU.mult)
                    else:
                        P.tt("pool", qk[:, m, :], qk[:, m, :], rsn[:, :], ALU.mult)
                    yield
                for j in range(4):
                    i = c * 4 + j
                    bank = pb[4]
                    self._proj_tm(wt, 768, 256, i, bank)
                    P.act(zsb[c % 2][:, j, :], bank[:, 0:256], AF.Silu)
                    yield

            def run_rr(gens):
                gens = list(gens)
                while gens:
                    for g in list(gens):
                        try:
                            next(g)
                        except StopIteration:
                            gens.remove(g)

            prev = None
            run_rr([chunk_front(0)])
            for c in range(4):
                for j in range(4):
                    i = c * 4 + j
                    prep_common(i, qkv[c % 2])
                    gens = [head_chain(i, qkv[c % 2], h) for h in range(4)]
                    if prev is not None:
                        gens.append(scan(prev))
                    if j == 1 and c + 1 < 4:
                        gens.append(chunk_front(c + 1))
                    run_rr(gens)
                    prev = i
            run_rr([scan(prev)])


def _prep_inputs(inputs, n_layers=2):
    f = lambda k: np.ascontiguousarray(np.asarray(inputs[k], dtype=np.float32)[:n_layers])
    shared = {"w_in": f("w_in"), "w_gate": f("w_gate"), "w_branch": f("w_branch"), "w_out": f("w_out"),
              "w_up": f("w_up"), "w_down": f("w_down"), "consts": CONST_ARR,
              "lp": np.stack([_layer_params(inputs, l) for l in range(n_layers)])}
    return shared


def kernel(**inputs):
    x = np.ascontiguousarray(np.asarray(inputs["x"], dtype=np.float32))
    n_cores = 8
    per = x.shape[0] // n_cores
    shared = _prep_inputs(inputs)
    nc = K(n_seq=per).build()
    in_maps = []
    for c in range(n_cores):
        m = dict(shared)
        m["x"] = x[c * per:(c + 1) * per].reshape(per * T, D)
        in_maps.append(m)
    res = run_bass_kernel_spmd(nc, in_maps, core_ids=list(range(n_cores)))
    out = np.stack([np.asarray(r["out"]).reshape(per, T, D) for r in res.results], axis=0)
    return out.reshape(x.shape).astype(np.float32)
```
